# Optimizing a Trainium2 kernel written in Bass

```python
import math
import jax
import jax.numpy as jnp
from jax import lax
import numpy as np

D_MODEL = 1024
BATCH = 1
SEQ = 16384
DEPTH = 2

GRID_W = 64
CTX_LEN = 256
N_EVEN = (DEPTH + 1) // 2
N_ODD = DEPTH // 2
N_MOD = 6
EPS = 1e-6

ATTN_HEADS = 4
ATTN_KV_HEADS = 2
ATTN_GROUP = ATTN_HEADS // ATTN_KV_HEADS
HEAD_DIM = 128
ROPE_THETA = 10000.0
ROPE_PAIRS = HEAD_DIM // 4
Q_BLOCK = 128

MLSTM_HEADS = 4
MLSTM_DK = 64
MLSTM_DV = 128
MLSTM_CHUNK = 128
N_DIRS = 2

A_Q = ATTN_HEADS * HEAD_DIM
A_KV = ATTN_KV_HEADS * HEAD_DIM
B_QK = MLSTM_HEADS * MLSTM_DK
B_V = MLSTM_HEADS * MLSTM_DV
B_G = N_DIRS * MLSTM_HEADS
EVEN_SPLITS = (A_Q, A_KV, A_KV, B_QK, B_QK, B_V, B_G, B_G, B_V)
EVEN_IN = A_Q + 2 * A_KV + 2 * B_QK + 2 * B_V + 2 * B_G
MIX_WIDTH = A_Q + B_V

HYENA_WIDTH = D_MODEL
SHORT_CONV = 3
FILTER_EMB = 33
FILTER_BANDS = (FILTER_EMB - 1) // 2
FILTER_HIDDEN = 64
FILTER_OUT_SCALE = 0.02
DECAY_TARGET = 1e-2
FAST_DECAY = 0.3
SLOW_DECAY = 1.5

N_EXPERTS = 32
TOP_K = 4
D_EXPERT = D_MODEL
SWIGLU_LIMIT = 7.0
SWIGLU_ALPHA = 1.702
MOE_BLOCK = 256

kernel_name = 'hybrid_gqa_mlstm_hyena_moe_dit'


def rmsnorm(x, gain):
    xf = x.astype(jnp.float32)
    y = xf * lax.rsqrt(jnp.mean(xf * xf, axis=-1, keepdims=True) + EPS)
    return y.astype(x.dtype) * gain


def modulate(x, shift, scale):
    return x * (1.0 + scale) + shift


def rope_tables(rows):
    row = jnp.repeat(jnp.arange(rows, dtype=jnp.int32), GRID_W)
    col = jnp.tile(jnp.arange(GRID_W, dtype=jnp.int32), rows)
    pos = jnp.stack([row, col], axis=-1).astype(jnp.float32)
    inv_freq = ROPE_THETA ** (-jnp.arange(ROPE_PAIRS, dtype=jnp.float32) / ROPE_PAIRS)
    ang = pos[:, :, None] * inv_freq
    return jnp.cos(ang), jnp.sin(ang)


def apply_rope(x, cos, sin):
    xs = x.astype(jnp.float32).reshape(*x.shape[:-1], 2, 2, ROPE_PAIRS)
    x1, x2 = xs[..., 0, :], xs[..., 1, :]
    c = cos[None, :, None]
    s = sin[None, :, None]
    out = jnp.stack([x1 * c - x2 * s, x2 * c + x1 * s], axis=-2)
    return out.reshape(x.shape).astype(x.dtype)


def attend(q, k, v):
    s = jnp.einsum('bqhgd,bkhd->bhgqk', q, k, preferred_element_type=jnp.float32) * (HEAD_DIM ** -0.5)
    p = jax.nn.softmax(s, axis=-1).astype(v.dtype)
    return jnp.einsum('bhgqk,bkhd->bqhgd', p, v)


def mlstm_chunkwise(q, k, v, log_i, log_f, state, return_h):
    L = q.shape[-2]
    nc = L // MLSTM_CHUNK
    n_lead = q.ndim - 2

    def to_chunks(a):
        lead = a.shape[:n_lead]
        a = a.reshape(*lead, nc, MLSTM_CHUNK, *a.shape[n_lead + 1:])
        return jnp.moveaxis(a, n_lead, 0)

    tril = jnp.tril(jnp.ones((MLSTM_CHUNK, MLSTM_CHUNK), dtype=bool))

    def step(carry, inp):
        C, n, m = carry
        qc, kc, vc, ic, fc = inp
        b = jnp.cumsum(fc, axis=-1)
        log_d = jnp.where(tril, b[..., :, None] - b[..., None, :] + ic[..., None, :], -jnp.inf)
        inter = b + m[..., None]
        m_row = jnp.maximum(inter, jnp.max(log_d, axis=-1))
        w_inter = jnp.exp(inter - m_row)
        s = jnp.einsum('...td,...sd->...ts', qc, kc) * jnp.exp(log_d - m_row[..., None])
        num = jnp.einsum('...ts,...sv->...tv', s, vc) + w_inter[..., None] * jnp.einsum('...vd,...td->...tv', C, qc)
        den = jnp.sum(s, axis=-1) + w_inter * jnp.einsum('...d,...td->...t', n, qc)
        h = num / jnp.maximum(jnp.abs(den), jnp.exp(-m_row))[..., None]
        b_last = b[..., -1]
        log_w = b_last[..., None] - b + ic
        m_new = jnp.maximum(b_last + m, jnp.max(log_w, axis=-1))
        w = jnp.exp(log_w - m_new[..., None])
        decay = jnp.exp(b_last + m - m_new)
        C_new = decay[..., None, None] * C + jnp.einsum('...s,...sv,...sd->...vd', w, vc, kc)
        n_new = decay[..., None] * n + jnp.einsum('...s,...sd->...d', w, kc)
        return (C_new, n_new, m_new), (h if return_h else None)

    xs = (to_chunks(q), to_chunks(k), to_chunks(v), to_chunks(log_i), to_chunks(log_f))
    new_state, hs = lax.scan(step, state, xs)
    if not return_h:
        return None, new_state
    hs = jnp.moveaxis(hs, 0, -3)
    return hs.reshape(*hs.shape[:-3], L, hs.shape[-1]), new_state


def mlstm_bidir(q, k, v, ig, fg, f_bias, state, return_h):
    def dirs(a):
        a = jnp.moveaxis(a.astype(jnp.float32), 1, 2)
        return jnp.stack([a, jnp.flip(a, axis=2)], axis=0)

    def gate_dirs(g):
        g = jnp.transpose(g.astype(jnp.float32), (2, 0, 3, 1))
        return jnp.stack([g[0], jnp.flip(g[1], axis=-1)], axis=0)

    log_i = gate_dirs(ig)
    log_f = jax.nn.log_sigmoid(gate_dirs(fg) + f_bias.astype(jnp.float32)[:, None, :, None])
    h, new_state = mlstm_chunkwise(dirs(q) * (MLSTM_DK ** -0.5), dirs(k), dirs(v), log_i, log_f, state, return_h)
    if not return_h:
        return None, new_state
    h = h[0] + jnp.flip(h[1], axis=2)
    return jnp.moveaxis(h, 1, 2).astype(v.dtype), new_state


def even_mixer(a_lat, a_ctx, cos, sin, w_in, b_in, q_gain, k_gain, f_bias, h_gain, w_out, b_out, ctx_out):
    B, L, _ = a_lat.shape
    n_ctx = a_ctx.shape[1]
    offsets = np.cumsum(EVEN_SPLITS)[:-1].tolist()
    pl = jnp.split(a_lat @ w_in + b_in, offsets, axis=-1)
    pc = jnp.split(a_ctx @ w_in + b_in, offsets, axis=-1)

    def attn_heads(parts, rope):
        n = parts[0].shape[1]
        q = rmsnorm(parts[0].reshape(B, n, ATTN_HEADS, HEAD_DIM), q_gain)
        k = rmsnorm(parts[1].reshape(B, n, ATTN_KV_HEADS, HEAD_DIM), k_gain)
        if rope:
            q = apply_rope(q, cos, sin)
            k = apply_rope(k, cos, sin)
        return (q.reshape(B, n, ATTN_KV_HEADS, ATTN_GROUP, HEAD_DIM), k,
                parts[2].reshape(B, n, ATTN_KV_HEADS, HEAD_DIM))

    def mlstm_heads(parts):
        n = parts[3].shape[1]
        return (parts[3].reshape(B, n, MLSTM_HEADS, MLSTM_DK), parts[4].reshape(B, n, MLSTM_HEADS, MLSTM_DK),
                parts[5].reshape(B, n, MLSTM_HEADS, MLSTM_DV), parts[6].reshape(B, n, N_DIRS, MLSTM_HEADS),
                parts[7].reshape(B, n, N_DIRS, MLSTM_HEADS))

    def mlstm_out(h, og):
        n = h.shape[1]
        return jax.nn.sigmoid(og) * rmsnorm(h, h_gain.reshape(MLSTM_HEADS, MLSTM_DV)).reshape(B, n, B_V)

    ql, kl, vl = attn_heads(pl, True)
    qc, kc, vc = attn_heads(pc, False)
    k_all = jnp.concatenate([kl, kc], axis=1)
    v_all = jnp.concatenate([vl, vc], axis=1)
    qb = jnp.moveaxis(ql.reshape(B, L // Q_BLOCK, Q_BLOCK, ATTN_KV_HEADS, ATTN_GROUP, HEAD_DIM), 1, 0)
    att_l = lax.map(lambda qq: attend(qq, k_all, v_all), qb)
    att_l = jnp.moveaxis(att_l, 0, 1).reshape(B, L, A_Q)

    state0 = (jnp.zeros((N_DIRS, B, MLSTM_HEADS, MLSTM_DV, MLSTM_DK), jnp.float32),
              jnp.zeros((N_DIRS, B, MLSTM_HEADS, MLSTM_DK), jnp.float32),
              jnp.zeros((N_DIRS, B, MLSTM_HEADS), jnp.float32))
    h_c, st_ctx = mlstm_bidir(*mlstm_heads(pc), f_bias, state0, ctx_out)
    h_l, _ = mlstm_bidir(*mlstm_heads(pl), f_bias, st_ctx, True)

    y_lat = jnp.concatenate([att_l, mlstm_out(h_l, pl[8])], axis=-1) @ w_out + b_out
    if not ctx_out:
        return y_lat, None
    att_c = attend(qc, kc, vc).reshape(B, n_ctx, A_Q)
    y_ctx = jnp.concatenate([att_c, mlstm_out(h_c, pc[8])], axis=-1) @ w_out + b_out
    return y_lat, y_ctx


def short_conv(z, w, b):
    ch = z.shape[-1]
    y = lax.conv_general_dilated(z, w[:, None, :].astype(z.dtype), window_strides=(1,), padding=((1, 1),),
                                 dimension_numbers=('NWC', 'WIO', 'NWC'), feature_group_count=ch)
    return y + b


def hyena_filters(n, w1, b1, freq, w2, b2, w3, b3, w4):
    f32 = jnp.float32
    t = jnp.linspace(0.0, 1.0, n, dtype=f32)[:, None]
    w = (2.0 * math.pi / n) * jnp.arange(n, dtype=f32)[:, None]
    f = jnp.linspace(1e-4, FILTER_BANDS - 1, FILTER_BANDS, dtype=f32)
    z = jnp.concatenate([t, jnp.cos(f * w), -jnp.sin(f * w)], axis=-1)
    fr = freq.astype(f32)
    h = jnp.sin(fr * (z @ w1.astype(f32) + b1.astype(f32)))
    h = jnp.sin(fr * (h @ w2.astype(f32) + b2.astype(f32)))
    h = jnp.sin(fr * (h @ w3.astype(f32) + b3.astype(f32)))
    h = h @ w4.astype(f32)
    deltas = jnp.abs(jnp.linspace(math.log(DECAY_TARGET) / SLOW_DECAY, math.log(DECAY_TARGET) / FAST_DECAY,
                                  HYENA_WIDTH, dtype=f32))
    decay = jnp.exp(-t * deltas)
    return h[:, :HYENA_WIDTH] * decay, h[:, HYENA_WIDTH:] * decay


def long_conv(v, h_fwd, h_bwd):
    L, ch = h_fwd.shape
    taps = jnp.concatenate([h_fwd, jnp.zeros((1, ch), h_fwd.dtype), h_bwd[:0:-1]], axis=0)
    vf = jnp.fft.rfft(v.astype(jnp.float32), n=2 * L, axis=1)
    y = jnp.fft.irfft(vf * jnp.fft.rfft(taps, axis=0)[None], n=2 * L, axis=1)[:, :L]
    return y.astype(v.dtype)


def hyena_mixer(u, w_in, b_in, conv_w, conv_b, fw1, fb1, ffreq, fw2, fb2, fw3, fb3, fw4, skip, w_out, b_out):
    n = u.shape[1]
    z = short_conv(u @ w_in + b_in, conv_w, conv_b)
    x0, x1, v = jnp.split(z, 3, axis=-1)
    h_fwd, h_bwd = hyena_filters(n, fw1, fb1, ffreq, fw2, fb2, fw3, fb3, fw4)
    v = v * x1
    v = long_conv(v, h_fwd, h_bwd) + v * skip
    return (v * x0) @ w_out + b_out


def moe_ffn(xt, router_w, router_b, w_gu, b_gu, w_down, b_down):
    T, D = xt.shape
    M = T * TOP_K
    n_blocks = -(-M // MOE_BLOCK) + N_EXPERTS
    logits = (xt @ router_w + router_b).astype(jnp.float32)
    top_logit, top_e = lax.top_k(logits, TOP_K)
    gate = jax.nn.softmax(top_logit, axis=-1)
    e_flat = top_e.reshape(M)
    order = jnp.argsort(e_flat)
    e_sorted = e_flat[order]
    tok_sorted = order // TOP_K
    g_sorted = gate.reshape(M)[order].astype(xt.dtype)
    counts = jnp.bincount(e_flat, length=N_EXPERTS)
    padded = (counts + MOE_BLOCK - 1) // MOE_BLOCK * MOE_BLOCK
    starts = jnp.cumsum(counts) - counts
    pad_ends = jnp.cumsum(padded)
    pad_starts = pad_ends - padded
    dest = pad_starts[e_sorted] + jnp.arange(M) - starts[e_sorted]
    buf = jnp.zeros((n_blocks * MOE_BLOCK, D), xt.dtype).at[dest].set(xt[tok_sorted])
    blk_e = jnp.minimum(jnp.searchsorted(pad_ends, jnp.arange(n_blocks) * MOE_BLOCK, side='right'), N_EXPERTS - 1)

    def expert_block(args):
        rows, e = args
        gu = rows @ w_gu[e] + b_gu[e]
        g = jnp.minimum(gu[:, :D_EXPERT], SWIGLU_LIMIT)
        u = jnp.clip(gu[:, D_EXPERT:], -SWIGLU_LIMIT, SWIGLU_LIMIT)
        return (g * jax.nn.sigmoid(SWIGLU_ALPHA * g) * (u + 1.0)) @ w_down[e] + b_down[e]

    out = lax.map(expert_block, (buf.reshape(n_blocks, MOE_BLOCK, D), blk_e)).reshape(-1, D)
    return jax.ops.segment_sum(out[dest] * g_sorted[:, None], tok_sorted, num_segments=T)


def setup_inputs(seed: int = 0) -> dict:
    key = jax.random.key(seed)
    keys = jax.random.split(key, 48)
    counter = [0]
    f32 = jnp.float32
    D = D_MODEL

    def nrm(shape, scale):
        k = keys[counter[0]]
        counter[0] += 1
        return jax.random.normal(k, shape, f32) * scale

    def gain(shape):
        return 1.0 + nrm(shape, 0.02)

    return {
        'x': nrm((BATCH, SEQ, D), 1.0),
        'c': nrm((BATCH, D), 1.0),
        'ctx': nrm((BATCH, CTX_LEN, D), 1.0),
        'c_ctx': nrm((D,), 1.0),
        'ada_w': nrm((DEPTH, D, N_MOD * D), 0.5 * D ** -0.5),
        'ada_b': nrm((DEPTH, N_MOD * D), 0.02),
        'norm1_g': gain((DEPTH, D)),
        'norm2_g': gain((DEPTH, D)),
        'ev_w_in': nrm((N_EVEN, D, EVEN_IN), D ** -0.5),
        'ev_b_in': nrm((N_EVEN, EVEN_IN), 0.02),
        'ev_q_gain': gain((N_EVEN, HEAD_DIM)),
        'ev_k_gain': gain((N_EVEN, HEAD_DIM)),
        'ev_f_bias': jnp.linspace(3.0, 6.0, MLSTM_HEADS, dtype=f32)[None, None, :] + nrm((N_EVEN, N_DIRS, MLSTM_HEADS), 0.1),
        'ev_h_gain': gain((N_EVEN, B_V)),
        'ev_w_out': nrm((N_EVEN, MIX_WIDTH, D), MIX_WIDTH ** -0.5),
        'ev_b_out': nrm((N_EVEN, D), 0.02),
        'od_w_in': nrm((N_ODD, D, 3 * HYENA_WIDTH), D ** -0.5),
        'od_b_in': nrm((N_ODD, 3 * HYENA_WIDTH), 0.02),
        'od_conv_w': nrm((N_ODD, SHORT_CONV, 3 * HYENA_WIDTH), SHORT_CONV ** -0.5),
        'od_conv_b': nrm((N_ODD, 3 * HYENA_WIDTH), 0.02),
        'od_filt_w1': nrm((N_ODD, FILTER_EMB, FILTER_HIDDEN), FILTER_EMB ** -0.5),
        'od_filt_b1': nrm((N_ODD, FILTER_HIDDEN), 0.1),
        'od_filt_freq': gain((N_ODD, FILTER_HIDDEN)),
        'od_filt_w2': nrm((N_ODD, FILTER_HIDDEN, FILTER_HIDDEN), FILTER_HIDDEN ** -0.5),
        'od_filt_b2': nrm((N_ODD, FILTER_HIDDEN), 0.1),
        'od_filt_w3': nrm((N_ODD, FILTER_HIDDEN, FILTER_HIDDEN), FILTER_HIDDEN ** -0.5),
        'od_filt_b3': nrm((N_ODD, FILTER_HIDDEN), 0.1),
        'od_filt_w4': nrm((N_ODD, FILTER_HIDDEN, 2 * HYENA_WIDTH), FILTER_OUT_SCALE * FILTER_HIDDEN ** -0.5),
        'od_skip': nrm((N_ODD, HYENA_WIDTH), 1.0),
        'od_w_out': nrm((N_ODD, HYENA_WIDTH, D), HYENA_WIDTH ** -0.5),
        'od_b_out': nrm((N_ODD, D), 0.02),
        'router_w': nrm((DEPTH, D, N_EXPERTS), D ** -0.5),
        'router_b': nrm((DEPTH, N_EXPERTS), 0.01),
        'moe_w_gu': nrm((DEPTH, N_EXPERTS, D, 2 * D_EXPERT), D ** -0.5),
        'moe_b_gu': nrm((DEPTH, N_EXPERTS, 2 * D_EXPERT), 0.02),
        'moe_w_down': nrm((DEPTH, N_EXPERTS, D_EXPERT, D), D_EXPERT ** -0.5),
        'moe_b_down': nrm((DEPTH, N_EXPERTS, D), 0.02),
        'final_g': gain((D,)),
    }


def reference(x, c, ctx, c_ctx, ada_w, ada_b, norm1_g, norm2_g, ev_w_in, ev_b_in, ev_q_gain, ev_k_gain,
              ev_f_bias, ev_h_gain, ev_w_out, ev_b_out, od_w_in, od_b_in, od_conv_w, od_conv_b, od_filt_w1,
              od_filt_b1, od_filt_freq, od_filt_w2, od_filt_b2, od_filt_w3, od_filt_b3, od_filt_w4, od_skip,
              od_w_out, od_b_out, router_w, router_b, moe_w_gu, moe_b_gu, moe_w_down, moe_b_down, final_g):
    B, L, D = x.shape
    n_ctx = ctx.shape[1]
    rows = L // GRID_W
    cos, sin = rope_tables(rows)
    h, hc = x, ctx
    for i in range(DEPTH):
        j = i // 2
        is_even = i % 2 == 0
        ctx_out = any(k % 2 == 0 for k in range(i + 1, DEPTH))
        need_ctx_in = is_even or ctx_out
        mod = jnp.split((jax.nn.silu(c) @ ada_w[i] + ada_b[i])[:, None, :], N_MOD, axis=-1)
        a = modulate(rmsnorm(h, norm1_g[i]), mod[0], mod[1])
        if need_ctx_in:
            cmod = jnp.split(jax.nn.silu(c_ctx) @ ada_w[i] + ada_b[i], N_MOD, axis=-1)
            ac = modulate(rmsnorm(hc, norm1_g[i]), cmod[0], cmod[1])
        if is_even:
            y, yc = even_mixer(a, ac, cos, sin, ev_w_in[j], ev_b_in[j], ev_q_gain[j], ev_k_gain[j], ev_f_bias[j],
                               ev_h_gain[j], ev_w_out[j], ev_b_out[j], ctx_out)
        else:
            hy = (od_w_in[j], od_b_in[j], od_conv_w[j], od_conv_b[j], od_filt_w1[j], od_filt_b1[j],
                  od_filt_freq[j], od_filt_w2[j], od_filt_b2[j], od_filt_w3[j], od_filt_b3[j], od_filt_w4[j],
                  od_skip[j], od_w_out[j], od_b_out[j])
            y = hyena_mixer(a, *hy)
            yc = hyena_mixer(ac, *hy) if ctx_out else None
        h = h + mod[2] * y
        tokens = [modulate(rmsnorm(h, norm2_g[i]), mod[3], mod[4]).reshape(B * L, D)]
        if ctx_out:
            hc = hc + cmod[2] * yc
            tokens.append(modulate(rmsnorm(hc, norm2_g[i]), cmod[3], cmod[4]).reshape(B * n_ctx, D))
        f = moe_ffn(jnp.concatenate(tokens, axis=0), router_w[i], router_b[i], moe_w_gu[i], moe_b_gu[i],
                    moe_w_down[i], moe_b_down[i])
        h = h + mod[5] * f[:B * L].reshape(B, L, D)
        if ctx_out:
            hc = hc + cmod[5] * f[B * L:].reshape(B, n_ctx, D)
    return rmsnorm(h, final_g)
```

```python
import numpy as np
import concourse.bass as bass
import concourse.mybir as mybir

F32 = mybir.dt.float32
BF16 = mybir.dt.bfloat16
I32 = mybir.dt.int32
U32 = mybir.dt.uint32
AF = mybir.ActivationFunctionType
ALU = mybir.AluOpType
AX = mybir.AxisListType


class Sched:
    CE = ("pe", "act", "dve", "pool", "sp")

    def __init__(self, nc, n_dma_sems=8):
        self.nc = nc
        self.ops = {e: [] for e in self.CE}
        self.sem = {}
        self.cnt = {}
        self.seen = {e: {} for e in self.CE}
        self.last_w = {}
        self.readers = {}
        self._stack = []
        for e in self.CE:
            self.sem[e] = nc.alloc_semaphore(name="s_" + e)
            self.cnt[e] = 0
        self.dq = {}
        for q in ("sp", "act", "pool"):
            lst = []
            for i in range(n_dma_sems):
                nm = "d_%s%d" % (q, i)
                self.sem[nm] = nc.alloc_semaphore(name=nm)
                self.cnt[nm] = 0
                lst.append(nm)
            self.dq[q] = [lst, 0]
        self.n_inst = 0
        self.limit = None
        self.nops = 0

    def _deps(self, eng, reads, writes):
        deps = {}
        def add(tok):
            f, v = tok
            if deps.get(f, 0) < v:
                deps[f] = v
        for k in reads:
            t = self.last_w.get(k)
            if t is not None:
                add(t)
        for k in writes:
            t = self.last_w.get(k)
            if t is not None:
                add(t)
            for t in self.readers.get(k, {}).items():
                if t[0] != eng:
                    add(t)
        out = []
        for f, v in deps.items():
            if f == eng and eng == "pe":
                continue
            if self.seen[eng].get(f, 0) >= v:
                continue
            self.seen[eng][f] = v
            out.append((f, v))
        return out

    def _commit(self, tok, reads, writes):
        for k in writes:
            self.last_w[k] = tok
            self.readers[k] = {}
        for k in reads:
            self.readers.setdefault(k, {})[tok[0]] = tok[1]

    def op(self, eng, fn, reads=(), writes=()):
        self.nops += 1
        if self.limit is not None and self.nops > self.limit:
            return None
        waits = self._deps(eng, reads, writes)
        self.cnt[eng] += 1
        tok = (eng, self.cnt[eng])
        sem = self.sem[eng]
        sems = self.sem
        def emit(e, fn=fn, waits=waits, sem=sem):
            for f, v in waits:
                e.wait_ge(sems[f], v)
            fn(e).then_inc(sem, 1)
        self.ops[eng].append(emit)
        self._commit(tok, reads, writes)
        self.n_inst += 1 + len(waits)
        return tok

    def dma(self, q, out, in_, reads=(), writes=(), **kw):
        self.nops += 1
        if self.limit is not None and self.nops > self.limit:
            return None
        lst, idx = self.dq[q]
        nm = lst[idx % len(lst)]
        self.dq[q][1] = idx + 1
        waits = self._deps(q, reads, writes)
        prev = self.cnt[nm]
        if prev > 0 and self.seen[q].get(nm, 0) < prev:
            self.seen[q][nm] = prev
            waits.append((nm, prev))
        self.cnt[nm] += 16
        tok = (nm, self.cnt[nm])
        sem = self.sem[nm]
        sems = self.sem
        def emit(e, waits=waits, sem=sem, out=out, in_=in_, kw=kw):
            for f, v in waits:
                e.wait_ge(sems[f], v)
            e.dma_start(out=out, in_=in_, **kw).then_inc(sem, 16)
        self.ops[q].append(emit)
        self._commit(tok, reads, writes)
        self.n_inst += 1 + len(waits)
        return tok

    def barrier(self):
        sems = self.sem
        for e in self.CE:
            waits = []
            for f, v in self.cnt.items():
                if f == e or v == 0:
                    continue
                if self.seen[e].get(f, 0) >= v:
                    continue
                self.seen[e][f] = v
                waits.append((f, v))
            if waits:
                def emit(eng, waits=waits):
                    for f, v in waits:
                        eng.wait_ge(sems[f], v)
                self.ops[e].append(emit)
                self.n_inst += len(waits)
        self.last_w = {}
        self.readers = {}

    def finish(self, out_keys):
        self.barrier()
        waits = []
        sems = self.sem
        def emit(e, waits=waits):
            for f, v in waits:
                e.wait_ge(sems[f], v)
        self.ops["sp"].append(emit)
        nc = self.nc
        ops = self.ops
        with nc.Block() as block:
            @block.tensor
            def _(e):
                for f in ops["pe"]:
                    f(e)
            @block.scalar
            def _(e):
                for f in ops["act"]:
                    f(e)
            @block.vector
            def _(e):
                for f in ops["dve"]:
                    f(e)
            @block.gpsimd
            def _(e):
                for f in ops["pool"]:
                    f(e)
            @block.sync
            def _(e):
                for f in ops["sp"]:
                    f(e)


from contextlib import ExitStack

D = 1024
NE = 32
EPS = 1e-6


class K:
    def __init__(self, nc, es):
        self.nc = nc
        self.es = es
        self.S = Sched(nc)
        self.nm = 0

    def sb(self, name, shape, dt=F32):
        return self.es.enter_context(self.nc.sbuf_tensor("s_" + name, shape, dt))

    def ps(self, name, shape, dt=F32):
        return self.es.enter_context(self.nc.psum_tensor("p_" + name, shape, dt))

    def dram(self, name, shape, dt=F32, kind="Internal"):
        return self.nc.dram_tensor(name, shape, dt, kind=kind).ap()

    def inp(self, name, shape, dt=F32):
        return self.nc.dram_tensor(name, shape, dt, kind="ExternalInput").ap()

    def outp(self, name, shape, dt=F32):
        return self.nc.dram_tensor(name, shape, dt, kind="ExternalOutput").ap()


class Rot:
    def __init__(self, k, name, shape, dt=F32, n=2, psum=False):
        self.t = [(k.ps if psum else k.sb)("%s%d" % (name, i), shape, dt) for i in range(n)]
        self.keys = ["%s%d" % (name, i) for i in range(n)]
        self.i = 0

    def next(self):
        j = self.i % len(self.t)
        self.i += 1
        return self.t[j], self.keys[j]


def load_fm(k, dst, dst_key, src2d, n, ident, scratch, scratch_key, pst, pst_key):
    S = k.S
    S.dma("sp", scratch[0:n, 0:128], src2d, writes=[scratch_key])
    S.op("pe", lambda e: e.transpose(out=pst[:, 0:n], in_=scratch[0:n, 0:128], identity=ident[0:n, 0:n]),
         reads=[scratch_key, "ident"], writes=[pst_key])
    S.op("dve", lambda e: e.tensor_copy(out=dst, in_=pst[:, 0:n]), reads=[pst_key], writes=[dst_key])


def adaln_fm(k, layer, c_in, cctx_in, ada_w, ada_b, ident, modfm, tmp):
    S = k.S
    sc, pst, wbuf = tmp["sc"], tmp["pst"], tmp["wbuf"]
    S.dma("sp", sc[0:8, 0:128], c_in.rearrange("o (k p) -> (o k) p", p=128), writes=["sc"])
    S.dma("sp", sc[8:16, 0:128], cctx_in.rearrange("(k p) -> k p", p=128), writes=["sc"])
    S.op("act", lambda e: e.activation(out=sc[0:16, 128:256], in_=sc[0:16, 0:128], func=AF.Silu), reads=["sc"], writes=["sc"])
    S.op("pe", lambda e: e.transpose(out=pst[:, 0:16], in_=sc[0:16, 128:256], identity=ident[0:16, 0:16]),
         reads=["sc", "ident"], writes=["pst"])
    cfm = tmp["cfm"]
    S.op("dve", lambda e: e.tensor_copy(out=cfm[:], in_=pst[:, 0:16]), reads=["pst"], writes=["cfm"])
    bfm = tmp["bfm"]
    load_fm(k, bfm[:], "bfm", ada_b[layer].rearrange("(j p) -> j p", p=128), 48, ident, sc, "sc", pst, "pst")
    aw = ada_w[layer].rearrange("(kc p) n -> p kc n", p=128)
    pm = tmp["pm"]
    for g in range(12):
        wt, wk = wbuf.next()
        S.dma("sp" if g % 2 == 0 else "act", wt[:], aw[:, :, g * 512:(g + 1) * 512], writes=[wk])
        for j in range(4):
            jj = g * 4 + j
            for kc in range(8):
                S.op("pe", lambda e, wt=wt, j=j, kc=kc, jj=jj: e.matmul(
                    pm[:, jj * 2:jj * 2 + 2], wt[:, kc, j * 128:(j + 1) * 128],
                    cfm[:, kc:kc + 9:8], start=(kc == 0), stop=(kc == 7)),
                    reads=[wk, "cfm"], writes=["pm"])
    for col in range(2):
        S.op("dve", lambda e, col=col: e.tensor_tensor(out=modfm[:, :, col], in0=pm[:, col:96:2], in1=bfm[:], op=ALU.add),
             reads=["pm", "bfm"], writes=["modfm"])


def bcast_rows(k, dst, dst_key, src_fm, src_key, n, ident, ones, diag, pb):
    S = k.S
    for j in range(n):
        dg, dk = diag.next()
        pt, pk = pb.next()
        S.op("dve", lambda e, dg=dg, j=j: e.tensor_scalar(out=dg[:], in0=ident[:], scalar1=src_fm[:, j:j + 1], scalar2=None, op0=ALU.mult),
             reads=["ident", src_key], writes=[dk])
        S.op("pe", lambda e, dg=dg, pt=pt: e.matmul(pt[:, 0:128], ones[:], dg[:], start=True, stop=True),
             reads=["ones", dk], writes=[pk])
        S.op("act", lambda e, pt=pt, j=j: e.copy(out=dst[:, j * 128:(j + 1) * 128], in_=pt[:, 0:128]),
             reads=[pk], writes=[dst_key])


class Arena:
    def __init__(self, k, nbytes=212000):
        self.k = k
        self.t = k.es.enter_context(k.nc.sbuf_tensor("arena", [128, nbytes], mybir.dt.uint8))
        self.n = nbytes
        self.off = 0

    def alloc(self, shape, dt=F32):
        sz = {F32: 4, BF16: 2, I32: 4, U32: 4}[dt]
        n = sz
        for s in shape[1:]:
            n *= s
        n_al = (n + 63) // 64 * 64
        assert self.off + n_al <= self.n, ("arena overflow", self.off, n_al, self.n)
        v = self.t[:, self.off:self.off + n].bitcast(dt)
        self.off += n_al
        if len(shape) == 3:
            v = v.rearrange("p (a b) -> p a b", a=shape[1])
        elif len(shape) == 4:
            v = v.rearrange("p (a b c) -> p a b c", a=shape[1], b=shape[2])
        if shape[0] < 128:
            v = v[0:shape[0]]
        return v

    def mark(self):
        return self.off

    def release(self, m):
        self.off = m


class RotA:
    def __init__(self, ar, name, shape, dt=F32, n=2):
        self.t = [ar.alloc(shape, dt) for _ in range(n)]
        self.keys = ["%s%d" % (name, i) for i in range(n)]
        self.i = 0

    def next(self):
        j = self.i % len(self.t)
        self.i += 1
        return self.t[j], self.keys[j]


def bc_load(k, dst, key, vec_ap, n):
    k.S.dma("sp", dst, vec_ap.rearrange("(o n) -> o n", o=1).broadcast_to([128, n]), writes=[key])


def build_post(nc, T=2048, TBM=1024, NEXP=32, final=False):
    NT = T // 128
    es = ExitStack()
    k = K(nc, es)
    S = k.S
    h_d = k.inp("h", [T, D]); mixT_d = k.inp("mixT", [D, T], BF16)
    wout_d = k.inp("w_out", [D, D]); bout_d = k.inp("b_out", [D])
    n2g_d = k.inp("norm2_g", [D]); modv_d = k.inp("modv", [6, D])
    rw_d = k.inp("router_w", [D, NE]); rb_d = k.inp("router_b", [NE])
    wgu_d = k.inp("w_gu", [NEXP, D, 2 * D]); bgu_d = k.inp("b_gu", [NE, 2 * D])
    wdn_d = k.inp("w_down", [NEXP, D, D]); bdn_d = k.inp("b_down", [NE, D])
    fg_d = k.inp("final_g", [D]); ident_d = k.inp("ident", [128, 128])
    out_d = k.outp("hout", [T, D])
    hmid_d = k.dram("hmid", [T, D])

    ident = k.sb("ident", [128, 128]); S.dma("sp", ident[:], ident_d, writes=["ident"])
    g2bc = k.sb("g2bc", [128, D]); bc_load(k, g2bc[:], "g2bc", modv_d[2], D)
    g5bc = k.sb("g5bc", [128, D]); bc_load(k, g5bc[:], "g5bc", modv_d[5], D)
    boutbc = k.sb("boutbc", [128, D]); bc_load(k, boutbc[:], "boutbc", bout_d, D)
    rbbc = k.sb("rbbc", [128, NE]); bc_load(k, rbbc[:], "rbbc", rb_d, NE)
    if final:
        fgbc = k.sb("fgbc", [128, D]); bc_load(k, fgbc[:], "fgbc", fg_d, D)
    pA = Rot(k, "pA", [128, 1024], n=2, psum=True)
    pB = Rot(k, "pB", [128, 512], n=2, psum=True)
    pC = Rot(k, "pC", [128, 512], n=2, psum=True)
    sc = k.sb("sc", [128, 128])
    fmv = k.sb("fmv", [128, 3, 8])
    for i, src in enumerate((n2g_d, modv_d[3], modv_d[4])):
        pt, pk = pC.next()
        load_fm(k, fmv[:, i, :], "fmv", src.rearrange("(j p) -> j p", p=128), 8, ident, sc, "sc", pt, pk)
    A2 = k.sb("A2", [128, 8])
    S.op("dve", lambda e: e.scalar_tensor_tensor(out=A2[:], in0=fmv[:, 2, :], scalar=1.0, in1=fmv[:, 0, :], op0=ALU.add, op1=ALU.mult),
         reads=["fmv"], writes=["A2"])
    rw = k.sb("rw", [128, 8, NE]); S.dma("sp", rw[:], rw_d.rearrange("(kc p) n -> p kc n", p=128), writes=["rw"])
    bgu = k.sb("bgu", [128, 16, NE])
    bgu_tm = k.sb("bgu_tm", [NE, 2 * D]); S.dma("sp", bgu_tm[:], bgu_d, writes=["bgu_tm"])
    for j in range(16):
        pt, pk = pC.next()
        S.op("pe", lambda e, pt=pt, j=j: e.transpose(out=pt[:, 0:NE], in_=bgu_tm[:, j * 128:(j + 1) * 128], identity=ident[0:NE, 0:NE]),
             reads=["bgu_tm", "ident"], writes=[pk])
        S.op("dve", lambda e, pt=pt, j=j: e.tensor_copy(out=bgu[:, j, :], in_=pt[:, 0:NE]), reads=[pk], writes=["bgu"])
    bdn = k.sb("bdn", [NE, D]); S.dma("sp", bdn[:], bdn_d, writes=["bdn"])
    wdn = k.sb("wdn", [128, 8, D], BF16)
    S.dma("pool", wdn[:], wout_d.rearrange("(kc p) n -> p kc n", p=128), writes=["wdn"])

    a2T = k.sb("a2T", [128, 8, T], BF16)
    gate = k.sb("gate", [128, NT, NE])
    ssq = k.sb("ssq", [128, NT]); rstd = k.sb("rstd", [128, NT])
    hin = Rot(k, "hin", [128, D], n=2)
    tmpf = Rot(k, "tmpf", [128, D], n=2)
    mixt = Rot(k, "mixt", [128, 8, 128], BF16, n=2)
    a2f = Rot(k, "a2f", [128, 8, 128], n=2)
    junk = k.sb("junk", [128, D], BF16)
    sm = Rot(k, "sm", [128, 4, NE], n=2)
    mixv = mixT_d.rearrange("(kc p) t -> p kc t", p=128)

    for tt in range(NT):
        ht, hk = hin.next(); mt, mk_ = mixt.next(); tf, tk = tmpf.next()
        S.dma("sp", ht[:], h_d[tt * 128:(tt + 1) * 128, :], writes=[hk])
        S.dma("act", mt[:], mixv[:, :, tt * 128:(tt + 1) * 128], writes=[mk_])
        py, pyk = pA.next()
        for cg in range(2):
            for kc in range(8):
                S.op("pe", lambda e, py=py, mt=mt, cg=cg, kc=kc: e.matmul(
                    py[:, cg * 512:(cg + 1) * 512], mt[:, kc, :], wdn[:, kc, cg * 512:(cg + 1) * 512],
                    start=(kc == 0), stop=(kc == 7)), reads=[mk_, "wdn"], writes=[pyk])
        S.op("dve", lambda e, tf=tf, py=py: e.tensor_tensor(out=tf[:], in0=py[:], in1=boutbc[:], op=ALU.add),
             reads=[pyk, "boutbc"], writes=[tk])
        S.op("pool", lambda e, tf=tf: e.tensor_tensor(out=tf[:], in0=tf[:], in1=g2bc[:], op=ALU.mult),
             reads=[tk, "g2bc"], writes=[tk])
        S.op("dve", lambda e, tf=tf, ht=ht: e.tensor_tensor(out=ht[:], in0=ht[:], in1=tf[:], op=ALU.add),
             reads=[tk, hk], writes=[hk])
        S.dma("sp", hmid_d[tt * 128:(tt + 1) * 128, :], ht[:], reads=[hk], writes=[("hmid", tt)])
        S.op("act", lambda e, ht=ht, tt=tt: e.activation(out=junk[:], in_=ht[:], func=AF.Square, accum_out=ssq[:, tt:tt + 1]),
             reads=[hk], writes=["junk", ("ssq", tt)])
        S.op("act", lambda e, tt=tt: e.activation(out=rstd[:, tt:tt + 1], in_=ssq[:, tt:tt + 1], func=AF.Sqrt, scale=1.0 / D, bias=EPS),
             reads=[("ssq", tt)], writes=[("rstd", tt)])
        S.op("dve", lambda e, tt=tt: e.reciprocal(out=rstd[:, tt:tt + 1], in_=rstd[:, tt:tt + 1]),
             reads=[("rstd", tt)], writes=[("rstd", tt)])
        S.op("dve", lambda e, tf=tf, ht=ht, tt=tt: e.tensor_scalar(out=tf[:], in0=ht[:], scalar1=rstd[:, tt:tt + 1], scalar2=None, op0=ALU.mult),
             reads=[hk, ("rstd", tt)], writes=[tk])
        pt, ptk = pA.next()
        for c in range(8):
            S.op("pe", lambda e, pt=pt, tf=tf, c=c: e.transpose(out=pt[:, c * 128:(c + 1) * 128], in_=tf[:, c * 128:(c + 1) * 128], identity=ident[:]),
                 reads=[tk, "ident"], writes=[ptk])
        af, ak = a2f.next()
        for c in range(8):
            S.op("act", lambda e, pt=pt, af=af, c=c: e.activation(out=af[:, c, :], in_=pt[:, c * 128:(c + 1) * 128], func=AF.Identity,
                                                                 scale=A2[:, c:c + 1], bias=fmv[:, 1, c:c + 1]),
                 reads=[ptk, "A2", "fmv"], writes=[ak])
        S.op("pool", lambda e, af=af, tt=tt: e.tensor_copy(out=a2T[:, :, tt * 128:(tt + 1) * 128], in_=af[:]),
             reads=[ak], writes=[("a2T", tt)])
        pl, plk = pC.next()
        for c in range(8):
            S.op("pe", lambda e, pl=pl, af=af, c=c: e.matmul(pl[:, 0:NE], af[:, c, :], rw[:, c, :], start=(c == 0), stop=(c == 7)),
                 reads=[ak, "rw"], writes=[plk])
        s4, sk = sm.next()
        S.op("dve", lambda e, s4=s4, pl=pl: e.tensor_tensor(out=s4[:, 0, :], in0=pl[:, 0:NE], in1=rbbc[:], op=ALU.add),
             reads=[plk, "rbbc"], writes=[sk])
        S.op("dve", lambda e, s4=s4: e.max(out=s4[:, 3, 0:8], in_=s4[:, 0, :]), reads=[sk], writes=[sk])
        S.op("dve", lambda e, s4=s4: e.tensor_scalar(out=s4[:, 1, :], in0=s4[:, 0, :], scalar1=s4[:, 3, 3:4], scalar2=None, op0=ALU.is_ge),
             reads=[sk], writes=[sk])
        S.op("dve", lambda e, s4=s4: e.tensor_scalar(out=s4[:, 3, 8:9], in0=s4[:, 3, 0:1], scalar1=-1.0, scalar2=None, op0=ALU.mult),
             reads=[sk], writes=[sk])
        S.op("act", lambda e, s4=s4: e.activation(out=s4[:, 2, :], in_=s4[:, 0, :], func=AF.Exp, bias=s4[:, 3, 8:9], scale=1.0),
             reads=[sk], writes=[sk])
        S.op("dve", lambda e, s4=s4: e.tensor_tensor(out=s4[:, 2, :], in0=s4[:, 2, :], in1=s4[:, 1, :], op=ALU.mult),
             reads=[sk], writes=[sk])
        S.op("dve", lambda e, s4=s4: e.tensor_reduce(out=s4[:, 3, 9:10], in_=s4[:, 2, :], axis=AX.X, op=ALU.add),
             reads=[sk], writes=[sk])
        S.op("dve", lambda e, s4=s4: e.reciprocal(out=s4[:, 3, 10:11], in_=s4[:, 3, 9:10]), reads=[sk], writes=[sk])
        S.op("dve", lambda e, s4=s4, tt=tt: e.tensor_scalar(out=gate[:, tt, :], in0=s4[:, 2, :], scalar1=s4[:, 3, 10:11], scalar2=None, op0=ALU.mult),
             reads=[sk], writes=[("gate", tt)])

    NTB = TBM // 128
    NB5 = TBM // 512
    acc = k.sb("acc", [128, NTB, D])
    hT = k.sb("hT", [128, 8, TBM], BF16)
    slab = Rot(k, "slab", [128, 8, 256], BF16, n=3)
    gsr = Rot(k, "gs", [128, 512], n=2); usr = Rot(k, "us", [128, 512], n=2); sgr = Rot(k, "sg", [128, 512], n=2)
    gTr = Rot(k, "gT", [NE, 128], n=2)
    wguv = wgu_d.rearrange("e (kc p) n -> e p kc n", p=128)
    wdnv = wdn_d.rearrange("e (kc p) n -> e p kc n", p=128)
    for ps_ in range(T // TBM):
        t0 = ps_ * NTB
        for tl in range(NTB):
            tt = t0 + tl
            pt, pk = pC.next(); gt, gk = gTr.next()
            S.op("pe", lambda e, pt=pt, tt=tt: e.transpose(out=pt[0:NE, 0:128], in_=gate[:, tt, :], identity=ident[:]),
                 reads=[("gate", tt), "ident"], writes=[pk])
            S.op("dve", lambda e, pt=pt, gt=gt: e.tensor_copy(out=gt[:], in_=pt[0:NE, 0:128]), reads=[pk], writes=[gk])
            pa, pak = pA.next()
            for cg in range(2):
                S.op("pe", lambda e, pa=pa, gt=gt, cg=cg: e.matmul(pa[:, cg * 512:(cg + 1) * 512], gt[:], bdn[:, cg * 512:(cg + 1) * 512], start=True, stop=True),
                     reads=[gk, "bdn"], writes=[pak])
            S.op("act", lambda e, pa=pa, tl=tl: e.copy(out=acc[:, tl, :], in_=pa[:]), reads=[pak], writes=[("acc", tl)])
        for ex in range(NEXP):
            S.dma("pool", wdn[:], wdnv[ex], writes=["wdn"])
            for j in range(8):
                sl, slk = slab.next()
                S.dma("pool", sl[:, :, 0:128], wguv[ex][:, :, j * 128:(j + 1) * 128], writes=[slk])
                S.dma("pool", sl[:, :, 128:256], wguv[ex][:, :, D + j * 128:D + (j + 1) * 128], writes=[slk])
                for tb in range(NB5):
                    pg, pgk = pA.next()
                    for half in range(2):
                        for kc in range(8):
                            S.op("pe", lambda e, pg=pg, sl=sl, half=half, kc=kc, tb=tb, t0=t0: e.matmul(
                                pg[:, half * 512:(half + 1) * 512], sl[:, kc, half * 128:(half + 1) * 128],
                                a2T[:, kc, t0 * 128 + tb * 512:t0 * 128 + (tb + 1) * 512], start=(kc == 0), stop=(kc == 7)),
                                reads=[slk] + [("a2T", t0 + tb * 4 + q) for q in range(4)], writes=[pgk])
                    gs, gsk = gsr.next(); us, usk = usr.next(); sg, sgk = sgr.next()
                    S.op("dve", lambda e, gs=gs, pg=pg, j=j, ex=ex: e.tensor_scalar(out=gs[:], in0=pg[:, 0:512], scalar1=bgu[:, j, ex:ex + 1], scalar2=7.0, op0=ALU.add, op1=ALU.min),
                         reads=[pgk, "bgu"], writes=[gsk])
                    S.op("act", lambda e, gs=gs, sg=sg: e.activation(out=sg[:], in_=gs[:], func=AF.Sigmoid, scale=1.702),
                         reads=[gsk], writes=[sgk])
                    S.op("dve", lambda e, us=us, pg=pg, j=j, ex=ex: e.tensor_scalar(out=us[:], in0=pg[:, 512:1024], scalar1=bgu[:, 8 + j, ex:ex + 1], scalar2=7.0, op0=ALU.add, op1=ALU.min),
                         reads=[pgk, "bgu"], writes=[usk])
                    S.op("dve", lambda e, us=us: e.tensor_scalar(out=us[:], in0=us[:], scalar1=-7.0, scalar2=1.0, op0=ALU.max, op1=ALU.add),
                         reads=[usk], writes=[usk])
                    S.op("pool", lambda e, gs=gs, sg=sg: e.tensor_tensor(out=gs[:], in0=gs[:], in1=sg[:], op=ALU.mult),
                         reads=[gsk, sgk], writes=[gsk])
                    S.op("pool", lambda e, gs=gs, us=us, j=j, tb=tb: e.tensor_tensor(out=hT[:, j, tb * 512:(tb + 1) * 512], in0=gs[:], in1=us[:], op=ALU.mult),
                         reads=[gsk, usk], writes=[("hT", j, tb)])
            for tl in range(NTB):
                for cg in range(2):
                    pd, pdk = pB.next()
                    for c in range(8):
                        S.op("pe", lambda e, pd=pd, tl=tl, c=c, cg=cg: e.matmul(pd[:], hT[:, c, tl * 128:(tl + 1) * 128], wdn[:, c, cg * 512:(cg + 1) * 512],
                                                                               start=(c == 0), stop=(c == 7)),
                             reads=[("hT", c, tl // 4), "wdn"], writes=[pdk])
                    S.op("dve", lambda e, pd=pd, tl=tl, cg=cg, ex=ex, t0=t0: e.scalar_tensor_tensor(
                        out=acc[:, tl, cg * 512:(cg + 1) * 512], in0=pd[:], scalar=gate[:, t0 + tl, ex:ex + 1],
                        in1=acc[:, tl, cg * 512:(cg + 1) * 512], op0=ALU.mult, op1=ALU.add),
                        reads=[pdk, ("gate", t0 + tl), ("acc", tl)], writes=[("acc", tl)])
        for tl in range(NTB):
            tt = t0 + tl
            ht, hk = hin.next()
            S.dma("sp", ht[:], hmid_d[tt * 128:(tt + 1) * 128, :], reads=[("hmid", tt)], writes=[hk])
            S.op("pool", lambda e, tl=tl: e.tensor_tensor(out=acc[:, tl, :], in0=acc[:, tl, :], in1=g5bc[:], op=ALU.mult),
                 reads=[("acc", tl), "g5bc"], writes=[("acc", tl)])
            S.op("dve", lambda e, ht=ht, tl=tl: e.tensor_tensor(out=ht[:], in0=ht[:], in1=acc[:, tl, :], op=ALU.add),
                 reads=[hk, ("acc", tl)], writes=[hk])
            if final:
                S.op("act", lambda e, ht=ht, tt=tt: e.activation(out=junk[:], in_=ht[:], func=AF.Square, accum_out=ssq[:, tt:tt + 1]),
                     reads=[hk], writes=["junk", ("ssq", tt)])
                S.op("act", lambda e, tt=tt: e.activation(out=rstd[:, tt:tt + 1], in_=ssq[:, tt:tt + 1], func=AF.Sqrt, scale=1.0 / D, bias=EPS),
                     reads=[("ssq", tt)], writes=[("rstd", tt)])
                S.op("dve", lambda e, tt=tt: e.reciprocal(out=rstd[:, tt:tt + 1], in_=rstd[:, tt:tt + 1]),
                     reads=[("rstd", tt)], writes=[("rstd", tt)])
                S.op("dve", lambda e, ht=ht, tt=tt: e.scalar_tensor_tensor(out=ht[:], in0=ht[:], scalar=rstd[:, tt:tt + 1], in1=fgbc[:], op0=ALU.mult, op1=ALU.mult),
                     reads=[hk, ("rstd", tt), "fgbc"], writes=[hk])
            S.dma("sp", out_d[tt * 128:(tt + 1) * 128, :], ht[:], reads=[hk], writes=[("out", tt)])
    S.finish([])
    es.close()
    return nc


def build_ada(nc, NL=2, W=768):
    es = ExitStack(); k = K(nc, es); S = k.S
    c_d = k.inp("c", [1, D]); cc_d = k.inp("c_ctx", [D]); aw_d = k.inp("ada_w", [NL, D, W]); ab_d = k.inp("ada_b", [NL, W])
    ident_d = k.inp("ident", [128, 128]); out_d = k.outp("modv", [NL, 2, W])
    ident = k.sb("ident", [128, 128]); S.dma("sp", ident[:], ident_d, writes=["ident"])
    sc = k.sb("sc", [16, 256])
    S.dma("sp", sc[0:8, 0:128], c_d.rearrange("o (k p) -> (o k) p", p=128), writes=["sc"])
    S.dma("sp", sc[8:16, 0:128], cc_d.rearrange("(k p) -> k p", p=128), writes=["sc"])
    S.op("act", lambda e: e.activation(out=sc[0:16, 128:256], in_=sc[0:16, 0:128], func=AF.Silu), reads=["sc"], writes=["sc"])
    pst = k.ps("pst", [128, 512])
    S.op("pe", lambda e: e.transpose(out=pst[:, 0:16], in_=sc[0:16, 128:256], identity=ident[0:16, 0:16]), reads=["sc", "ident"], writes=["pst"])
    cfm = k.sb("cfm", [128, 16])
    S.op("dve", lambda e: e.tensor_copy(out=cfm[:], in_=pst[:, 0:16]), reads=["pst"], writes=["cfm"])
    pm = Rot(k, "pm", [128, 512], n=2, psum=True)
    for l in range(NL):
        w = k.sb("w%d" % l, [128, 8, W]); S.dma("sp", w[:], aw_d[l].rearrange("(kc p) n -> p kc n", p=128), writes=["w%d" % l])
        bb = k.sb("bb%d" % l, [2, W]); S.dma("act", bb[:], ab_d[l].rearrange("(o n) -> o n", o=1).broadcast_to([2, W]), writes=["bb%d" % l])
        res = k.sb("res%d" % l, [2, W])
        for c0 in range(0, W, 512):
            cw = min(512, W - c0)
            p, pk = pm.next()
            for kc in range(8):
                S.op("pe", lambda e, p=p, w=w, kc=kc, c0=c0, cw=cw: e.matmul(p[0:2, 0:cw], cfm[:, kc:kc + 9:8], w[:, kc, c0:c0 + cw], start=(kc == 0), stop=(kc == 7)),
                     reads=["cfm", "w%d" % l], writes=[pk])
            S.op("dve", lambda e, p=p, res=res, bb=bb, c0=c0, cw=cw: e.tensor_tensor(out=res[:, c0:c0 + cw], in0=p[0:2, 0:cw], in1=bb[:, c0:c0 + cw], op=ALU.add),
                 reads=[pk, "bb%d" % l], writes=["res%d" % l])
        S.dma("sp", out_d[l], res[:], reads=["res%d" % l], writes=[("o", l)])
    S.finish([]); es.close()
    return nc


def norm_mod_T(k, x_src, ntiles, A, B, aT_fn, pA, ident, tag="n"):
    S = k.S
    xin = Rot(k, tag + "xin", [128, D], n=2)
    junk = k.sb(tag + "junk", [128, D], BF16)
    ssq = k.sb(tag + "ssq", [128, ntiles]); rstd = k.sb(tag + "rstd", [128, ntiles])
    for i in range(ntiles):
        xt, xk = xin.next()
        S.dma("sp" if i % 2 == 0 else "act", xt[:], x_src(i), writes=[xk])
        S.op("act", lambda e, xt=xt, i=i: e.activation(out=junk[:], in_=xt[:], func=AF.Square, accum_out=ssq[:, i:i + 1]),
             reads=[xk], writes=[tag + "junk", (tag + "ssq", i)])
        S.op("act", lambda e, i=i: e.activation(out=rstd[:, i:i + 1], in_=ssq[:, i:i + 1], func=AF.Sqrt, scale=1.0 / D, bias=EPS),
             reads=[(tag + "ssq", i)], writes=[(tag + "rstd", i)])
        S.op("dve", lambda e, i=i: e.reciprocal(out=rstd[:, i:i + 1], in_=rstd[:, i:i + 1]), reads=[(tag + "rstd", i)], writes=[(tag + "rstd", i)])
        S.op("dve", lambda e, xt=xt, i=i: e.tensor_scalar(out=xt[:], in0=xt[:], scalar1=rstd[:, i:i + 1], scalar2=None, op0=ALU.mult),
             reads=[xk, (tag + "rstd", i)], writes=[xk])
        pt, ptk = pA.next()
        for c in range(8):
            S.op("pe", lambda e, pt=pt, xt=xt, c=c: e.transpose(out=pt[:, c * 128:(c + 1) * 128], in_=xt[:, c * 128:(c + 1) * 128], identity=ident[:]),
                 reads=[xk, "ident"], writes=[ptk])
        dst, dk = aT_fn(i)
        sA, sB, skeys = A(i), B(i), ["A1", "B1"]
        for c in range(8):
            S.op("act", lambda e, pt=pt, dst=dst, c=c, sA=sA, sB=sB: e.activation(out=dst[:, c, :], in_=pt[:, c * 128:(c + 1) * 128], func=AF.Identity,
                                                                               scale=sA[:, c:c + 1], bias=sB[:, c:c + 1]),
                 reads=[ptk] + skeys, writes=[dk])


def load_AB(k, g_d, shift_rows, scale_rows, ident, pC, nvar):
    S = k.S
    sc = k.sb("ABsc", [128, 128])
    fm = k.sb("ABfm", [128, 1 + 2 * nvar, 8])
    srcs = [g_d] + list(shift_rows) + list(scale_rows)
    for i, src in enumerate(srcs):
        pt, pk = pC.next()
        load_fm(k, fm[:, i, :], "B1", src.rearrange("(j p) -> j p", p=128), 8, ident, sc, "ABsc", pt, pk)
    A = k.sb("ABA", [128, nvar, 8])
    for v in range(nvar):
        S.op("dve", lambda e, v=v: e.scalar_tensor_tensor(out=A[:, v, :], in0=fm[:, 1 + nvar + v, :], scalar=1.0, in1=fm[:, 0, :], op0=ALU.add, op1=ALU.mult),
             reads=["B1"], writes=["A1"])
    return A, fm


def bcast_ap(ap, n):
    a = [list(x) for x in ap.ap]
    return bass.AP(ap.tensor, ap.offset, [a[0], [0, n]] + a[1:])


def build_proj0(nc, TL=2048, TC=256):
    NTL, NTC = TL // 128, TC // 128
    NT = NTL + NTC
    E_IN = 2576
    es = ExitStack(); k = K(nc, es); S = k.S
    x_d = k.inp("x", [TL, D]); ctx_d = k.inp("ctx", [TC, D]); modv_d = k.inp("modv", [2, 6, D]); n1g_d = k.inp("norm1_g", [D])
    win_d = k.inp("w_in", [D, E_IN]); bin_d = k.inp("b_in", [E_IN]); qg_d = k.inp("q_gain", [128]); kg_d = k.inp("k_gain", [128])
    cos_d = k.inp("cos", [TL, 64]); sin_d = k.inp("sin", [TL, 64]); ident_d = k.inp("ident", [128, 128])
    QT_d = k.outp("QT", [128, 4, TL], BF16); KT_d = k.outp("KT", [128, 2, TL + TC], BF16)
    V_d = k.outp("V", [TL + TC, 256], BF16); PM_d = k.outp("PM", [TL + TC, 1552])
    ident = k.sb("ident", [128, 128]); S.dma("sp", ident[:], ident_d, writes=["ident"])
    pA = Rot(k, "pA", [128, 1024], n=2, psum=True); pB = Rot(k, "pB", [128, 512], n=2, psum=True); pC = Rot(k, "pC", [128, 512], n=2, psum=True)
    A, fm = load_AB(k, n1g_d, [modv_d[0, 0], modv_d[1, 0]], [modv_d[0, 1], modv_d[1, 1]], ident, pC, 2)
    win = k.sb("win", [128, 8, E_IN], BF16)
    S.dma("pool", win[:], win_d.rearrange("(kc p) n -> p kc n", p=128), writes=["win"])
    binbc = k.sb("binbc", [128, E_IN]); bc_load(k, binbc[:], "binbc", bin_d, E_IN)
    gbc = k.sb("gbc", [128, 2, 128]); bc_load(k, gbc[:, 0, :], "gbc", qg_d, 128); bc_load(k, gbc[:, 1, :], "gbc", kg_d, 128)
    aTr = Rot(k, "aT", [128, 8, 128], BF16, n=2)
    aT_cur = {}

    def aT_fn(i):
        t, kk = aTr.next(); aT_cur[i] = (t, kk); return t, kk
    x_src = lambda i: (x_d[i * 128:(i + 1) * 128, :] if i < NTL else ctx_d[(i - NTL) * 128:(i - NTL + 1) * 128, :])
    var = lambda i: 0 if i < NTL else 1
    pr = Rot(k, "p", [128, E_IN], n=2)
    sqr = k.sb("sq", [128, 768]); hs = Rot(k, "hs", [128, 8], n=2)
    cs = Rot(k, "cs", [128, 2, 64], n=2)
    rt = Rot(k, "rt", [128, 4, 6, 64], n=1)
    qkT = Rot(k, "qkT", [128, 6, 128], BF16, n=2); vb = Rot(k, "vb", [128, 256], BF16, n=2)
    groups = [(0, 512), (512, 512), (1024, 512), (1536, 512), (2048, 512), (2560, 16)]
    xin = Rot(k, "xin", [128, D], n=2); junk = k.sb("junk", [128, D], BF16)
    ssq = k.sb("ssq", [128, NT]); rstd = k.sb("rstd", [128, NT])
    for i in range(NT):
        lat = i < NTL
        tok0 = i * 128 if lat else TL + (i - NTL) * 128
        xt, xk = xin.next()
        S.dma("sp", xt[:], x_src(i), writes=[xk])
        S.op("act", lambda e, xt=xt, i=i: e.activation(out=junk[:], in_=xt[:], func=AF.Square, accum_out=ssq[:, i:i + 1]), reads=[xk], writes=["junk", ("ssq", i)])
        S.op("act", lambda e, i=i: e.activation(out=rstd[:, i:i + 1], in_=ssq[:, i:i + 1], func=AF.Sqrt, scale=1.0 / D, bias=EPS), reads=[("ssq", i)], writes=[("rstd", i)])
        S.op("dve", lambda e, i=i: e.reciprocal(out=rstd[:, i:i + 1], in_=rstd[:, i:i + 1]), reads=[("rstd", i)], writes=[("rstd", i)])
        S.op("dve", lambda e, xt=xt, i=i: e.tensor_scalar(out=xt[:], in0=xt[:], scalar1=rstd[:, i:i + 1], scalar2=None, op0=ALU.mult), reads=[xk, ("rstd", i)], writes=[xk])
        pt, ptk = pA.next()
        for c in range(8):
            S.op("pe", lambda e, pt=pt, xt=xt, c=c: e.transpose(out=pt[:, c * 128:(c + 1) * 128], in_=xt[:, c * 128:(c + 1) * 128], identity=ident[:]), reads=[xk, "ident"], writes=[ptk])
        at, ak = aTr.next()
        v = var(i)
        for c in range(8):
            S.op("act", lambda e, pt=pt, at=at, c=c, v=v: e.activation(out=at[:, c, :], in_=pt[:, c * 128:(c + 1) * 128], func=AF.Identity,
                                                                    scale=A[:, v, c:c + 1], bias=fm[:, 1 + v, c:c + 1]), reads=[ptk, "A1", "B1"], writes=[ak])
        p, pk = pr.next()
        for (c0, cw) in groups:
            pg, pgk = pB.next()
            for kc in range(8):
                S.op("pe", lambda e, pg=pg, at=at, kc=kc, c0=c0, cw=cw: e.matmul(pg[:, 0:cw], at[:, kc, :], win[:, kc, c0:c0 + cw], start=(kc == 0), stop=(kc == 7)),
                     reads=[ak, "win"], writes=[pgk])
            S.op("dve", lambda e, p=p, pg=pg, c0=c0, cw=cw: e.tensor_tensor(out=p[:, c0:c0 + cw], in0=pg[:, 0:cw], in1=binbc[:, c0:c0 + cw], op=ALU.add),
                 reads=[pgk, "binbc"], writes=[pk])
        S.op("pool", lambda e, p=p: e.tensor_tensor(out=sqr[:], in0=p[:, 0:768], in1=p[:, 0:768], op=ALU.mult), reads=[pk], writes=["sq"])
        h8, hk = hs.next()
        S.op("dve", lambda e, h8=h8: e.tensor_reduce(out=h8[:, 0:6], in_=sqr[:].rearrange("p (h d) -> p h d", h=6), axis=AX.X, op=ALU.add), reads=["sq"], writes=[hk])
        S.op("act", lambda e, h8=h8: e.activation(out=h8[:, 0:6], in_=h8[:, 0:6], func=AF.Sqrt, scale=1.0 / 128, bias=EPS), reads=[hk], writes=[hk])
        S.op("dve", lambda e, h8=h8: e.reciprocal(out=h8[:, 0:6], in_=h8[:, 0:6]), reads=[hk], writes=[hk])
        for h in range(6):
            S.op("dve", lambda e, p=p, h=h, h8=h8: e.scalar_tensor_tensor(out=p[:, h * 128:(h + 1) * 128], in0=p[:, h * 128:(h + 1) * 128], scalar=h8[:, h:h + 1],
                                                                          in1=gbc[:, 0 if h < 4 else 1, :], op0=ALU.mult, op1=ALU.mult), reads=[pk, hk, "gbc"], writes=[pk])
        if lat:
            ct, ck = cs.next()
            S.dma("act", ct[:, 0, :], cos_d[tok0:tok0 + 128, :], writes=[ck]); S.dma("act", ct[:, 1, :], sin_d[tok0:tok0 + 128, :], writes=[ck])
            r, rk = rt.next()
            x5 = p[:, 0:768].rearrange("p (h a t d) -> p h a t d", h=6, a=2, t=2)
            x1, x2 = x5[:, :, :, 0, :], x5[:, :, :, 1, :]
            cosb = bcast_ap(ct[:, 0, :].rearrange("p (a d) -> p a d", a=2), 6); sinb = bcast_ap(ct[:, 1, :].rearrange("p (a d) -> p a d", a=2), 6)
            r4 = [r[:, q, :, :].rearrange("p h (a d) -> p h a d", a=2) for q in range(4)]
            S.op("dve", lambda e, r4=r4, x1=x1, cosb=cosb: e.tensor_tensor(out=r4[0], in0=x1, in1=cosb, op=ALU.mult), reads=[pk, ck], writes=[rk])
            S.op("pool", lambda e, r4=r4, x2=x2, sinb=sinb: e.tensor_tensor(out=r4[1], in0=x2, in1=sinb, op=ALU.mult), reads=[pk, ck], writes=[rk])
            S.op("dve", lambda e, r4=r4, x2=x2, cosb=cosb: e.tensor_tensor(out=r4[2], in0=x2, in1=cosb, op=ALU.mult), reads=[pk, ck], writes=[rk])
            S.op("pool", lambda e, r4=r4, x1=x1, sinb=sinb: e.tensor_tensor(out=r4[3], in0=x1, in1=sinb, op=ALU.mult), reads=[pk, ck], writes=[rk])
            S.op("dve", lambda e, r4=r4, x1=x1: e.tensor_tensor(out=x1, in0=r4[0], in1=r4[1], op=ALU.subtract), reads=[rk], writes=[pk])
            S.op("pool", lambda e, r4=r4, x2=x2: e.tensor_tensor(out=x2, in0=r4[2], in1=r4[3], op=ALU.add), reads=[rk], writes=[pk])
        pq, pqk = pA.next()
        for h in range(6):
            S.op("pe", lambda e, pq=pq, p=p, h=h: e.transpose(out=pq[:, h * 128:(h + 1) * 128], in_=p[:, h * 128:(h + 1) * 128], identity=ident[:]), reads=[pk, "ident"], writes=[pqk])
        qt, qk_ = qkT.next()
        S.op("act", lambda e, qt=qt, pq=pq: e.copy(out=qt[:].rearrange("p h d -> p (h d)"), in_=pq[:, 0:768]), reads=[pqk], writes=[qk_])
        if lat:
            S.dma("sp", QT_d[:, :, tok0:tok0 + 128], qt[:, 0:4, :], reads=[qk_], writes=[("QT", i)])
        S.dma("sp", KT_d[:, :, tok0:tok0 + 128], qt[:, 4:6, :], reads=[qk_], writes=[("KT", i)])
        vt, vk = vb.next()
        S.op("act", lambda e, vt=vt, p=p: e.copy(out=vt[:], in_=p[:, 768:1024]), reads=[pk], writes=[vk])
        S.dma("sp", V_d[tok0:tok0 + 128, :], vt[:], reads=[vk], writes=[("V", i)])
        S.dma("sp", PM_d[tok0:tok0 + 128, :], p[:, 1024:E_IN], reads=[pk], writes=[("PM", i)])
    S.finish([]); es.close()
    return nc


def build_attn(nc, TQ=2048, NK=16640):
    NKT = NK // 128
    es = ExitStack(); k = K(nc, es); S = k.S
    QT_d = k.inp("QT", [128, 4, TQ], BF16); KT_d = k.inp("KT", [128, 2, NK], BF16); V_d = k.inp("V", [NK, 256], BF16)
    hf_d = k.inp("hf", [TQ, 512]); hb_d = k.inp("hb", [TQ, 512]); og_d = k.inp("og", [TQ, 512]); hg_d = k.inp("h_gain", [512])
    ident_d = k.inp("ident", [128, 128])
    mix_d = k.outp("mixT", [D, TQ], BF16)
    ident = k.sb("ident", [128, 128]); S.dma("sp", ident[:], ident_d, writes=["ident"])
    KT = k.sb("KT", [128, 2, NK], BF16); V = k.sb("V", [128, NKT, 256], BF16); QT = k.sb("QT", [128, 4, TQ], BF16)
    S.dma("sp", QT[:], QT_d, writes=["QT"])
    NCH = 13 if NKT % 13 == 0 else 1
    step = NKT // NCH
    Vv = V_d.rearrange("(t p) c -> p t c", p=128)
    for ci in range(NCH):
        S.dma("sp", KT[:, :, ci * step * 128:(ci + 1) * step * 128], KT_d[:, :, ci * step * 128:(ci + 1) * step * 128], writes=[("KT", ci)])
        S.dma("act", V[:, ci * step:(ci + 1) * step, :], Vv[:, ci * step:(ci + 1) * step, :], writes=[("V", ci)])
    ones = k.sb("ones", [128, 128], BF16); S.op("dve", lambda e: e.memset(ones[:], 1.0), writes=["ones"])
    pST = Rot(k, "pST", [128, 512], n=2, psum=True); pOT = Rot(k, "pOT", [128, 512], n=2, psum=True)
    pDN = Rot(k, "pDN", [128, 512], n=2, psum=True); pM = Rot(k, "pM", [128, 512], n=2, psum=True)
    PT = Rot(k, "PT", [128, 512], BF16, n=3)
    rd = Rot(k, "rd", [128, 512], n=2); ob = Rot(k, "ob", [128, 512], BF16, n=2)
    sc = 128.0 ** -0.5
    for h in range(4):
        g = h // 2
        for qb in range(TQ // 512):
            ot, otk = pOT.next(); dn, dnk = pDN.next()
            for kt in range(NKT):
                st, stk = pST.next(); pt, ptk = PT.next()
                S.op("pe", lambda e, st=st, g=g, kt=kt, h=h, qb=qb: e.matmul(st[:], KT[:, g, kt * 128:(kt + 1) * 128], QT[:, h, qb * 512:(qb + 1) * 512], start=True, stop=True),
                     reads=[("KT", kt // step), "QT"], writes=[stk])
                S.op("act", lambda e, st=st, pt=pt: e.activation(out=pt[:], in_=st[:], func=AF.Exp, scale=sc), reads=[stk], writes=[ptk])
                S.op("pe", lambda e, ot=ot, pt=pt, g=g, kt=kt: e.matmul(ot[:], V[:, kt, g * 128:(g + 1) * 128], pt[:], start=(kt == 0), stop=(kt == NKT - 1)),
                     reads=[("V", kt // step), ptk], writes=[otk])
                S.op("pe", lambda e, dn=dn, pt=pt, kt=kt: e.matmul(dn[:], ones[:], pt[:], start=(kt == 0), stop=(kt == NKT - 1)),
                     reads=["ones", ptk], writes=[dnk])
            r, rk = rd.next(); o, ok = ob.next()
            S.op("dve", lambda e, r=r, dn=dn: e.reciprocal(out=r[:], in_=dn[:]), reads=[dnk], writes=[rk])
            S.op("dve", lambda e, o=o, ot=ot, r=r: e.tensor_tensor(out=o[:], in0=ot[:], in1=r[:], op=ALU.mult), reads=[otk, rk], writes=[ok])
            S.dma("sp", mix_d[h * 128:(h + 1) * 128, qb * 512:(qb + 1) * 512], o[:], reads=[ok], writes=[("mix", h, qb)])
    hgbc = k.sb("hgbc", [128, 512]); bc_load(k, hgbc[:], "hgbc", hg_d, 512)
    hfr = Rot(k, "hfr", [128, 512], n=2); hbr = Rot(k, "hbr", [128, 512], n=2); ogr = Rot(k, "ogr", [128, 512], n=2)
    sq = k.sb("sq", [128, 512]); h4 = Rot(k, "h4", [128, 4], n=2); mt = Rot(k, "mt", [128, 4, 128], BF16, n=2)
    for i in range(TQ // 128):
        a, ak = hfr.next(); b, bk = hbr.next(); o, ok = ogr.next()
        S.dma("sp", a[:], hf_d[i * 128:(i + 1) * 128, :], writes=[ak]); S.dma("act", b[:], hb_d[i * 128:(i + 1) * 128, :], writes=[bk])
        S.dma("sp", o[:], og_d[i * 128:(i + 1) * 128, :], writes=[ok])
        S.op("dve", lambda e, a=a, b=b: e.tensor_tensor(out=a[:], in0=a[:], in1=b[:], op=ALU.add), reads=[ak, bk], writes=[ak])
        S.op("pool", lambda e, a=a: e.tensor_tensor(out=sq[:], in0=a[:], in1=a[:], op=ALU.mult), reads=[ak], writes=["sq"])
        hh, hk = h4.next()
        S.op("dve", lambda e, hh=hh: e.tensor_reduce(out=hh[:], in_=sq[:].rearrange("p (h d) -> p h d", h=4), axis=AX.X, op=ALU.add), reads=["sq"], writes=[hk])
        S.op("act", lambda e, hh=hh: e.activation(out=hh[:], in_=hh[:], func=AF.Sqrt, scale=1.0 / 128, bias=EPS), reads=[hk], writes=[hk])
        S.op("dve", lambda e, hh=hh: e.reciprocal(out=hh[:], in_=hh[:]), reads=[hk], writes=[hk])
        S.op("act", lambda e, o=o: e.activation(out=o[:], in_=o[:], func=AF.Sigmoid), reads=[ok], writes=[ok])
        for h in range(4):
            S.op("dve", lambda e, a=a, hh=hh, h=h: e.scalar_tensor_tensor(out=a[:, h * 128:(h + 1) * 128], in0=a[:, h * 128:(h + 1) * 128], scalar=hh[:, h:h + 1],
                                                                          in1=hgbc[:, h * 128:(h + 1) * 128], op0=ALU.mult, op1=ALU.mult), reads=[ak, hk, "hgbc"], writes=[ak])
        S.op("pool", lambda e, a=a, o=o: e.tensor_tensor(out=a[:], in0=a[:], in1=o[:], op=ALU.mult), reads=[ak, ok], writes=[ak])
        pm, pmk = pM.next()
        for h in range(4):
            S.op("pe", lambda e, pm=pm, a=a, h=h: e.transpose(out=pm[:, h * 128:(h + 1) * 128], in_=a[:, h * 128:(h + 1) * 128], identity=ident[:]), reads=[ak, "ident"], writes=[pmk])
        m, mk_ = mt.next()
        S.op("act", lambda e, m=m, pm=pm: e.copy(out=m[:].rearrange("p h d -> p (h d)"), in_=pm[:]), reads=[pmk], writes=[mk_])
        S.dma("sp", mix_d[512:1024, i * 128:(i + 1) * 128].rearrange("(h p) t -> p h t", p=128), m[:], reads=[mk_], writes=[("mixm", i)])
    S.finish([]); es.close()
    return nc


def build_mlstm(nc, NS=16640, NCTX=2):
    NC = NS // 128
    es = ExitStack(); k = K(nc, es); S = k.S
    qT_d = k.inp("qT", [64, NS]); kT_d = k.inp("kT", [64, NS]); k_d = k.inp("k", [NS, 64]); v_d = k.inp("v", [NS, 128])
    ig_d = k.inp("ig", [128, NC]); fg_d = k.inp("fg", [128, NC]); fb_d = k.inp("fb", [128, 1]); tri_d = k.inp("tri", [128, 128]); ident_d = k.inp("ident", [128, 128])
    h_d = k.outp("h", [NS - NCTX * 128, 128])
    ident = k.sb("ident", [128, 128]); S.dma("sp", ident[:], ident_d, writes=["ident"])
    tri = k.sb("tri", [128, 128]); S.dma("sp", tri[:], tri_d, writes=["tri"])
    onesf = k.sb("onesf", [128, 128]); S.op("dve", lambda e: e.memset(onesf[:], 1.0), writes=["onesf"])
    qT = k.sb("qT", [64, NS], BF16); kT = k.sb("kT", [64, NS], BF16)
    S.dma("pool", qT[:], qT_d, writes=["qT"]); S.dma("pool", kT[:], kT_d, writes=["kT"])
    kk = k.sb("k", [128, NC, 64])
    for c0 in range(0, NC, 64):
        c1 = min(NC, c0 + 64)
        S.dma("sp", kk[:, c0:c1, :], k_d[c0 * 128:c1 * 128, :].rearrange("(c p) d -> p c d", p=128), writes=["k"])
    v1 = k.sb("v1", [128, NC, 129], BF16)
    S.op("dve", lambda e: e.memset(v1[:, :, 128:129], 1.0), writes=["v1"])
    for c0 in range(0, NC, 64):
        c1 = min(NC, c0 + 64)
        S.dma("pool", v1[:, c0:c1, 0:128], v_d[c0 * 128:c1 * 128, :].rearrange("(c p) d -> p c d", p=128), writes=["v1"])
    names = ["ig", "fg", "logf", "b", "blast", "LW", "U", "abc", "mnext", "mprev", "cm", "mrow", "w", "decay", "rowf", "colf8", "winter8", "emr", "tmp"]
    A = {n: k.sb("a_" + n, [128, NC]) for n in names}
    fb = k.sb("fb", [128, 2]); S.dma("sp", fb[:, 0:1], fb_d, writes=["fb"])
    S.op("dve", lambda e: e.tensor_scalar(out=fb[:, 1:2], in0=fb[:, 0:1], scalar1=-1.0, scalar2=None, op0=ALU.mult), reads=["fb"], writes=["fb"])
    S.dma("sp", A["ig"][:], ig_d, writes=["ig"]); S.dma("sp", A["fg"][:], fg_d, writes=["fg"])
    pP = Rot(k, "pP", [128, 512], n=8, psum=True)

    def ew(eng, fn, reads, writes):
        S.op(eng, fn, reads=reads, writes=writes)
    ew("act", lambda e: e.activation(out=A["tmp"][:], in_=A["fg"][:], func=AF.Exp, scale=-1.0, bias=fb[:, 1:2]), ["fg", "fb"], ["tmp"])
    ew("act", lambda e: e.activation(out=A["tmp"][:], in_=A["tmp"][:], func=AF.Ln, bias=1.0), ["tmp"], ["tmp"])
    ew("dve", lambda e: e.tensor_scalar(out=A["logf"][:], in0=A["tmp"][:], scalar1=-1.0, scalar2=None, op0=ALU.mult), ["tmp"], ["logf"])
    p1, p1k = pP.next(); p2, p2k = pP.next()
    ew("pe", lambda e: e.matmul(p1[:, 0:NC], tri[:], A["logf"][:], start=True, stop=True), ["tri", "logf"], [p1k])
    ew("pe", lambda e: e.matmul(p2[:, 0:NC], onesf[:], A["logf"][:], start=True, stop=True), ["onesf", "logf"], [p2k])
    ew("dve", lambda e: e.tensor_copy(out=A["b"][:], in_=p1[:, 0:NC]), [p1k], ["b"])
    ew("dve", lambda e: e.tensor_copy(out=A["blast"][:], in_=p2[:, 0:NC]), [p2k], ["blast"])
    ew("dve", lambda e: e.tensor_tensor(out=A["U"][:], in0=A["ig"][:], in1=A["b"][:], op=ALU.subtract), ["ig", "b"], ["U"])
    ew("dve", lambda e: e.tensor_tensor(out=A["LW"][:], in0=A["U"][:], in1=A["blast"][:], op=ALU.add), ["U", "blast"], ["LW"])
    zer = k.sb("zer", [128, 128]); S.op("dve", lambda e: e.memset(zer[:], 0.0), writes=["zer"])
    am = k.sb("am", [128, 1]); lh = k.sb("lh", [128, 128]); ut = k.sb("ut", [128, 128]); cmt = k.sb("cmt", [128, 128])
    for c0 in range(0, NC, 128):
        cw = min(128, NC - c0)
        pt, ptk = pP.next()
        ew("pe", lambda e, pt=pt, c0=c0, cw=cw: e.transpose(out=pt[0:cw, 0:128], in_=A["LW"][:, c0:c0 + cw], identity=ident[:]), ["LW", "ident"], [ptk])
        ew("dve", lambda e, pt=pt, cw=cw: e.tensor_reduce(out=am[0:cw, :], in_=pt[0:cw, 0:128], axis=AX.X, op=ALU.max), [ptk], ["am"])
        ew("dve", lambda e, cw=cw: e.tensor_scalar(out=lh[0:cw, :], in0=onesf[0:cw, :], scalar1=am[0:cw, 0:1], scalar2=None, op0=ALU.mult), ["am", "onesf"], ["lh"])
        pa, pak = pP.next()
        ew("pe", lambda e, pa=pa, cw=cw: e.matmul(pa[:, 0:cw], lh[0:cw, :], ident[0:cw, 0:cw], start=True, stop=True), ["lh", "ident"], [pak])
        ew("dve", lambda e, pa=pa, c0=c0, cw=cw: e.tensor_copy(out=A["abc"][:, c0:c0 + cw], in_=pa[:, 0:cw]), [pak], ["abc"])
        pu, puk = pP.next()
        ew("pe", lambda e, pu=pu, c0=c0, cw=cw: e.transpose(out=pu[0:cw, 0:128], in_=A["U"][:, c0:c0 + cw], identity=ident[:]), ["U", "ident"], [puk])
        ew("dve", lambda e, pu=pu, cw=cw: e.tensor_copy(out=ut[0:cw, :], in_=pu[0:cw, 0:128]), [puk], ["ut"])
        ew("dve", lambda e, cw=cw: e.tensor_tensor_scan(out=cmt[0:cw, :], data0=zer[0:cw, :], data1=ut[0:cw, :], initial=-1e30, op0=ALU.add, op1=ALU.max), ["ut", "zer"], ["cmt"])
        pc, pck = pP.next()
        ew("pe", lambda e, pc=pc, cw=cw: e.transpose(out=pc[:, 0:cw], in_=cmt[0:cw, :], identity=ident[0:cw, 0:cw]), ["cmt", "ident"], [pck])
        ew("dve", lambda e, pc=pc, c0=c0, cw=cw: e.tensor_copy(out=A["cm"][:, c0:c0 + cw], in_=pc[:, 0:cw]), [pck], ["cm"])
    ew("dve", lambda e: e.tensor_tensor_scan(out=A["mnext"][:], data0=A["blast"][:], data1=A["abc"][:], initial=0.0, op0=ALU.add, op1=ALU.max), ["blast", "abc"], ["mnext"])
    ew("dve", lambda e: e.memset(A["mprev"][:, 0:1], 0.0), [], ["mprev"])
    ew("dve", lambda e: e.tensor_copy(out=A["mprev"][:, 1:NC], in_=A["mnext"][:, 0:NC - 1]), ["mnext"], ["mprev"])
    ew("dve", lambda e: e.tensor_tensor(out=A["mrow"][:], in0=A["mprev"][:], in1=A["cm"][:], op=ALU.max), ["mprev", "cm"], ["mrow"])
    ew("dve", lambda e: e.tensor_tensor(out=A["mrow"][:], in0=A["mrow"][:], in1=A["b"][:], op=ALU.add), ["mrow", "b"], ["mrow"])
    ew("dve", lambda e: e.tensor_tensor(out=A["tmp"][:], in0=A["LW"][:], in1=A["mnext"][:], op=ALU.subtract), ["LW", "mnext"], ["tmp"])
    ew("act", lambda e: e.activation(out=A["w"][:], in_=A["tmp"][:], func=AF.Exp), ["tmp"], ["w"])
    ew("dve", lambda e: e.tensor_tensor(out=A["tmp"][:], in0=A["blast"][:], in1=A["mprev"][:], op=ALU.add), ["blast", "mprev", "w"], ["tmp"])
    ew("dve", lambda e: e.tensor_tensor(out=A["tmp"][:], in0=A["tmp"][:], in1=A["mnext"][:], op=ALU.subtract), ["tmp", "mnext"], ["tmp"])
    ew("act", lambda e: e.activation(out=A["decay"][:], in_=A["tmp"][:], func=AF.Exp), ["tmp"], ["decay"])
    ew("dve", lambda e: e.tensor_tensor(out=A["tmp"][:], in0=A["b"][:], in1=A["mrow"][:], op=ALU.subtract), ["b", "mrow", "decay"], ["tmp"])
    ew("act", lambda e: e.activation(out=A["rowf"][:], in_=A["tmp"][:], func=AF.Exp), ["tmp"], ["rowf"])
    ew("dve", lambda e: e.tensor_tensor(out=A["tmp"][:], in0=A["tmp"][:], in1=A["mprev"][:], op=ALU.add), ["tmp", "mprev", "rowf"], ["tmp"])
    ew("dve", lambda e: e.tensor_scalar(out=A["tmp"][:], in0=A["tmp"][:], scalar1=-float(np.log(8.0)), scalar2=None, op0=ALU.add), ["tmp"], ["tmp"])
    ew("act", lambda e: e.activation(out=A["winter8"][:], in_=A["tmp"][:], func=AF.Exp), ["tmp"], ["winter8"])
    ew("dve", lambda e: e.tensor_scalar(out=A["tmp"][:], in0=A["U"][:], scalar1=-float(np.log(8.0)), scalar2=None, op0=ALU.add), ["U", "winter8"], ["tmp"])
    ew("act", lambda e: e.activation(out=A["colf8"][:], in_=A["tmp"][:], func=AF.Exp), ["tmp"], ["colf8"])
    ew("act", lambda e: e.activation(out=A["emr"][:], in_=A["mrow"][:], func=AF.Exp, scale=-1.0), ["mrow"], ["emr"])
    pre = ["w", "decay", "rowf", "colf8", "winter8", "emr"]
    Cx = k.sb("Cx", [64, 129]); S.op("dve", lambda e: e.memset(Cx[:], 0.0), writes=["Cx"])
    Cb = Rot(k, "Cb", [64, 129], BF16, n=2); stm = Rot(k, "stm", [128, 128], BF16, n=2); kw = Rot(k, "kw", [128, 64], BF16, n=2)
    t1 = Rot(k, "t1", [128, 129], n=2); dd = Rot(k, "dd", [128, 2], n=2); ho = Rot(k, "ho", [128, 128], n=2)
    for c in range(NC):
        cs = slice(c * 128, (c + 1) * 128)
        if c >= NCTX:
            ps, psk = pP.next()
            ew("pe", lambda e, ps=ps, cs=cs: e.matmul(ps[:, 0:128], kT[:, cs], qT[:, cs], start=True, stop=True), ["kT", "qT"], [psk])
            sm, smk = stm.next()
            ew("dve", lambda e, sm=sm, ps=ps, c=c: e.scalar_tensor_tensor(out=sm[:], in0=ps[:, 0:128], scalar=A["colf8"][:, c:c + 1], in1=tri[:], op0=ALU.mult, op1=ALU.mult),
               [psk, "colf8", "tri"], [smk])
            pa, pak = pP.next()
            ew("pe", lambda e, pa=pa, sm=sm, c=c: e.matmul(pa[:, 0:129], sm[:], v1[:, c, :], start=True, stop=True), [smk, "v1"], [pak])
            cb, cbk = Cb.next()
            ew("act", lambda e, cb=cb: e.copy(out=cb[:], in_=Cx[:]), ["Cx"], [cbk])
            pb, pbk = pP.next()
            ew("pe", lambda e, pb=pb, cb=cb, cs=cs: e.matmul(pb[:, 0:129], qT[:, cs], cb[:], start=True, stop=True), ["qT", cbk], [pbk])
            tt, ttk = t1.next()
            ew("act", lambda e, tt=tt, pa=pa, c=c: e.activation(out=tt[:], in_=pa[:, 0:129], func=AF.Identity, scale=A["rowf"][:, c:c + 1]), [pak, "rowf"], [ttk])
            ew("dve", lambda e, tt=tt, pb=pb, c=c: e.scalar_tensor_tensor(out=tt[:], in0=pb[:, 0:129], scalar=A["winter8"][:, c:c + 1], in1=tt[:], op0=ALU.mult, op1=ALU.add),
               [pbk, "winter8", ttk], [ttk])
            d2, d2k = dd.next()
            ew("dve", lambda e, d2=d2, tt=tt: e.tensor_scalar(out=d2[:, 1:2], in0=tt[:, 128:129], scalar1=-1.0, scalar2=None, op0=ALU.mult), [ttk], [d2k])
            ew("dve", lambda e, d2=d2, tt=tt: e.tensor_tensor(out=d2[:, 0:1], in0=tt[:, 128:129], in1=d2[:, 1:2], op=ALU.max), [ttk, d2k], [d2k])
            ew("dve", lambda e, d2=d2, c=c: e.tensor_tensor(out=d2[:, 0:1], in0=d2[:, 0:1], in1=A["emr"][:, c:c + 1], op=ALU.max), [d2k, "emr"], [d2k])
            ew("dve", lambda e, d2=d2: e.reciprocal(out=d2[:, 1:2], in_=d2[:, 0:1]), [d2k], [d2k])
            hh, hhk = ho.next()
            ew("dve", lambda e, hh=hh, tt=tt, d2=d2: e.tensor_scalar(out=hh[:], in0=tt[:, 0:128], scalar1=d2[:, 1:2], scalar2=None, op0=ALU.mult), [ttk, d2k], [hhk])
            S.dma("sp", h_d[(c - NCTX) * 128:(c - NCTX + 1) * 128, :], hh[:], reads=[hhk], writes=[("h", c)])
        if c < NC - 1:
            kq, kqk = kw.next()
            ew("pool", lambda e, kq=kq, c=c: e.tensor_scalar(out=kq[:], in0=kk[:, c, :], scalar1=A["w"][:, c:c + 1], scalar2=None, op0=ALU.mult), ["k", "w"], [kqk])
            pu, puk = pP.next()
            ew("pe", lambda e, pu=pu, kq=kq, c=c: e.matmul(pu[0:64, 0:129], kq[:], v1[:, c, :], start=True, stop=True), [kqk, "v1"], [puk])
            ew("dve", lambda e, pu=pu, c=c: e.scalar_tensor_tensor(out=Cx[:], in0=Cx[:], scalar=A["decay"][0:64, c:c + 1], in1=pu[0:64, 0:129], op0=ALU.mult, op1=ALU.add),
               ["Cx", "decay", puk], ["Cx"])
    S.finish([]); es.close()
    return nc


def build_hyproj(nc, T=2048):
    NT = T // 128
    es = ExitStack(); k = K(nc, es); S = k.S
    h_d = k.inp("h", [T, D]); modv_d = k.inp("modv", [6, D]); n1g_d = k.inp("norm1_g", [D])
    win_d = k.inp("w_in", [D, 3 * D]); binfm_d = k.inp("b_in_fm", [128, 24]); ident_d = k.inp("ident", [128, 128])
    z_d = k.outp("z0T", [3 * D, T])
    ident = k.sb("ident", [128, 128]); S.dma("sp", ident[:], ident_d, writes=["ident"])
    pA = Rot(k, "pA", [128, 1024], n=2, psum=True); pB = Rot(k, "pB", [128, 512], n=2, psum=True); pC = Rot(k, "pC", [128, 512], n=2, psum=True)
    A, fm = load_AB(k, n1g_d, [modv_d[0]], [modv_d[1]], ident, pC, 1)
    win = k.sb("win", [128, 8, 3 * D], BF16)
    S.dma("pool", win[:], win_d.rearrange("(kc p) n -> p kc n", p=128), writes=["win"])
    binfm = k.sb("binfm", [128, 24]); S.dma("sp", binfm[:], binfm_d, writes=["binfm"])
    aT = k.sb("aT", [128, 8, T], BF16)
    xin = Rot(k, "xin", [128, D], n=2); junk = k.sb("junk", [128, D], BF16)
    ssq = k.sb("ssq", [128, NT]); rstd = k.sb("rstd", [128, NT])
    for i in range(NT):
        xt, xk = xin.next()
        S.dma("sp", xt[:], h_d[i * 128:(i + 1) * 128, :], writes=[xk])
        S.op("act", lambda e, xt=xt, i=i: e.activation(out=junk[:], in_=xt[:], func=AF.Square, accum_out=ssq[:, i:i + 1]), reads=[xk], writes=["junk", ("ssq", i)])
        S.op("act", lambda e, i=i: e.activation(out=rstd[:, i:i + 1], in_=ssq[:, i:i + 1], func=AF.Sqrt, scale=1.0 / D, bias=EPS), reads=[("ssq", i)], writes=[("rstd", i)])
        S.op("dve", lambda e, i=i: e.reciprocal(out=rstd[:, i:i + 1], in_=rstd[:, i:i + 1]), reads=[("rstd", i)], writes=[("rstd", i)])
        S.op("dve", lambda e, xt=xt, i=i: e.tensor_scalar(out=xt[:], in0=xt[:], scalar1=rstd[:, i:i + 1], scalar2=None, op0=ALU.mult), reads=[xk, ("rstd", i)], writes=[xk])
        pt, ptk = pA.next()
        for c in range(8):
            S.op("pe", lambda e, pt=pt, xt=xt, c=c: e.transpose(out=pt[:, c * 128:(c + 1) * 128], in_=xt[:, c * 128:(c + 1) * 128], identity=ident[:]), reads=[xk, "ident"], writes=[ptk])
        for c in range(8):
            S.op("act", lambda e, pt=pt, c=c, i=i: e.activation(out=aT[:, c, i * 128:(i + 1) * 128], in_=pt[:, c * 128:(c + 1) * 128], func=AF.Identity,
                                                              scale=A[:, 0, c:c + 1], bias=fm[:, 1, c:c + 1]), reads=[ptk, "A1", "B1"], writes=[("aT", i)])
    zo = Rot(k, "zo", [128, 512], n=3)
    for j in range(24):
        for tb in range(T // 512):
            pg, pgk = pB.next()
            for kc in range(8):
                S.op("pe", lambda e, pg=pg, j=j, tb=tb, kc=kc: e.matmul(pg[:], win[:, kc, j * 128:(j + 1) * 128], aT[:, kc, tb * 512:(tb + 1) * 512], start=(kc == 0), stop=(kc == 7)),
                     reads=["win"] + [("aT", tb * 4 + q) for q in range(4)], writes=[pgk])
            z, zk = zo.next()
            S.op("act", lambda e, z=z, pg=pg, j=j: e.activation(out=z[:], in_=pg[:], func=AF.Identity, bias=binfm[:, j:j + 1], scale=1.0), reads=[pgk, "binfm"], writes=[zk])
            S.dma("sp", z_d[j * 128:(j + 1) * 128, tb * 512:(tb + 1) * 512], z[:], reads=[zk], writes=[("z", j, tb)])
    S.finish([]); es.close()
    return nc


TWO_PI = float(2 * np.pi)
MAGIC = 12582912.0


def build_hyconv(nc, L=16384, G=8, upto=4, ngroups=None, limit3=None):
    N2 = 2 * L
    assert L == 16384
    es = ExitStack(); k = K(nc, es); S = k.S
    z0_d = k.inp("z0", [3, 128, L]); cw_d = k.inp("cw", [128, 9]); cb_d = k.inp("cb", [128, 3]); skip_d = k.inp("skip", [128, 1]); ndel_d = k.inp("ndelta", [128, 1])
    w1_d = k.inp("fw1", [33, 64]); w2_d = k.inp("fw2", [64, 64]); w3_d = k.inp("fw3", [64, 64]); w4_d = k.inp("fw4", [64, 256])
    fbf_d = k.inp("fbf", [64, 4])
    zf_d = k.inp("zfeat", [33, N2]); tn_d = k.inp("tnorm", [N2])
    F1_d = k.inp("F1", [128, 2, 512]); TW_d = k.inp("TW", [128, 512]); F2_d = k.inp("F2", [128, 3, 128])
    Gt_d = k.inp("Gt", [128, 2, 256]); ITW_d = k.inp("ITW", [128, 2, 256]); F3_d = k.inp("F3", [128, 2, 2, 128])
    out_d = k.outp("zT", [128, L], BF16)
    vx_d = k.dram("vx_s", [128, L]); x0_d = k.dram("x0_s", [128, L]); taps_d = k.dram("taps_s", [128, N2]); y_d = k.dram("y_s", [128, L])
    pP = Rot(k, "pP", [128, 512], n=8, psum=True)
    ar = Arena(k, 204000)
    cw = k.sb("cw", [128, 9]); S.dma("sp", cw[:], cw_d, writes=["cw"]); cb = k.sb("cb", [128, 3]); S.dma("sp", cb[:], cb_d, writes=["cb"])
    skip = k.sb("skip", [128, 1]); S.dma("sp", skip[:], skip_d, writes=["skip"]); ndel = k.sb("ndel", [128, 1]); S.dma("sp", ndel[:], ndel_d, writes=["ndel"])
    TB = 2048
    m0 = ar.mark()
    zb = RotA(ar, "zb", [128, 3, TB + 2], n=2); yb = RotA(ar, "yb", [128, 3, TB], n=2)
    for b in range(L // TB):
        t0 = b * TB
        z, zk = zb.next(); y, yk = yb.next()
        lo = max(t0 - 1, 0); hi = min(t0 + TB + 1, L)
        if b == 0:
            S.op("dve", lambda e, z=z: e.memset(z[:, :, 0:1], 0.0), writes=[zk])
        if b == L // TB - 1:
            S.op("dve", lambda e, z=z: e.memset(z[:, :, TB + 1:TB + 2], 0.0), writes=[zk])
        S.dma("sp", z[:, :, lo - (t0 - 1):hi - (t0 - 1)], z0_d[:, :, lo:hi].rearrange("g p t -> p g t"), writes=[zk])
        for gi in range(3):
            eng = "dve"
            S.op(eng, lambda e, z=z, y=y, gi=gi: e.tensor_scalar(out=y[:, gi, :], in0=z[:, gi, 1:TB + 1], scalar1=cw[:, gi * 3 + 1:gi * 3 + 2], scalar2=cb[:, gi:gi + 1], op0=ALU.mult, op1=ALU.add),
                 reads=[zk, "cw", "cb"], writes=[(yk, gi)])
            S.op(eng, lambda e, z=z, y=y, gi=gi: e.scalar_tensor_tensor(out=y[:, gi, :], in0=z[:, gi, 0:TB], scalar=cw[:, gi * 3:gi * 3 + 1], in1=y[:, gi, :], op0=ALU.mult, op1=ALU.add),
                 reads=[zk, "cw", (yk, gi)], writes=[(yk, gi)])
            S.op(eng, lambda e, z=z, y=y, gi=gi: e.scalar_tensor_tensor(out=y[:, gi, :], in0=z[:, gi, 2:TB + 2], scalar=cw[:, gi * 3 + 2:gi * 3 + 3], in1=y[:, gi, :], op0=ALU.mult, op1=ALU.add),
                 reads=[zk, "cw", (yk, gi)], writes=[(yk, gi)])
        S.op("dve", lambda e, y=y: e.tensor_tensor(out=y[:, 2, :], in0=y[:, 2, :], in1=y[:, 1, :], op=ALU.mult), reads=[(yk, 1), (yk, 2)], writes=[(yk, 2)])
        S.dma("sp", vx_d[:, t0:t0 + TB], y[:, 2, :], reads=[(yk, 2)], writes=[("vx", b)])
        S.dma("act", x0_d[:, t0:t0 + TB], y[:, 0, :], reads=[(yk, 0)], writes=[("x0", b)])
    if upto < 2:
        S.finish([]); es.close(); return nc
    S.barrier(); ar.release(m0)
    w1 = ar.alloc([33, 64]); S.dma("sp", w1[:], w1_d, writes=["fw"]); w2 = ar.alloc([64, 64]); S.dma("sp", w2[:], w2_d, writes=["fw"])
    w3 = ar.alloc([64, 64]); S.dma("sp", w3[:], w3_d, writes=["fw"]); w4 = ar.alloc([64, 256]); S.dma("sp", w4[:], w4_d, writes=["fw"])
    fbf = ar.alloc([64, 8]); S.dma("sp", fbf[:, 0:4], fbf_d, writes=["fbf"])
    for i in range(3):
        S.op("dve", lambda e, i=i: e.tensor_tensor(out=fbf[:, 4 + i:5 + i], in0=fbf[:, 0:1], in1=fbf[:, 1 + i:2 + i], op=ALU.mult), reads=["fbf"], writes=["fbf"])
    zfr = RotA(ar, "zf", [33, 512], n=2); tnr = RotA(ar, "tn", [128, 512], n=2)
    pre = RotA(ar, "pre", [64, 512], n=2); tq = RotA(ar, "tq", [64, 512], n=2); hh = RotA(ar, "hh", [64, 512], n=2); tp = RotA(ar, "tp", [128, 512], n=2)
    ws = [w1, w2, w3]
    for b in range(N2 // 512):
        zf, zfk = zfr.next(); tn, tnk = tnr.next()
        S.dma("sp", zf[:], zf_d[:, b * 512:(b + 1) * 512], writes=[zfk])
        S.dma("act", tn[:], tn_d[b * 512:(b + 1) * 512].rearrange("(o n) -> o n", o=1).broadcast_to([128, 512]), writes=[tnk])
        cur, curk, kin = zf, zfk, 33
        for li in range(3):
            pp, ppk = pP.next()
            S.op("pe", lambda e, pp=pp, cur=cur, li=li, kin=kin: e.matmul(pp[0:64, :], ws[li][0:kin, :], cur[0:kin, :], start=True, stop=True), reads=["fw", curk], writes=[ppk])
            pr, prk = pre.next(); t, tk = tq.next(); h, hk = hh.next()
            S.op("act", lambda e, pr=pr, pp=pp, li=li: e.activation(out=pr[:], in_=pp[0:64, :], func=AF.Identity, scale=fbf[:, 0:1], bias=fbf[:, 4 + li:5 + li]), reads=[ppk, "fbf"], writes=[prk])
            S.op("dve", lambda e, t=t, pr=pr: e.tensor_scalar(out=t[:], in0=pr[:], scalar1=1.0 / TWO_PI, scalar2=MAGIC, op0=ALU.mult, op1=ALU.add), reads=[prk], writes=[tk])
            S.op("dve", lambda e, t=t: e.tensor_scalar(out=t[:], in0=t[:], scalar1=-MAGIC, scalar2=None, op0=ALU.add), reads=[tk], writes=[tk])
            S.op("dve", lambda e, t=t, pr=pr: e.scalar_tensor_tensor(out=t[:], in0=t[:], scalar=-TWO_PI, in1=pr[:], op0=ALU.mult, op1=ALU.add), reads=[tk, prk], writes=[tk])
            S.op("dve", lambda e, t=t: e.tensor_scalar(out=t[:], in0=t[:], scalar1=3.14159, scalar2=-3.14159, op0=ALU.min, op1=ALU.max), reads=[tk], writes=[tk])
            S.op("act", lambda e, h=h, t=t: e.activation(out=h[:], in_=t[:], func=AF.Sin), reads=[tk], writes=[hk])
            cur, curk, kin = h, hk, 64
        po, pok = pP.next()
        col0 = 0 if b < (L // 512) else 128
        S.op("pe", lambda e, po=po, cur=cur, col0=col0: e.matmul(po[:], w4[:, col0:col0 + 128], cur[:], start=True, stop=True), reads=["fw", curk], writes=[pok])
        S.op("act", lambda e, tn=tn: e.activation(out=tn[:], in_=tn[:], func=AF.Exp, scale=ndel[:, 0:1]), reads=[tnk, "ndel"], writes=[tnk])
        tpp, tpk = tp.next()
        S.op("dve", lambda e, tpp=tpp, po=po, tn=tn: e.tensor_tensor(out=tpp[:], in0=po[:], in1=tn[:], op=ALU.mult), reads=[pok, tnk], writes=[tpk])
        S.dma("sp", taps_d[:, b * 512:(b + 1) * 512], tpp[:], reads=[tpk], writes=[("taps", b)])
    if upto < 3:
        S.finish([]); es.close(); return nc
    S.barrier(); ar.release(m0)
    if limit3 is not None:
        S.nops = 0; S.limit = limit3
    F1 = ar.alloc([128, 2, 512]); S.dma("sp", F1[:], F1_d, writes=["tab"]); TW = ar.alloc([128, 512]); S.dma("sp", TW[:], TW_d, writes=["tab"])
    F2 = ar.alloc([128, 3, 128]); S.dma("sp", F2[:], F2_d, writes=["tab"]); Gt = ar.alloc([128, 2, 256]); S.dma("sp", Gt[:], Gt_d, writes=["tab"])
    ITW = ar.alloc([128, 2, 256]); S.dma("sp", ITW[:], ITW_d, writes=["tab"]); F3 = ar.alloc([128, 2, 2, 128]); S.dma("sp", F3[:], F3_d, writes=["tab"])
    xv = ar.alloc([128, G, 128]); xh = ar.alloc([128, 2, G, 128])
    B = {n: ar.alloc([128, G, 256]) for n in ["yre", "yim", "tre", "tim", "ta", "tb", "vre", "vim", "hre", "him"]}
    zre = ar.alloc([128, 2, G, 128]); zim = ar.alloc([128, 2, G, 128]); zr2 = ar.alloc([128, 2, G, 128]); zi2 = ar.alloc([128, 2, G, 128])
    zta = ar.alloc([128, 2, G, 128]); ztb = ar.alloc([128, 2, G, 128])
    yo = ar.alloc([128, G, 128])
    twc = bcast_ap(TW[:, 0:256], G); tws = bcast_ap(TW[:, 256:512], G)

    def fwd_fft(src_fn, nchunk, ore, oim, tag):
        for c in range(G):
            pp, ppk = pP.next()
            for q in range(nchunk):
                S.op("pe", lambda e, pp=pp, c=c, q=q: e.matmul(pp[:], src_fn(c, q), F1[:, q, :], start=(q == 0), stop=(q == nchunk - 1)), reads=[tag, "tab"], writes=[ppk])
            if c % 2 == 0:
                S.op("act", lambda e, pp=pp, c=c: e.copy(out=B["yre"][:, c, :], in_=pp[:, 0:256]), reads=[ppk], writes=["yre"])
                S.op("act", lambda e, pp=pp, c=c: e.copy(out=B["yim"][:, c, :], in_=pp[:, 256:512]), reads=[ppk], writes=["yim"])
            else:
                S.op("dve", lambda e, pp=pp, c=c: e.tensor_copy(out=B["yre"][:, c, :], in_=pp[:, 0:256]), reads=[ppk], writes=["yre"])
                S.op("dve", lambda e, pp=pp, c=c: e.tensor_copy(out=B["yim"][:, c, :], in_=pp[:, 256:512]), reads=[ppk], writes=["yim"])
        S.op("dve", lambda e: e.tensor_tensor(out=B["ta"][:], in0=B["yre"][:], in1=twc, op=ALU.mult), reads=["yre", "tab"], writes=["ta"])
        S.op("pool", lambda e: e.tensor_tensor(out=B["tb"][:], in0=B["yim"][:], in1=tws, op=ALU.mult), reads=["yim", "tab"], writes=["tb"])
        S.op("dve", lambda e: e.tensor_tensor(out=B["tre"][:], in0=B["ta"][:], in1=B["tb"][:], op=ALU.add), reads=["ta", "tb"], writes=["tre"])
        S.op("dve", lambda e: e.tensor_tensor(out=B["ta"][:], in0=B["yim"][:], in1=twc, op=ALU.mult), reads=["yim", "tab", "tre"], writes=["ta"])
        S.op("pool", lambda e: e.tensor_tensor(out=B["tb"][:], in0=B["yre"][:], in1=tws, op=ALU.mult), reads=["yre", "tab", "tre"], writes=["tb"])
        S.op("dve", lambda e: e.tensor_tensor(out=B["tim"][:], in0=B["ta"][:], in1=B["tb"][:], op=ALU.subtract), reads=["ta", "tb"], writes=["tim"])
        for j in range(G // 2):
            rre = B["tre"][:, 2 * j:2 * j + 2, :].rearrange("p c k -> p (c k)"); rim = B["tim"][:, 2 * j:2 * j + 2, :].rearrange("p c k -> p (c k)")
            p1, p1k = pP.next(); p2, p2k = pP.next()
            S.op("pe", lambda e, p1=p1, rre=rre: e.matmul(p1[:], F2[:, 0, :], rre, start=True, stop=False), reads=["tre", "tab"], writes=[p1k])
            S.op("pe", lambda e, p1=p1, rim=rim: e.matmul(p1[:], F2[:, 1, :], rim, start=False, stop=True), reads=["tim", "tab"], writes=[p1k])
            S.op("pe", lambda e, p2=p2, rim=rim: e.matmul(p2[:], F2[:, 0, :], rim, start=True, stop=False), reads=["tim", "tab"], writes=[p2k])
            S.op("pe", lambda e, p2=p2, rre=rre: e.matmul(p2[:], F2[:, 2, :], rre, start=False, stop=True), reads=["tre", "tab"], writes=[p2k])
            S.op("act", lambda e, p1=p1, j=j: e.copy(out=ore[:, 2 * j:2 * j + 2, :].rearrange("p c k -> p (c k)"), in_=p1[:]), reads=[p1k], writes=[tag + "re"])
            S.op("dve", lambda e, p2=p2, j=j: e.tensor_copy(out=oim[:, 2 * j:2 * j + 2, :].rearrange("p c k -> p (c k)"), in_=p2[:]), reads=[p2k], writes=[tag + "im"])

    for g in range(ngroups if ngroups else 128 // G):
        cs = slice(g * G, (g + 1) * G)
        S.dma("sp", xv[:], vx_d[cs, :].rearrange("c (n2 n1) -> n2 c n1", n1=128), reads=[("vx", b) for b in range(L // TB)], writes=["xv"])
        for q in range(2):
            S.dma("act", xh[:, q], taps_d[cs, q * L:(q + 1) * L].rearrange("c (n2 n1) -> n2 c n1", n1=128), writes=["xh"])
        fwd_fft(lambda c, q: xv[:, c, :], 1, B["vre"], B["vim"], "xv")
        fwd_fft(lambda c, q: xh[:, q, c, :], 2, B["hre"], B["him"], "xh")
        S.op("dve", lambda e: e.tensor_tensor(out=B["ta"][:], in0=B["vre"][:], in1=B["hre"][:], op=ALU.mult), reads=["xvre", "xhre", "tre", "tim"], writes=["ta"])
        S.op("pool", lambda e: e.tensor_tensor(out=B["tb"][:], in0=B["vim"][:], in1=B["him"][:], op=ALU.mult), reads=["xvim", "xhim", "tre", "tim"], writes=["tb"])
        S.op("dve", lambda e: e.tensor_tensor(out=B["tre"][:], in0=B["ta"][:], in1=B["tb"][:], op=ALU.subtract), reads=["ta", "tb"], writes=["tre"])
        S.op("dve", lambda e: e.tensor_tensor(out=B["ta"][:], in0=B["vre"][:], in1=B["him"][:], op=ALU.mult), reads=["xvre", "xhim", "tre"], writes=["ta"])
        S.op("pool", lambda e: e.tensor_tensor(out=B["tb"][:], in0=B["vim"][:], in1=B["hre"][:], op=ALU.mult), reads=["xvim", "xhre", "tre"], writes=["tb"])
        S.op("dve", lambda e: e.tensor_tensor(out=B["tim"][:], in0=B["ta"][:], in1=B["tb"][:], op=ALU.add), reads=["ta", "tb"], writes=["tim"])
        for c in range(G):
            for q in range(2):
                pz, pzk = pP.next()
                S.op("pe", lambda e, pz=pz, c=c, q=q: e.matmul(pz[:, 0:256], B["tre"][:, c, q * 128:(q + 1) * 128], Gt[:, 0, :], start=True, stop=False), reads=["tre", "tab"], writes=[pzk])
                S.op("pe", lambda e, pz=pz, c=c, q=q: e.matmul(pz[:, 0:256], B["tim"][:, c, q * 128:(q + 1) * 128], Gt[:, 1, :], start=False, stop=True), reads=["tim", "tab"], writes=[pzk])
                if q == 0:
                    S.op("act", lambda e, pz=pz, c=c, q=q: e.copy(out=zre[:, q, c, :], in_=pz[:, 0:128]), reads=[pzk], writes=["zre"])
                    S.op("act", lambda e, pz=pz, c=c, q=q: e.copy(out=zim[:, q, c, :], in_=pz[:, 128:256]), reads=[pzk], writes=["zim"])
                else:
                    S.op("dve", lambda e, pz=pz, c=c, q=q: e.tensor_copy(out=zre[:, q, c, :], in_=pz[:, 0:128]), reads=[pzk], writes=["zre"])
                    S.op("dve", lambda e, pz=pz, c=c, q=q: e.tensor_copy(out=zim[:, q, c, :], in_=pz[:, 128:256]), reads=[pzk], writes=["zim"])
        for q in range(2):
            ic = bcast_ap(ITW[:, q, 0:128], G); isn = bcast_ap(ITW[:, q, 128:256], G)
            S.op("dve", lambda e, q=q, ic=ic: e.tensor_tensor(out=zta[:, q], in0=zre[:, q], in1=ic, op=ALU.mult), reads=["zre", "tab"], writes=[("zta", q)])
            S.op("pool", lambda e, q=q, isn=isn: e.tensor_tensor(out=ztb[:, q], in0=zim[:, q], in1=isn, op=ALU.mult), reads=["zim", "tab"], writes=[("ztb", q)])
            S.op("dve", lambda e, q=q: e.tensor_tensor(out=zr2[:, q], in0=zta[:, q], in1=ztb[:, q], op=ALU.subtract), reads=[("zta", q), ("ztb", q)], writes=[("zr2", q)])
            S.op("dve", lambda e, q=q, isn=isn: e.tensor_tensor(out=zta[:, q], in0=zre[:, q], in1=isn, op=ALU.mult), reads=["zre", "tab", ("zr2", q)], writes=[("zta", q)])
            S.op("pool", lambda e, q=q, ic=ic: e.tensor_tensor(out=ztb[:, q], in0=zim[:, q], in1=ic, op=ALU.mult), reads=["zim", "tab", ("zr2", q)], writes=[("ztb", q)])
            S.op("dve", lambda e, q=q: e.tensor_tensor(out=zi2[:, q], in0=zta[:, q], in1=ztb[:, q], op=ALU.add), reads=[("zta", q), ("ztb", q)], writes=[("zi2", q)])
        for j in range(G // 4):
            py, pyk = pP.next()
            n = 0
            for q in range(2):
                for (zz, zk, ri) in ((zr2, "zr2", 0), (zi2, "zi2", 1)):
                    rhs = zz[:, q, 4 * j:4 * j + 4, :].rearrange("p c m -> p (c m)")
                    S.op("pe", lambda e, py=py, q=q, ri=ri, rhs=rhs, n=n: e.matmul(py[:], F3[:, q, ri, :], rhs, start=(n == 0), stop=(n == 3)), reads=[(zk, q), "tab"], writes=[pyk])
                    n += 1
            S.op("act", lambda e, py=py, j=j: e.copy(out=yo[:, 4 * j:4 * j + 4, :].rearrange("p c m -> p (c m)"), in_=py[:]), reads=[pyk], writes=["yo"])
        S.dma("sp", y_d[cs, :].rearrange("c (m2 m1) -> m2 c m1", m1=128), yo[:], reads=["yo"], writes=[("y", g)])
    if upto < 4:
        S.finish([]); es.close(); return nc
    S.barrier(); ar.release(m0)
    fa = RotA(ar, "fa", [128, TB], n=2); fbb = RotA(ar, "fbb", [128, TB], n=2); fc = RotA(ar, "fc", [128, TB], n=2); fo = RotA(ar, "fo", [128, TB], BF16, n=2)
    for b in range(L // TB):
        t0 = b * TB
        a, ak = fa.next(); bb, bk = fbb.next(); c, ck = fc.next(); o, ok = fo.next()
        S.dma("sp", a[:], y_d[:, t0:t0 + TB], writes=[ak]); S.dma("act", bb[:], vx_d[:, t0:t0 + TB], writes=[bk]); S.dma("sp", c[:], x0_d[:, t0:t0 + TB], writes=[ck])
        S.op("dve", lambda e, a=a, bb=bb: e.scalar_tensor_tensor(out=a[:], in0=bb[:], scalar=skip[:, 0:1], in1=a[:], op0=ALU.mult, op1=ALU.add), reads=[ak, bk, "skip"], writes=[ak])
        S.op("pool", lambda e, a=a, c=c, o=o: e.tensor_tensor(out=o[:], in0=a[:], in1=c[:], op=ALU.mult), reads=[ak, ck], writes=[ok])
        S.dma("sp", out_d[:, t0:t0 + TB], o[:], reads=[ok], writes=[("o", b)])
    S.finish([]); es.close()
    return nc


def hy_tables(L=16384):
    N = 2 * L
    f32 = np.float32
    p = np.arange(128)
    n2 = (np.arange(2)[:, None] * 128 + p[None, :]).T
    k2 = np.arange(256)
    ang = 2 * np.pi * (n2[:, :, None] * k2[None, None, :] % 256) / 256
    F1 = np.concatenate([np.cos(ang), -np.sin(ang)], -1).astype(f32)
    th = 2 * np.pi * (p[:, None] * k2[None, :]) / N
    TW = np.concatenate([np.cos(th), np.sin(th)], -1).astype(f32)
    a2 = 2 * np.pi * (p[:, None] * p[None, :] % 128) / 128
    F2 = np.stack([np.cos(a2), np.sin(a2), -np.sin(a2)], 1).astype(f32)
    Gt = np.stack([np.concatenate([np.cos(a2), np.sin(a2)], -1), np.concatenate([-np.sin(a2), np.cos(a2)], -1)], 1).astype(f32)
    ph = 2 * np.pi * (n2[:, :, None] * p[None, None, :]) / N
    ITW = np.concatenate([np.cos(ph), np.sin(ph)], -1).astype(f32)
    a3 = 2 * np.pi * (n2[:, :, None] * p[None, None, :] % 256) / 256
    F3 = np.stack([np.cos(a3) / N, -np.sin(a3) / N], 2).astype(f32)
    lag = np.concatenate([np.arange(L), L - np.arange(L)]).astype(np.int64)
    t = np.linspace(0.0, 1.0, L, dtype=f32)
    w = (2.0 * np.pi / L) * np.arange(L, dtype=f32)
    f = np.linspace(1e-4, 15, 16, dtype=f32)
    feat = np.concatenate([t[:, None], np.cos(f * w[:, None]), -np.sin(f * w[:, None])], -1).astype(f32)
    lagc = np.minimum(lag, L - 1)
    zfeat = np.ascontiguousarray(feat[lagc].T)
    tnorm = t[lagc].copy(); tnorm[L] = 1e6
    return dict(F1=F1, TW=TW, F2=F2, Gt=Gt, ITW=ITW, F3=F3, zfeat=zfeat, tnorm=tnorm.astype(f32))


import math
import ml_dtypes
from concourse.bass_utils import run_bass_kernel_spmd

NCORES = 8
_BF = ml_dtypes.bfloat16


def _run(build, maps, **kw):
    import sys, time
    t0 = time.time()
    nc = bass.Bass("TRN2", target_bir_lowering=False)
    build(nc, **kw)
    res = run_bass_kernel_spmd(nc, maps, core_ids=list(range(len(maps))))
    print("[launch %s] %.1fs" % (build.__name__, time.time() - t0), file=sys.stderr, flush=True)
    return res.results


def _rope_tables(L, W=64):
    row = np.repeat(np.arange(L // W), W); col = np.tile(np.arange(W), L // W)
    pos = np.stack([row, col], -1).astype(np.float32)
    inv = (np.float32(10000.0) ** (-np.arange(32, dtype=np.float32) / np.float32(32))).astype(np.float32)
    ang = pos[:, :, None] * inv
    return np.cos(ang).astype(np.float32).reshape(L, 64), np.sin(ang).astype(np.float32).reshape(L, 64)


def kernel(x, c, ctx, c_ctx, ada_w, ada_b, norm1_g, norm2_g, ev_w_in, ev_b_in, ev_q_gain, ev_k_gain,
           ev_f_bias, ev_h_gain, ev_w_out, ev_b_out, od_w_in, od_b_in, od_conv_w, od_conv_b, od_filt_w1,
           od_filt_b1, od_filt_freq, od_filt_w2, od_filt_b2, od_filt_w3, od_filt_b3, od_filt_w4, od_skip,
           od_w_out, od_b_out, router_w, router_b, moe_w_gu, moe_b_gu, moe_w_down, moe_b_down, final_g):
    f32 = np.float32
    A = lambda a: np.ascontiguousarray(np.asarray(a))
    x = np.asarray(x, f32); ctx = np.asarray(ctx, f32)
    L = x.shape[1]; TQ = L // NCORES; NCT = ctx.shape[1]
    ident = np.eye(128, dtype=f32)
    W = 6 * D // NCORES
    r = _run(build_ada, [dict(c=A(c), c_ctx=A(c_ctx), ada_w=A(ada_w[:, :, i * W:(i + 1) * W]), ada_b=A(ada_b[:, i * W:(i + 1) * W]), ident=ident) for i in range(NCORES)])
    modv = np.concatenate([q["modv"] for q in r], axis=-1)
    cosf, sinf = _rope_tables(L)
    mv0 = A(modv[0].reshape(2, 6, D))
    r = _run(build_proj0, [dict(x=A(x[0, i * TQ:(i + 1) * TQ]), ctx=A(ctx[0]), modv=mv0, norm1_g=A(norm1_g[0]), w_in=A(ev_w_in[0]), b_in=A(ev_b_in[0]),
                                q_gain=A(ev_q_gain[0]), k_gain=A(ev_k_gain[0]), cos=A(cosf[i * TQ:(i + 1) * TQ]), sin=A(sinf[i * TQ:(i + 1) * TQ]), ident=ident)
                           for i in range(NCORES)], TL=TQ, TC=NCT)
    QT = [q["QT"] for q in r]
    KT_all = A(np.concatenate([q["KT"][:, :, :TQ] for q in r] + [r[0]["KT"][:, :, TQ:]], axis=2))
    V_all = A(np.concatenate([q["V"][:TQ] for q in r] + [r[0]["V"][TQ:]], axis=0))
    PM_lat = np.concatenate([q["PM"][:TQ] for q in r], axis=0); PM_ctx = r[0]["PM"][TQ:]
    tri = np.triu(np.ones((128, 128), f32))
    maps = []
    for d in range(2):
        for hh in range(4):
            def seq(cols):
                a, b = PM_ctx[:, cols], PM_lat[:, cols]
                if d == 1:
                    a, b = a[::-1], b[::-1]
                return np.concatenate([a, b], axis=0)
            q_ = seq(slice(hh * 64, (hh + 1) * 64)); k_ = seq(slice(256 + hh * 64, 256 + (hh + 1) * 64)); v_ = seq(slice(512 + hh * 128, 512 + (hh + 1) * 128))
            ig_ = seq(slice(1024 + d * 4 + hh, 1024 + d * 4 + hh + 1))[:, 0]; fg_ = seq(slice(1032 + d * 4 + hh, 1032 + d * 4 + hh + 1))[:, 0]
            NCk = q_.shape[0] // 128
            maps.append(dict(qT=A(q_.T), kT=A(k_.T), k=A(k_), v=A(v_), ig=A(ig_.reshape(NCk, 128).T), fg=A(fg_.reshape(NCk, 128).T),
                             fb=np.full((128, 1), ev_f_bias[0, d, hh], f32), tri=tri, ident=ident))
    r = _run(build_mlstm, maps, NS=L + NCT, NCTX=NCT // 128)
    hf = np.concatenate([r[hh]["h"] for hh in range(4)], axis=1)
    hb = np.concatenate([r[4 + hh]["h"][::-1] for hh in range(4)], axis=1)
    r = _run(build_attn, [dict(QT=QT[i], KT=KT_all, V=V_all, hf=A(hf[i * TQ:(i + 1) * TQ]), hb=A(hb[i * TQ:(i + 1) * TQ]), og=A(PM_lat[i * TQ:(i + 1) * TQ, 1040:1552]),
                               h_gain=A(ev_h_gain[0]), ident=ident) for i in range(NCORES)], TQ=TQ, NK=L + NCT)
    mix0 = [q["mixT"] for q in r]

    NPC = 4
    TP = L // NPC

    def post(layer, hs, mix, w_out, b_out, final):
        hcat = np.concatenate(hs, axis=0); mcat = np.concatenate(mix, axis=1)
        r_ = _run(build_post, [dict(h=A(hcat[i * TP:(i + 1) * TP]), mixT=A(mcat[:, i * TP:(i + 1) * TP]), w_out=A(w_out), b_out=A(b_out), norm2_g=A(norm2_g[layer]),
                                    modv=A(modv[layer, 0].reshape(6, D)), router_w=A(router_w[layer]), router_b=A(router_b[layer]), w_gu=A(moe_w_gu[layer]),
                                    b_gu=A(moe_b_gu[layer]), w_down=A(moe_w_down[layer]), b_down=A(moe_b_down[layer]), final_g=A(final_g), ident=ident)
                               for i in range(NPC)], T=TP, TBM=512, NEXP=NE, final=final)
        hall = np.concatenate([q["hout"] for q in r_], axis=0)
        return [dict(hout=A(hall[i * TQ:(i + 1) * TQ])) for i in range(NCORES)]
    r = post(0, [A(x[0, i * TQ:(i + 1) * TQ]) for i in range(NCORES)], mix0, ev_w_out[0], ev_b_out[0], False)
    h1 = [q["hout"] for q in r]
    r = _run(build_hyproj, [dict(h=h1[i], modv=A(modv[1, 0].reshape(6, D)), norm1_g=A(norm1_g[1]), w_in=A(od_w_in[0]), b_in_fm=A(od_b_in[0].reshape(24, 128).T), ident=ident)
                            for i in range(NCORES)], T=TQ)
    z0T = np.concatenate([q["z0T"] for q in r], axis=1)
    tabs = hy_tables(L)
    deltas = np.abs(np.linspace(math.log(1e-2) / 1.5, math.log(1e-2) / 0.3, D, dtype=f32))
    maps = []
    for j in range(NCORES):
        ch = slice(j * 128, (j + 1) * 128)
        cw = np.stack([od_conv_w[0][:, g * D + j * 128:g * D + (j + 1) * 128] for g in range(3)], 0)
        maps.append(dict(z0=A(np.stack([z0T[g * D + j * 128:g * D + (j + 1) * 128] for g in range(3)], 0)), cw=A(cw.transpose(2, 0, 1).reshape(128, 9)),
                         cb=A(np.stack([od_conv_b[0][g * D + j * 128:g * D + (j + 1) * 128] for g in range(3)], 1)), skip=A(od_skip[0][ch].reshape(128, 1)),
                         ndelta=A((-deltas[ch]).reshape(128, 1).astype(f32)), fw1=A(od_filt_w1[0]), fw2=A(od_filt_w2[0]), fw3=A(od_filt_w3[0]),
                         fw4=A(np.concatenate([od_filt_w4[0][:, ch], od_filt_w4[0][:, D + j * 128:D + (j + 1) * 128]], 1)),
                         fbf=A(np.stack([od_filt_freq[0], od_filt_b1[0], od_filt_b2[0], od_filt_b3[0]], 1)), **tabs))
    r = _run(build_hyconv, maps, L=L)
    zT = np.concatenate([q["zT"] for q in r], axis=0)
    mix1 = [A(zT[:, i * TQ:(i + 1) * TQ]) for i in range(NCORES)]
    r = post(1, h1, mix1, od_w_out[0], od_b_out[0], True)
    out = np.concatenate([q["hout"] for q in r], axis=0)[None]
    return out.astype(np.float32)
```

```python
import numpy as np
import concourse.bass as bass
import concourse.mybir as mybir

F32 = mybir.dt.float32
BF16 = mybir.dt.bfloat16
I32 = mybir.dt.int32
U32 = mybir.dt.uint32
AF = mybir.ActivationFunctionType
ALU = mybir.AluOpType
AX = mybir.AxisListType


class Sched:
    CE = ("pe", "act", "dve", "pool", "sp")

    def __init__(self, nc, n_dma_sems=8):
        self.nc = nc
        self.ops = {e: [] for e in self.CE}
        self.sem = {}
        self.cnt = {}
        self.seen = {e: {} for e in self.CE}
        self.last_w = {}
        self.readers = {}
        self._stack = []
        for e in self.CE:
            self.sem[e] = nc.alloc_semaphore(name="s_" + e)
            self.cnt[e] = 0
        self.dq = {}
        for q in ("sp", "act", "pool"):
            lst = []
            for i in range(n_dma_sems):
                nm = "d_%s%d" % (q, i)
                self.sem[nm] = nc.alloc_semaphore(name=nm)
                self.cnt[nm] = 0
                lst.append(nm)
            self.dq[q] = [lst, 0]
        self.n_inst = 0
        self.limit = None
        self.nops = 0

    def _deps(self, eng, reads, writes):
        deps = {}
        def add(tok):
            f, v = tok
            if deps.get(f, 0) < v:
                deps[f] = v
        for k in reads:
            t = self.last_w.get(k)
            if t is not None:
                add(t)
        for k in writes:
            t = self.last_w.get(k)
            if t is not None:
                add(t)
            for t in self.readers.get(k, {}).items():
                if t[0] != eng:
                    add(t)
        out = []
        for f, v in deps.items():
            if f == eng and eng == "pe":
                continue
            if self.seen[eng].get(f, 0) >= v:
                continue
            self.seen[eng][f] = v
            out.append((f, v))
        return out

    def _commit(self, tok, reads, writes):
        for k in writes:
            self.last_w[k] = tok
            self.readers[k] = {}
        for k in reads:
            self.readers.setdefault(k, {})[tok[0]] = tok[1]

    def op(self, eng, fn, reads=(), writes=()):
        self.nops += 1
        if self.limit is not None and self.nops > self.limit:
            return None
        waits = self._deps(eng, reads, writes)
        self.cnt[eng] += 1
        tok = (eng, self.cnt[eng])
        sem = self.sem[eng]
        sems = self.sem
        def emit(e, fn=fn, waits=waits, sem=sem):
            for f, v in waits:
                e.wait_ge(sems[f], v)
            fn(e).then_inc(sem, 1)
        self.ops[eng].append(emit)
        self._commit(tok, reads, writes)
        self.n_inst += 1 + len(waits)
        return tok

    def dma(self, q, out, in_, reads=(), writes=(), **kw):
        self.nops += 1
        if self.limit is not None and self.nops > self.limit:
            return None
        lst, idx = self.dq[q]
        nm = lst[idx % len(lst)]
        self.dq[q][1] = idx + 1
        waits = self._deps(q, reads, writes)
        prev = self.cnt[nm]
        if prev > 0 and self.seen[q].get(nm, 0) < prev:
            self.seen[q][nm] = prev
            waits.append((nm, prev))
        self.cnt[nm] += 16
        tok = (nm, self.cnt[nm])
        sem = self.sem[nm]
        sems = self.sem
        def emit(e, waits=waits, sem=sem, out=out, in_=in_, kw=kw):
            for f, v in waits:
                e.wait_ge(sems[f], v)
            e.dma_start(out=out, in_=in_, **kw).then_inc(sem, 16)
        self.ops[q].append(emit)
        self._commit(tok, reads, writes)
        self.n_inst += 1 + len(waits)
        return tok

    def barrier(self):
        sems = self.sem
        for e in self.CE:
            waits = []
            for f, v in self.cnt.items():
                if f == e or v == 0:
                    continue
                if self.seen[e].get(f, 0) >= v:
                    continue
                self.seen[e][f] = v
                waits.append((f, v))
            if waits:
                def emit(eng, waits=waits):
                    for f, v in waits:
                        eng.wait_ge(sems[f], v)
                self.ops[e].append(emit)
                self.n_inst += len(waits)
        self.last_w = {}
        self.readers = {}

    def finish(self, out_keys):
        self.barrier()
        waits = []
        sems = self.sem
        def emit(e, waits=waits):
            for f, v in waits:
                e.wait_ge(sems[f], v)
        self.ops["sp"].append(emit)
        nc = self.nc
        ops = self.ops
        with nc.Block() as block:
            @block.tensor
            def _(e):
                for f in ops["pe"]:
                    f(e)
            @block.scalar
            def _(e):
                for f in ops["act"]:
                    f(e)
            @block.vector
            def _(e):
                for f in ops["dve"]:
                    f(e)
            @block.gpsimd
            def _(e):
                for f in ops["pool"]:
                    f(e)
            @block.sync
            def _(e):
                for f in ops["sp"]:
                    f(e)


from contextlib import ExitStack

D = 1024
NE = 32
EPS = 1e-6


class K:
    def __init__(self, nc, es):
        self.nc = nc
        self.es = es
        self.S = Sched(nc)
        self.nm = 0

    def sb(self, name, shape, dt=F32):
        return self.es.enter_context(self.nc.sbuf_tensor("s_" + name, shape, dt))

    def ps(self, name, shape, dt=F32):
        return self.es.enter_context(self.nc.psum_tensor("p_" + name, shape, dt))

    def dram(self, name, shape, dt=F32, kind="Internal"):
        return self.nc.dram_tensor(name, shape, dt, kind=kind).ap()

    def inp(self, name, shape, dt=F32):
        return self.nc.dram_tensor(name, shape, dt, kind="ExternalInput").ap()

    def outp(self, name, shape, dt=F32):
        return self.nc.dram_tensor(name, shape, dt, kind="ExternalOutput").ap()


class Rot:
    def __init__(self, k, name, shape, dt=F32, n=2, psum=False):
        self.t = [(k.ps if psum else k.sb)("%s%d" % (name, i), shape, dt) for i in range(n)]
        self.keys = ["%s%d" % (name, i) for i in range(n)]
        self.i = 0

    def next(self):
        j = self.i % len(self.t)
        self.i += 1
        return self.t[j], self.keys[j]


def load_fm(k, dst, dst_key, src2d, n, ident, scratch, scratch_key, pst, pst_key):
    S = k.S
    S.dma("sp", scratch[0:n, 0:128], src2d, writes=[scratch_key])
    S.op("pe", lambda e: e.transpose(out=pst[:, 0:n], in_=scratch[0:n, 0:128], identity=ident[0:n, 0:n]),
         reads=[scratch_key, "ident"], writes=[pst_key])
    S.op("dve", lambda e: e.tensor_copy(out=dst, in_=pst[:, 0:n]), reads=[pst_key], writes=[dst_key])


def adaln_fm(k, layer, c_in, cctx_in, ada_w, ada_b, ident, modfm, tmp):
    S = k.S
    sc, pst, wbuf = tmp["sc"], tmp["pst"], tmp["wbuf"]
    S.dma("sp", sc[0:8, 0:128], c_in.rearrange("o (k p) -> (o k) p", p=128), writes=["sc"])
    S.dma("sp", sc[8:16, 0:128], cctx_in.rearrange("(k p) -> k p", p=128), writes=["sc"])
    S.op("act", lambda e: e.activation(out=sc[0:16, 128:256], in_=sc[0:16, 0:128], func=AF.Silu), reads=["sc"], writes=["sc"])
    S.op("pe", lambda e: e.transpose(out=pst[:, 0:16], in_=sc[0:16, 128:256], identity=ident[0:16, 0:16]),
         reads=["sc", "ident"], writes=["pst"])
    cfm = tmp["cfm"]
    S.op("dve", lambda e: e.tensor_copy(out=cfm[:], in_=pst[:, 0:16]), reads=["pst"], writes=["cfm"])
    bfm = tmp["bfm"]
    load_fm(k, bfm[:], "bfm", ada_b[layer].rearrange("(j p) -> j p", p=128), 48, ident, sc, "sc", pst, "pst")
    aw = ada_w[layer].rearrange("(kc p) n -> p kc n", p=128)
    pm = tmp["pm"]
    for g in range(12):
        wt, wk = wbuf.next()
        S.dma("sp" if g % 2 == 0 else "act", wt[:], aw[:, :, g * 512:(g + 1) * 512], writes=[wk])
        for j in range(4):
            jj = g * 4 + j
            for kc in range(8):
                S.op("pe", lambda e, wt=wt, j=j, kc=kc, jj=jj: e.matmul(
                    pm[:, jj * 2:jj * 2 + 2], wt[:, kc, j * 128:(j + 1) * 128],
                    cfm[:, kc:kc + 9:8], start=(kc == 0), stop=(kc == 7)),
                    reads=[wk, "cfm"], writes=["pm"])
    for col in range(2):
        S.op("dve", lambda e, col=col: e.tensor_tensor(out=modfm[:, :, col], in0=pm[:, col:96:2], in1=bfm[:], op=ALU.add),
             reads=["pm", "bfm"], writes=["modfm"])


def bcast_rows(k, dst, dst_key, src_fm, src_key, n, ident, ones, diag, pb):
    S = k.S
    for j in range(n):
        dg, dk = diag.next()
        pt, pk = pb.next()
        S.op("dve", lambda e, dg=dg, j=j: e.tensor_scalar(out=dg[:], in0=ident[:], scalar1=src_fm[:, j:j + 1], scalar2=None, op0=ALU.mult),
             reads=["ident", src_key], writes=[dk])
        S.op("pe", lambda e, dg=dg, pt=pt: e.matmul(pt[:, 0:128], ones[:], dg[:], start=True, stop=True),
             reads=["ones", dk], writes=[pk])
        S.op("act", lambda e, pt=pt, j=j: e.copy(out=dst[:, j * 128:(j + 1) * 128], in_=pt[:, 0:128]),
             reads=[pk], writes=[dst_key])


class Arena:
    def __init__(self, k, nbytes=212000):
        self.k = k
        self.t = k.es.enter_context(k.nc.sbuf_tensor("arena", [128, nbytes], mybir.dt.uint8))
        self.n = nbytes
        self.off = 0

    def alloc(self, shape, dt=F32):
        sz = {F32: 4, BF16: 2, I32: 4, U32: 4}[dt]
        n = sz
        for s in shape[1:]:
            n *= s
        n_al = (n + 63) // 64 * 64
        assert self.off + n_al <= self.n, ("arena overflow", self.off, n_al, self.n)
        v = self.t[:, self.off:self.off + n].bitcast(dt)
        self.off += n_al
        if len(shape) == 3:
            v = v.rearrange("p (a b) -> p a b", a=shape[1])
        elif len(shape) == 4:
            v = v.rearrange("p (a b c) -> p a b c", a=shape[1], b=shape[2])
        if shape[0] < 128:
            v = v[0:shape[0]]
        return v

    def mark(self):
        return self.off

    def release(self, m):
        self.off = m


class RotA:
    def __init__(self, ar, name, shape, dt=F32, n=2):
        self.t = [ar.alloc(shape, dt) for _ in range(n)]
        self.keys = ["%s%d" % (name, i) for i in range(n)]
        self.i = 0

    def next(self):
        j = self.i % len(self.t)
        self.i += 1
        return self.t[j], self.keys[j]


def bc_load(k, dst, key, vec_ap, n):
    k.S.dma("sp", dst, vec_ap.rearrange("(o n) -> o n", o=1).broadcast_to([128, n]), writes=[key])


def build_post(nc, T=2048, TBM=1024, NEXP=32, final=False):
    NT = T // 128
    es = ExitStack()
    k = K(nc, es)
    S = k.S
    h_d = k.inp("h", [T, D]); mixT_d = k.inp("mixT", [D, T], BF16)
    wout_d = k.inp("w_out", [D, D]); bout_d = k.inp("b_out", [D])
    n2g_d = k.inp("norm2_g", [D]); modv_d = k.inp("modv", [6, D])
    rw_d = k.inp("router_w", [D, NE]); rb_d = k.inp("router_b", [NE])
    wgu_d = k.inp("w_gu", [NEXP, D, 2 * D]); bgu_d = k.inp("b_gu", [NE, 2 * D])
    wdn_d = k.inp("w_down", [NEXP, D, D]); bdn_d = k.inp("b_down", [NE, D])
    fg_d = k.inp("final_g", [D]); ident_d = k.inp("ident", [128, 128])
    out_d = k.outp("hout", [T, D])
    hmid_d = k.dram("hmid", [T, D])

    ident = k.sb("ident", [128, 128]); S.dma("sp", ident[:], ident_d, writes=["ident"])
    g2bc = k.sb("g2bc", [128, D]); bc_load(k, g2bc[:], "g2bc", modv_d[2], D)
    g5bc = k.sb("g5bc", [128, D]); bc_load(k, g5bc[:], "g5bc", modv_d[5], D)
    boutbc = k.sb("boutbc", [128, D]); bc_load(k, boutbc[:], "boutbc", bout_d, D)
    rbbc = k.sb("rbbc", [128, NE]); bc_load(k, rbbc[:], "rbbc", rb_d, NE)
    if final:
        fgbc = k.sb("fgbc", [128, D]); bc_load(k, fgbc[:], "fgbc", fg_d, D)
    pA = Rot(k, "pA", [128, 1024], n=2, psum=True)
    pB = Rot(k, "pB", [128, 512], n=2, psum=True)
    pC = Rot(k, "pC", [128, 512], n=2, psum=True)
    sc = k.sb("sc", [128, 128])
    fmv = k.sb("fmv", [128, 3, 8])
    for i, src in enumerate((n2g_d, modv_d[3], modv_d[4])):
        pt, pk = pC.next()
        load_fm(k, fmv[:, i, :], "fmv", src.rearrange("(j p) -> j p", p=128), 8, ident, sc, "sc", pt, pk)
    A2 = k.sb("A2", [128, 8])
    S.op("dve", lambda e: e.scalar_tensor_tensor(out=A2[:], in0=fmv[:, 2, :], scalar=1.0, in1=fmv[:, 0, :], op0=ALU.add, op1=ALU.mult),
         reads=["fmv"], writes=["A2"])
    rw = k.sb("rw", [128, 8, NE]); S.dma("sp", rw[:], rw_d.rearrange("(kc p) n -> p kc n", p=128), writes=["rw"])
    bgu = k.sb("bgu", [128, 16, NE])
    bgu_tm = k.sb("bgu_tm", [NE, 2 * D]); S.dma("sp", bgu_tm[:], bgu_d, writes=["bgu_tm"])
    for j in range(16):
        pt, pk = pC.next()
        S.op("pe", lambda e, pt=pt, j=j: e.transpose(out=pt[:, 0:NE], in_=bgu_tm[:, j * 128:(j + 1) * 128], identity=ident[0:NE, 0:NE]),
             reads=["bgu_tm", "ident"], writes=[pk])
        S.op("dve", lambda e, pt=pt, j=j: e.tensor_copy(out=bgu[:, j, :], in_=pt[:, 0:NE]), reads=[pk], writes=["bgu"])
    bdn = k.sb("bdn", [NE, D]); S.dma("sp", bdn[:], bdn_d, writes=["bdn"])
    wdn = k.sb("wdn", [128, 8, D], BF16)
    S.dma("pool", wdn[:], wout_d.rearrange("(kc p) n -> p kc n", p=128), writes=["wdn"])

    a2T = k.sb("a2T", [128, 8, T], BF16)
    gate = k.sb("gate", [128, NT, NE])
    ssq = k.sb("ssq", [128, NT]); rstd = k.sb("rstd", [128, NT])
    hin = Rot(k, "hin", [128, D], n=2)
    tmpf = Rot(k, "tmpf", [128, D], n=2)
    mixt = Rot(k, "mixt", [128, 8, 128], BF16, n=2)
    a2f = Rot(k, "a2f", [128, 8, 128], n=2)
    junk = k.sb("junk", [128, D], BF16)
    sm = Rot(k, "sm", [128, 4, NE], n=2)
    mixv = mixT_d.rearrange("(kc p) t -> p kc t", p=128)

    for tt in range(NT):
        ht, hk = hin.next(); mt, mk_ = mixt.next(); tf, tk = tmpf.next()
        S.dma("sp", ht[:], h_d[tt * 128:(tt + 1) * 128, :], writes=[hk])
        S.dma("act", mt[:], mixv[:, :, tt * 128:(tt + 1) * 128], writes=[mk_])
        py, pyk = pA.next()
        for cg in range(2):
            for kc in range(8):
                S.op("pe", lambda e, py=py, mt=mt, cg=cg, kc=kc: e.matmul(
                    py[:, cg * 512:(cg + 1) * 512], mt[:, kc, :], wdn[:, kc, cg * 512:(cg + 1) * 512],
                    start=(kc == 0), stop=(kc == 7)), reads=[mk_, "wdn"], writes=[pyk])
        S.op("dve", lambda e, tf=tf, py=py: e.tensor_tensor(out=tf[:], in0=py[:], in1=boutbc[:], op=ALU.add),
             reads=[pyk, "boutbc"], writes=[tk])
        S.op("pool", lambda e, tf=tf: e.tensor_tensor(out=tf[:], in0=tf[:], in1=g2bc[:], op=ALU.mult),
             reads=[tk, "g2bc"], writes=[tk])
        S.op("dve", lambda e, tf=tf, ht=ht: e.tensor_tensor(out=ht[:], in0=ht[:], in1=tf[:], op=ALU.add),
             reads=[tk, hk], writes=[hk])
        S.dma("sp", hmid_d[tt * 128:(tt + 1) * 128, :], ht[:], reads=[hk], writes=[("hmid", tt)])
        S.op("act", lambda e, ht=ht, tt=tt: e.activation(out=junk[:], in_=ht[:], func=AF.Square, accum_out=ssq[:, tt:tt + 1]),
             reads=[hk], writes=["junk", ("ssq", tt)])
        S.op("act", lambda e, tt=tt: e.activation(out=rstd[:, tt:tt + 1], in_=ssq[:, tt:tt + 1], func=AF.Sqrt, scale=1.0 / D, bias=EPS),
             reads=[("ssq", tt)], writes=[("rstd", tt)])
        S.op("dve", lambda e, tt=tt: e.reciprocal(out=rstd[:, tt:tt + 1], in_=rstd[:, tt:tt + 1]),
             reads=[("rstd", tt)], writes=[("rstd", tt)])
        S.op("dve", lambda e, tf=tf, ht=ht, tt=tt: e.tensor_scalar(out=tf[:], in0=ht[:], scalar1=rstd[:, tt:tt + 1], scalar2=None, op0=ALU.mult),
             reads=[hk, ("rstd", tt)], writes=[tk])
        pt, ptk = pA.next()
        for c in range(8):
            S.op("pe", lambda e, pt=pt, tf=tf, c=c: e.transpose(out=pt[:, c * 128:(c + 1) * 128], in_=tf[:, c * 128:(c + 1) * 128], identity=ident[:]),
                 reads=[tk, "ident"], writes=[ptk])
        af, ak = a2f.next()
        for c in range(8):
            S.op("act", lambda e, pt=pt, af=af, c=c: e.activation(out=af[:, c, :], in_=pt[:, c * 128:(c + 1) * 128], func=AF.Identity,
                                                                 scale=A2[:, c:c + 1], bias=fmv[:, 1, c:c + 1]),
                 reads=[ptk, "A2", "fmv"], writes=[ak])
        S.op("pool", lambda e, af=af, tt=tt: e.tensor_copy(out=a2T[:, :, tt * 128:(tt + 1) * 128], in_=af[:]),
             reads=[ak], writes=[("a2T", tt)])
        pl, plk = pC.next()
        for c in range(8):
            S.op("pe", lambda e, pl=pl, af=af, c=c: e.matmul(pl[:, 0:NE], af[:, c, :], rw[:, c, :], start=(c == 0), stop=(c == 7)),
                 reads=[ak, "rw"], writes=[plk])
        s4, sk = sm.next()
        S.op("dve", lambda e, s4=s4, pl=pl: e.tensor_tensor(out=s4[:, 0, :], in0=pl[:, 0:NE], in1=rbbc[:], op=ALU.add),
             reads=[plk, "rbbc"], writes=[sk])
        S.op("dve", lambda e, s4=s4: e.max(out=s4[:, 3, 0:8], in_=s4[:, 0, :]), reads=[sk], writes=[sk])
        S.op("dve", lambda e, s4=s4: e.tensor_scalar(out=s4[:, 1, :], in0=s4[:, 0, :], scalar1=s4[:, 3, 3:4], scalar2=None, op0=ALU.is_ge),
             reads=[sk], writes=[sk])
        S.op("dve", lambda e, s4=s4: e.tensor_scalar(out=s4[:, 3, 8:9], in0=s4[:, 3, 0:1], scalar1=-1.0, scalar2=None, op0=ALU.mult),
             reads=[sk], writes=[sk])
        S.op("act", lambda e, s4=s4: e.activation(out=s4[:, 2, :], in_=s4[:, 0, :], func=AF.Exp, bias=s4[:, 3, 8:9], scale=1.0),
             reads=[sk], writes=[sk])
        S.op("dve", lambda e, s4=s4: e.tensor_tensor(out=s4[:, 2, :], in0=s4[:, 2, :], in1=s4[:, 1, :], op=ALU.mult),
             reads=[sk], writes=[sk])
        S.op("dve", lambda e, s4=s4: e.tensor_reduce(out=s4[:, 3, 9:10], in_=s4[:, 2, :], axis=AX.X, op=ALU.add),
             reads=[sk], writes=[sk])
        S.op("dve", lambda e, s4=s4: e.reciprocal(out=s4[:, 3, 10:11], in_=s4[:, 3, 9:10]), reads=[sk], writes=[sk])
        S.op("dve", lambda e, s4=s4, tt=tt: e.tensor_scalar(out=gate[:, tt, :], in0=s4[:, 2, :], scalar1=s4[:, 3, 10:11], scalar2=None, op0=ALU.mult),
             reads=[sk], writes=[("gate", tt)])

    NTB = TBM // 128
    NB5 = TBM // 512
    acc = k.sb("acc", [128, NTB, D])
    hT = k.sb("hT", [128, 8, TBM], BF16)
    slab = Rot(k, "slab", [128, 8, 256], BF16, n=3)
    gsr = Rot(k, "gs", [128, 512], n=2); usr = Rot(k, "us", [128, 512], n=2); sgr = Rot(k, "sg", [128, 512], n=2)
    gTr = Rot(k, "gT", [NE, 128], n=2)
    wguv = wgu_d.rearrange("e (kc p) n -> e p kc n", p=128)
    wdnv = wdn_d.rearrange("e (kc p) n -> e p kc n", p=128)
    for ps_ in range(T // TBM):
        t0 = ps_ * NTB
        for tl in range(NTB):
            tt = t0 + tl
            pt, pk = pC.next(); gt, gk = gTr.next()
            S.op("pe", lambda e, pt=pt, tt=tt: e.transpose(out=pt[0:NE, 0:128], in_=gate[:, tt, :], identity=ident[:]),
                 reads=[("gate", tt), "ident"], writes=[pk])
            S.op("dve", lambda e, pt=pt, gt=gt: e.tensor_copy(out=gt[:], in_=pt[0:NE, 0:128]), reads=[pk], writes=[gk])
            pa, pak = pA.next()
            for cg in range(2):
                S.op("pe", lambda e, pa=pa, gt=gt, cg=cg: e.matmul(pa[:, cg * 512:(cg + 1) * 512], gt[:], bdn[:, cg * 512:(cg + 1) * 512], start=True, stop=True),
                     reads=[gk, "bdn"], writes=[pak])
            S.op("act", lambda e, pa=pa, tl=tl: e.copy(out=acc[:, tl, :], in_=pa[:]), reads=[pak], writes=[("acc", tl)])
        for ex in range(NEXP):
            S.dma("pool", wdn[:], wdnv[ex], writes=["wdn"])
            for j in range(8):
                sl, slk = slab.next()
                S.dma("pool", sl[:, :, 0:128], wguv[ex][:, :, j * 128:(j + 1) * 128], writes=[slk])
                S.dma("pool", sl[:, :, 128:256], wguv[ex][:, :, D + j * 128:D + (j + 1) * 128], writes=[slk])
                for tb in range(NB5):
                    pg, pgk = pA.next()
                    for half in range(2):
                        for kc in range(8):
                            S.op("pe", lambda e, pg=pg, sl=sl, half=half, kc=kc, tb=tb, t0=t0: e.matmul(
                                pg[:, half * 512:(half + 1) * 512], sl[:, kc, half * 128:(half + 1) * 128],
                                a2T[:, kc, t0 * 128 + tb * 512:t0 * 128 + (tb + 1) * 512], start=(kc == 0), stop=(kc == 7)),
                                reads=[slk] + [("a2T", t0 + tb * 4 + q) for q in range(4)], writes=[pgk])
                    gs, gsk = gsr.next(); us, usk = usr.next(); sg, sgk = sgr.next()
                    S.op("dve", lambda e, gs=gs, pg=pg, j=j, ex=ex: e.tensor_scalar(out=gs[:], in0=pg[:, 0:512], scalar1=bgu[:, j, ex:ex + 1], scalar2=7.0, op0=ALU.add, op1=ALU.min),
                         reads=[pgk, "bgu"], writes=[gsk])
                    S.op("act", lambda e, gs=gs, sg=sg: e.activation(out=sg[:], in_=gs[:], func=AF.Sigmoid, scale=1.702),
                         reads=[gsk], writes=[sgk])
                    S.op("dve", lambda e, us=us, pg=pg, j=j, ex=ex: e.tensor_scalar(out=us[:], in0=pg[:, 512:1024], scalar1=bgu[:, 8 + j, ex:ex + 1], scalar2=7.0, op0=ALU.add, op1=ALU.min),
                         reads=[pgk, "bgu"], writes=[usk])
                    S.op("dve", lambda e, us=us: e.tensor_scalar(out=us[:], in0=us[:], scalar1=-7.0, scalar2=1.0, op0=ALU.max, op1=ALU.add),
                         reads=[usk], writes=[usk])
                    S.op("pool", lambda e, gs=gs, sg=sg: e.tensor_tensor(out=gs[:], in0=gs[:], in1=sg[:], op=ALU.mult),
                         reads=[gsk, sgk], writes=[gsk])
                    S.op("pool", lambda e, gs=gs, us=us, j=j, tb=tb: e.tensor_tensor(out=hT[:, j, tb * 512:(tb + 1) * 512], in0=gs[:], in1=us[:], op=ALU.mult),
                         reads=[gsk, usk], writes=[("hT", j, tb)])
            for tl in range(NTB):
                for cg in range(2):
                    pd, pdk = pB.next()
                    for c in range(8):
                        S.op("pe", lambda e, pd=pd, tl=tl, c=c, cg=cg: e.matmul(pd[:], hT[:, c, tl * 128:(tl + 1) * 128], wdn[:, c, cg * 512:(cg + 1) * 512],
                                                                               start=(c == 0), stop=(c == 7)),
                             reads=[("hT", c, tl // 4), "wdn"], writes=[pdk])
                    S.op("dve", lambda e, pd=pd, tl=tl, cg=cg, ex=ex, t0=t0: e.scalar_tensor_tensor(
                        out=acc[:, tl, cg * 512:(cg + 1) * 512], in0=pd[:], scalar=gate[:, t0 + tl, ex:ex + 1],
                        in1=acc[:, tl, cg * 512:(cg + 1) * 512], op0=ALU.mult, op1=ALU.add),
                        reads=[pdk, ("gate", t0 + tl), ("acc", tl)], writes=[("acc", tl)])
        for tl in range(NTB):
            tt = t0 + tl
            ht, hk = hin.next()
            S.dma("sp", ht[:], hmid_d[tt * 128:(tt + 1) * 128, :], reads=[("hmid", tt)], writes=[hk])
            S.op("pool", lambda e, tl=tl: e.tensor_tensor(out=acc[:, tl, :], in0=acc[:, tl, :], in1=g5bc[:], op=ALU.mult),
                 reads=[("acc", tl), "g5bc"], writes=[("acc", tl)])
            S.op("dve", lambda e, ht=ht, tl=tl: e.tensor_tensor(out=ht[:], in0=ht[:], in1=acc[:, tl, :], op=ALU.add),
                 reads=[hk, ("acc", tl)], writes=[hk])
            if final:
                S.op("act", lambda e, ht=ht, tt=tt: e.activation(out=junk[:], in_=ht[:], func=AF.Square, accum_out=ssq[:, tt:tt + 1]),
                     reads=[hk], writes=["junk", ("ssq", tt)])
                S.op("act", lambda e, tt=tt: e.activation(out=rstd[:, tt:tt + 1], in_=ssq[:, tt:tt + 1], func=AF.Sqrt, scale=1.0 / D, bias=EPS),
                     reads=[("ssq", tt)], writes=[("rstd", tt)])
                S.op("dve", lambda e, tt=tt: e.reciprocal(out=rstd[:, tt:tt + 1], in_=rstd[:, tt:tt + 1]),
                     reads=[("rstd", tt)], writes=[("rstd", tt)])
                S.op("dve", lambda e, ht=ht, tt=tt: e.scalar_tensor_tensor(out=ht[:], in0=ht[:], scalar=rstd[:, tt:tt + 1], in1=fgbc[:], op0=ALU.mult, op1=ALU.mult),
                     reads=[hk, ("rstd", tt), "fgbc"], writes=[hk])
            S.dma("sp", out_d[tt * 128:(tt + 1) * 128, :], ht[:], reads=[hk], writes=[("out", tt)])
    S.finish([])
    es.close()
    return nc


def build_ada(nc, NL=2, W=768):
    es = ExitStack(); k = K(nc, es); S = k.S
    c_d = k.inp("c", [1, D]); cc_d = k.inp("c_ctx", [D]); aw_d = k.inp("ada_w", [NL, D, W]); ab_d = k.inp("ada_b", [NL, W])
    ident_d = k.inp("ident", [128, 128]); out_d = k.outp("modv", [NL, 2, W])
    ident = k.sb("ident", [128, 128]); S.dma("sp", ident[:], ident_d, writes=["ident"])
    sc = k.sb("sc", [16, 256])
    S.dma("sp", sc[0:8, 0:128], c_d.rearrange("o (k p) -> (o k) p", p=128), writes=["sc"])
    S.dma("sp", sc[8:16, 0:128], cc_d.rearrange("(k p) -> k p", p=128), writes=["sc"])
    S.op("act", lambda e: e.activation(out=sc[0:16, 128:256], in_=sc[0:16, 0:128], func=AF.Silu), reads=["sc"], writes=["sc"])
    pst = k.ps("pst", [128, 512])
    S.op("pe", lambda e: e.transpose(out=pst[:, 0:16], in_=sc[0:16, 128:256], identity=ident[0:16, 0:16]), reads=["sc", "ident"], writes=["pst"])
    cfm = k.sb("cfm", [128, 16])
    S.op("dve", lambda e: e.tensor_copy(out=cfm[:], in_=pst[:, 0:16]), reads=["pst"], writes=["cfm"])
    pm = Rot(k, "pm", [128, 512], n=2, psum=True)
    for l in range(NL):
        w = k.sb("w%d" % l, [128, 8, W]); S.dma("sp", w[:], aw_d[l].rearrange("(kc p) n -> p kc n", p=128), writes=["w%d" % l])
        bb = k.sb("bb%d" % l, [2, W]); S.dma("act", bb[:], ab_d[l].rearrange("(o n) -> o n", o=1).broadcast_to([2, W]), writes=["bb%d" % l])
        res = k.sb("res%d" % l, [2, W])
        for c0 in range(0, W, 512):
            cw = min(512, W - c0)
            p, pk = pm.next()
            for kc in range(8):
                S.op("pe", lambda e, p=p, w=w, kc=kc, c0=c0, cw=cw: e.matmul(p[0:2, 0:cw], cfm[:, kc:kc + 9:8], w[:, kc, c0:c0 + cw], start=(kc == 0), stop=(kc == 7)),
                     reads=["cfm", "w%d" % l], writes=[pk])
            S.op("dve", lambda e, p=p, res=res, bb=bb, c0=c0, cw=cw: e.tensor_tensor(out=res[:, c0:c0 + cw], in0=p[0:2, 0:cw], in1=bb[:, c0:c0 + cw], op=ALU.add),
                 reads=[pk, "bb%d" % l], writes=["res%d" % l])
        S.dma("sp", out_d[l], res[:], reads=["res%d" % l], writes=[("o", l)])
    S.finish([]); es.close()
    return nc


def norm_mod_T(k, x_src, ntiles, A, B, aT_fn, pA, ident, tag="n"):
    S = k.S
    xin = Rot(k, tag + "xin", [128, D], n=2)
    junk = k.sb(tag + "junk", [128, D], BF16)
    ssq = k.sb(tag + "ssq", [128, ntiles]); rstd = k.sb(tag + "rstd", [128, ntiles])
    for i in range(ntiles):
        xt, xk = xin.next()
        S.dma("sp" if i % 2 == 0 else "act", xt[:], x_src(i), writes=[xk])
        S.op("act", lambda e, xt=xt, i=i: e.activation(out=junk[:], in_=xt[:], func=AF.Square, accum_out=ssq[:, i:i + 1]),
             reads=[xk], writes=[tag + "junk", (tag + "ssq", i)])
        S.op("act", lambda e, i=i: e.activation(out=rstd[:, i:i + 1], in_=ssq[:, i:i + 1], func=AF.Sqrt, scale=1.0 / D, bias=EPS),
             reads=[(tag + "ssq", i)], writes=[(tag + "rstd", i)])
        S.op("dve", lambda e, i=i: e.reciprocal(out=rstd[:, i:i + 1], in_=rstd[:, i:i + 1]), reads=[(tag + "rstd", i)], writes=[(tag + "rstd", i)])
        S.op("dve", lambda e, xt=xt, i=i: e.tensor_scalar(out=xt[:], in0=xt[:], scalar1=rstd[:, i:i + 1], scalar2=None, op0=ALU.mult),
             reads=[xk, (tag + "rstd", i)], writes=[xk])
        pt, ptk = pA.next()
        for c in range(8):
            S.op("pe", lambda e, pt=pt, xt=xt, c=c: e.transpose(out=pt[:, c * 128:(c + 1) * 128], in_=xt[:, c * 128:(c + 1) * 128], identity=ident[:]),
                 reads=[xk, "ident"], writes=[ptk])
        dst, dk = aT_fn(i)
        sA, sB, skeys = A(i), B(i), ["A1", "B1"]
        for c in range(8):
            S.op("act", lambda e, pt=pt, dst=dst, c=c, sA=sA, sB=sB: e.activation(out=dst[:, c, :], in_=pt[:, c * 128:(c + 1) * 128], func=AF.Identity,
                                                                               scale=sA[:, c:c + 1], bias=sB[:, c:c + 1]),
                 reads=[ptk] + skeys, writes=[dk])


def load_AB(k, g_d, shift_rows, scale_rows, ident, pC, nvar):
    S = k.S
    sc = k.sb("ABsc", [128, 128])
    fm = k.sb("ABfm", [128, 1 + 2 * nvar, 8])
    srcs = [g_d] + list(shift_rows) + list(scale_rows)
    for i, src in enumerate(srcs):
        pt, pk = pC.next()
        load_fm(k, fm[:, i, :], "B1", src.rearrange("(j p) -> j p", p=128), 8, ident, sc, "ABsc", pt, pk)
    A = k.sb("ABA", [128, nvar, 8])
    for v in range(nvar):
        S.op("dve", lambda e, v=v: e.scalar_tensor_tensor(out=A[:, v, :], in0=fm[:, 1 + nvar + v, :], scalar=1.0, in1=fm[:, 0, :], op0=ALU.add, op1=ALU.mult),
             reads=["B1"], writes=["A1"])
    return A, fm


def bcast_ap(ap, n):
    a = [list(x) for x in ap.ap]
    return bass.AP(ap.tensor, ap.offset, [a[0], [0, n]] + a[1:])


def build_proj0(nc, TL=2048, TC=256):
    NTL, NTC = TL // 128, TC // 128
    NT = NTL + NTC
    E_IN = 2576
    es = ExitStack(); k = K(nc, es); S = k.S
    x_d = k.inp("x", [TL, D]); ctx_d = k.inp("ctx", [TC, D]); modv_d = k.inp("modv", [2, 6, D]); n1g_d = k.inp("norm1_g", [D])
    win_d = k.inp("w_in", [D, E_IN]); bin_d = k.inp("b_in", [E_IN]); qg_d = k.inp("q_gain", [128]); kg_d = k.inp("k_gain", [128])
    cos_d = k.inp("cos", [TL, 64]); sin_d = k.inp("sin", [TL, 64]); ident_d = k.inp("ident", [128, 128])
    QT_d = k.outp("QT", [128, 4, TL], BF16); KT_d = k.outp("KT", [128, 2, TL + TC], BF16)
    V_d = k.outp("V", [TL + TC, 256], BF16); PM_d = k.outp("PM", [TL + TC, 1552])
    ident = k.sb("ident", [128, 128]); S.dma("sp", ident[:], ident_d, writes=["ident"])
    pA = Rot(k, "pA", [128, 1024], n=2, psum=True); pB = Rot(k, "pB", [128, 512], n=2, psum=True); pC = Rot(k, "pC", [128, 512], n=2, psum=True)
    A, fm = load_AB(k, n1g_d, [modv_d[0, 0], modv_d[1, 0]], [modv_d[0, 1], modv_d[1, 1]], ident, pC, 2)
    win = k.sb("win", [128, 8, E_IN], BF16)
    S.dma("pool", win[:], win_d.rearrange("(kc p) n -> p kc n", p=128), writes=["win"])
    binbc = k.sb("binbc", [128, E_IN]); bc_load(k, binbc[:], "binbc", bin_d, E_IN)
    gbc = k.sb("gbc", [128, 2, 128]); bc_load(k, gbc[:, 0, :], "gbc", qg_d, 128); bc_load(k, gbc[:, 1, :], "gbc", kg_d, 128)
    aTr = Rot(k, "aT", [128, 8, 128], BF16, n=2)
    aT_cur = {}

    def aT_fn(i):
        t, kk = aTr.next(); aT_cur[i] = (t, kk); return t, kk
    x_src = lambda i: (x_d[i * 128:(i + 1) * 128, :] if i < NTL else ctx_d[(i - NTL) * 128:(i - NTL + 1) * 128, :])
    var = lambda i: 0 if i < NTL else 1
    pr = Rot(k, "p", [128, E_IN], n=2)
    sqr = k.sb("sq", [128, 768]); hs = Rot(k, "hs", [128, 8], n=2)
    cs = Rot(k, "cs", [128, 2, 64], n=2)
    rt = Rot(k, "rt", [128, 4, 6, 64], n=1)
    qkT = Rot(k, "qkT", [128, 6, 128], BF16, n=2); vb = Rot(k, "vb", [128, 256], BF16, n=2)
    groups = [(0, 512), (512, 512), (1024, 512), (1536, 512), (2048, 512), (2560, 16)]
    xin = Rot(k, "xin", [128, D], n=2); junk = k.sb("junk", [128, D], BF16)
    ssq = k.sb("ssq", [128, NT]); rstd = k.sb("rstd", [128, NT])
    for i in range(NT):
        lat = i < NTL
        tok0 = i * 128 if lat else TL + (i - NTL) * 128
        xt, xk = xin.next()
        S.dma("sp", xt[:], x_src(i), writes=[xk])
        S.op("act", lambda e, xt=xt, i=i: e.activation(out=junk[:], in_=xt[:], func=AF.Square, accum_out=ssq[:, i:i + 1]), reads=[xk], writes=["junk", ("ssq", i)])
        S.op("act", lambda e, i=i: e.activation(out=rstd[:, i:i + 1], in_=ssq[:, i:i + 1], func=AF.Sqrt, scale=1.0 / D, bias=EPS), reads=[("ssq", i)], writes=[("rstd", i)])
        S.op("dve", lambda e, i=i: e.reciprocal(out=rstd[:, i:i + 1], in_=rstd[:, i:i + 1]), reads=[("rstd", i)], writes=[("rstd", i)])
        S.op("dve", lambda e, xt=xt, i=i: e.tensor_scalar(out=xt[:], in0=xt[:], scalar1=rstd[:, i:i + 1], scalar2=None, op0=ALU.mult), reads=[xk, ("rstd", i)], writes=[xk])
        pt, ptk = pA.next()
        for c in range(8):
            S.op("pe", lambda e, pt=pt, xt=xt, c=c: e.transpose(out=pt[:, c * 128:(c + 1) * 128], in_=xt[:, c * 128:(c + 1) * 128], identity=ident[:]), reads=[xk, "ident"], writes=[ptk])
        at, ak = aTr.next()
        v = var(i)
        for c in range(8):
            S.op("act", lambda e, pt=pt, at=at, c=c, v=v: e.activation(out=at[:, c, :], in_=pt[:, c * 128:(c + 1) * 128], func=AF.Identity,
                                                                    scale=A[:, v, c:c + 1], bias=fm[:, 1 + v, c:c + 1]), reads=[ptk, "A1", "B1"], writes=[ak])
        p, pk = pr.next()
        for (c0, cw) in groups:
            pg, pgk = pB.next()
            for kc in range(8):
                S.op("pe", lambda e, pg=pg, at=at, kc=kc, c0=c0, cw=cw: e.matmul(pg[:, 0:cw], at[:, kc, :], win[:, kc, c0:c0 + cw], start=(kc == 0), stop=(kc == 7)),
                     reads=[ak, "win"], writes=[pgk])
            S.op("dve", lambda e, p=p, pg=pg, c0=c0, cw=cw: e.tensor_tensor(out=p[:, c0:c0 + cw], in0=pg[:, 0:cw], in1=binbc[:, c0:c0 + cw], op=ALU.add),
                 reads=[pgk, "binbc"], writes=[pk])
        S.op("pool", lambda e, p=p: e.tensor_tensor(out=sqr[:], in0=p[:, 0:768], in1=p[:, 0:768], op=ALU.mult), reads=[pk], writes=["sq"])
        h8, hk = hs.next()
        S.op("dve", lambda e, h8=h8: e.tensor_reduce(out=h8[:, 0:6], in_=sqr[:].rearrange("p (h d) -> p h d", h=6), axis=AX.X, op=ALU.add), reads=["sq"], writes=[hk])
        S.op("act", lambda e, h8=h8: e.activation(out=h8[:, 0:6], in_=h8[:, 0:6], func=AF.Sqrt, scale=1.0 / 128, bias=EPS), reads=[hk], writes=[hk])
        S.op("dve", lambda e, h8=h8: e.reciprocal(out=h8[:, 0:6], in_=h8[:, 0:6]), reads=[hk], writes=[hk])
        for h in range(6):
            S.op("dve", lambda e, p=p, h=h, h8=h8: e.scalar_tensor_tensor(out=p[:, h * 128:(h + 1) * 128], in0=p[:, h * 128:(h + 1) * 128], scalar=h8[:, h:h + 1],
                                                                          in1=gbc[:, 0 if h < 4 else 1, :], op0=ALU.mult, op1=ALU.mult), reads=[pk, hk, "gbc"], writes=[pk])
        if lat:
            ct, ck = cs.next()
            S.dma("act", ct[:, 0, :], cos_d[tok0:tok0 + 128, :], writes=[ck]); S.dma("act", ct[:, 1, :], sin_d[tok0:tok0 + 128, :], writes=[ck])
            r, rk = rt.next()
            x5 = p[:, 0:768].rearrange("p (h a t d) -> p h a t d", h=6, a=2, t=2)
            x1, x2 = x5[:, :, :, 0, :], x5[:, :, :, 1, :]
            cosb = bcast_ap(ct[:, 0, :].rearrange("p (a d) -> p a d", a=2), 6); sinb = bcast_ap(ct[:, 1, :].rearrange("p (a d) -> p a d", a=2), 6)
            r4 = [r[:, q, :, :].rearrange("p h (a d) -> p h a d", a=2) for q in range(4)]
            S.op("dve", lambda e, r4=r4, x1=x1, cosb=cosb: e.tensor_tensor(out=r4[0], in0=x1, in1=cosb, op=ALU.mult), reads=[pk, ck], writes=[rk])
            S.op("pool", lambda e, r4=r4, x2=x2, sinb=sinb: e.tensor_tensor(out=r4[1], in0=x2, in1=sinb, op=ALU.mult), reads=[pk, ck], writes=[rk])
            S.op("dve", lambda e, r4=r4, x2=x2, cosb=cosb: e.tensor_tensor(out=r4[2], in0=x2, in1=cosb, op=ALU.mult), reads=[pk, ck], writes=[rk])
            S.op("pool", lambda e, r4=r4, x1=x1, sinb=sinb: e.tensor_tensor(out=r4[3], in0=x1, in1=sinb, op=ALU.mult), reads=[pk, ck], writes=[rk])
            S.op("dve", lambda e, r4=r4, x1=x1: e.tensor_tensor(out=x1, in0=r4[0], in1=r4[1], op=ALU.subtract), reads=[rk], writes=[pk])
            S.op("pool", lambda e, r4=r4, x2=x2: e.tensor_tensor(out=x2, in0=r4[2], in1=r4[3], op=ALU.add), reads=[rk], writes=[pk])
        pq, pqk = pA.next()
        for h in range(6):
            S.op("pe", lambda e, pq=pq, p=p, h=h: e.transpose(out=pq[:, h * 128:(h + 1) * 128], in_=p[:, h * 128:(h + 1) * 128], identity=ident[:]), reads=[pk, "ident"], writes=[pqk])
        qt, qk_ = qkT.next()
        S.op("act", lambda e, qt=qt, pq=pq: e.copy(out=qt[:].rearrange("p h d -> p (h d)"), in_=pq[:, 0:768]), reads=[pqk], writes=[qk_])
        if lat:
            S.dma("sp", QT_d[:, :, tok0:tok0 + 128], qt[:, 0:4, :], reads=[qk_], writes=[("QT", i)])
        S.dma("sp", KT_d[:, :, tok0:tok0 + 128], qt[:, 4:6, :], reads=[qk_], writes=[("KT", i)])
        vt, vk = vb.next()
        S.op("act", lambda e, vt=vt, p=p: e.copy(out=vt[:], in_=p[:, 768:1024]), reads=[pk], writes=[vk])
        S.dma("sp", V_d[tok0:tok0 + 128, :], vt[:], reads=[vk], writes=[("V", i)])
        S.dma("sp", PM_d[tok0:tok0 + 128, :], p[:, 1024:E_IN], reads=[pk], writes=[("PM", i)])
    S.finish([]); es.close()
    return nc


def build_attn(nc, TQ=2048, NK=16640):
    NKT = NK // 128
    es = ExitStack(); k = K(nc, es); S = k.S
    QT_d = k.inp("QT", [128, 4, TQ], BF16); KT_d = k.inp("KT", [128, 2, NK], BF16); V_d = k.inp("V", [NK, 256], BF16)
    hf_d = k.inp("hf", [TQ, 512]); hb_d = k.inp("hb", [TQ, 512]); og_d = k.inp("og", [TQ, 512]); hg_d = k.inp("h_gain", [512])
    ident_d = k.inp("ident", [128, 128])
    mix_d = k.outp("mixT", [D, TQ], BF16)
    ident = k.sb("ident", [128, 128]); S.dma("sp", ident[:], ident_d, writes=["ident"])
    KT = k.sb("KT", [128, 2, NK], BF16); V = k.sb("V", [128, NKT, 256], BF16); QT = k.sb("QT", [128, 4, TQ], BF16)
    S.dma("sp", QT[:], QT_d, writes=["QT"])
    NCH = 13 if NKT % 13 == 0 else 1
    step = NKT // NCH
    Vv = V_d.rearrange("(t p) c -> p t c", p=128)
    for ci in range(NCH):
        S.dma("sp", KT[:, :, ci * step * 128:(ci + 1) * step * 128], KT_d[:, :, ci * step * 128:(ci + 1) * step * 128], writes=[("KT", ci)])
        S.dma("act", V[:, ci * step:(ci + 1) * step, :], Vv[:, ci * step:(ci + 1) * step, :], writes=[("V", ci)])
    ones = k.sb("ones", [128, 128], BF16); S.op("dve", lambda e: e.memset(ones[:], 1.0), writes=["ones"])
    pST = Rot(k, "pST", [128, 512], n=2, psum=True); pOT = Rot(k, "pOT", [128, 512], n=2, psum=True)
    pDN = Rot(k, "pDN", [128, 512], n=2, psum=True); pM = Rot(k, "pM", [128, 512], n=2, psum=True)
    PT = Rot(k, "PT", [128, 512], BF16, n=3)
    rd = Rot(k, "rd", [128, 512], n=2); ob = Rot(k, "ob", [128, 512], BF16, n=2)
    sc = 128.0 ** -0.5
    for h in range(4):
        g = h // 2
        for qb in range(TQ // 512):
            ot, otk = pOT.next(); dn, dnk = pDN.next()
            for kt in range(NKT):
                st, stk = pST.next(); pt, ptk = PT.next()
                S.op("pe", lambda e, st=st, g=g, kt=kt, h=h, qb=qb: e.matmul(st[:], KT[:, g, kt * 128:(kt + 1) * 128], QT[:, h, qb * 512:(qb + 1) * 512], start=True, stop=True),
                     reads=[("KT", kt // step), "QT"], writes=[stk])
                S.op("act", lambda e, st=st, pt=pt: e.activation(out=pt[:], in_=st[:], func=AF.Exp, scale=sc), reads=[stk], writes=[ptk])
                S.op("pe", lambda e, ot=ot, pt=pt, g=g, kt=kt: e.matmul(ot[:], V[:, kt, g * 128:(g + 1) * 128], pt[:], start=(kt == 0), stop=(kt == NKT - 1)),
                     reads=[("V", kt // step), ptk], writes=[otk])
                S.op("pe", lambda e, dn=dn, pt=pt, kt=kt: e.matmul(dn[:], ones[:], pt[:], start=(kt == 0), stop=(kt == NKT - 1)),
                     reads=["ones", ptk], writes=[dnk])
            r, rk = rd.next(); o, ok = ob.next()
            S.op("dve", lambda e, r=r, dn=dn: e.reciprocal(out=r[:], in_=dn[:]), reads=[dnk], writes=[rk])
            S.op("dve", lambda e, o=o, ot=ot, r=r: e.tensor_tensor(out=o[:], in0=ot[:], in1=r[:], op=ALU.mult), reads=[otk, rk], writes=[ok])
            S.dma("sp", mix_d[h * 128:(h + 1) * 128, qb * 512:(qb + 1) * 512], o[:], reads=[ok], writes=[("mix", h, qb)])
    hgbc = k.sb("hgbc", [128, 512]); bc_load(k, hgbc[:], "hgbc", hg_d, 512)
    hfr = Rot(k, "hfr", [128, 512], n=2); hbr = Rot(k, "hbr", [128, 512], n=2); ogr = Rot(k, "ogr", [128, 512], n=2)
    sq = k.sb("sq", [128, 512]); h4 = Rot(k, "h4", [128, 4], n=2); mt = Rot(k, "mt", [128, 4, 128], BF16, n=2)
    for i in range(TQ // 128):
        a, ak = hfr.next(); b, bk = hbr.next(); o, ok = ogr.next()
        S.dma("sp", a[:], hf_d[i * 128:(i + 1) * 128, :], writes=[ak]); S.dma("act", b[:], hb_d[i * 128:(i + 1) * 128, :], writes=[bk])
        S.dma("sp", o[:], og_d[i * 128:(i + 1) * 128, :], writes=[ok])
        S.op("dve", lambda e, a=a, b=b: e.tensor_tensor(out=a[:], in0=a[:], in1=b[:], op=ALU.add), reads=[ak, bk], writes=[ak])
        S.op("pool", lambda e, a=a: e.tensor_tensor(out=sq[:], in0=a[:], in1=a[:], op=ALU.mult), reads=[ak], writes=["sq"])
        hh, hk = h4.next()
        S.op("dve", lambda e, hh=hh: e.tensor_reduce(out=hh[:], in_=sq[:].rearrange("p (h d) -> p h d", h=4), axis=AX.X, op=ALU.add), reads=["sq"], writes=[hk])
        S.op("act", lambda e, hh=hh: e.activation(out=hh[:], in_=hh[:], func=AF.Sqrt, scale=1.0 / 128, bias=EPS), reads=[hk], writes=[hk])
        S.op("dve", lambda e, hh=hh: e.reciprocal(out=hh[:], in_=hh[:]), reads=[hk], writes=[hk])
        S.op("act", lambda e, o=o: e.activation(out=o[:], in_=o[:], func=AF.Sigmoid), reads=[ok], writes=[ok])
        for h in range(4):
            S.op("dve", lambda e, a=a, hh=hh, h=h: e.scalar_tensor_tensor(out=a[:, h * 128:(h + 1) * 128], in0=a[:, h * 128:(h + 1) * 128], scalar=hh[:, h:h + 1],
                                                                          in1=hgbc[:, h * 128:(h + 1) * 128], op0=ALU.mult, op1=ALU.mult), reads=[ak, hk, "hgbc"], writes=[ak])
        S.op("pool", lambda e, a=a, o=o: e.tensor_tensor(out=a[:], in0=a[:], in1=o[:], op=ALU.mult), reads=[ak, ok], writes=[ak])
        pm, pmk = pM.next()
        for h in range(4):
            S.op("pe", lambda e, pm=pm, a=a, h=h: e.transpose(out=pm[:, h * 128:(h + 1) * 128], in_=a[:, h * 128:(h + 1) * 128], identity=ident[:]), reads=[ak, "ident"], writes=[pmk])
        m, mk_ = mt.next()
        S.op("act", lambda e, m=m, pm=pm: e.copy(out=m[:].rearrange("p h d -> p (h d)"), in_=pm[:]), reads=[pmk], writes=[mk_])
        S.dma("sp", mix_d[512:1024, i * 128:(i + 1) * 128].rearrange("(h p) t -> p h t", p=128), m[:], reads=[mk_], writes=[("mixm", i)])
    S.finish([]); es.close()
    return nc


def build_mlstm(nc, NS=16640, NCTX=2):
    NC = NS // 128
    es = ExitStack(); k = K(nc, es); S = k.S
    qT_d = k.inp("qT", [64, NS]); kT_d = k.inp("kT", [64, NS]); k_d = k.inp("k", [NS, 64]); v_d = k.inp("v", [NS, 128])
    ig_d = k.inp("ig", [128, NC]); fg_d = k.inp("fg", [128, NC]); fb_d = k.inp("fb", [128, 1]); tri_d = k.inp("tri", [128, 128]); ident_d = k.inp("ident", [128, 128])
    h_d = k.outp("h", [NS - NCTX * 128, 128])
    ident = k.sb("ident", [128, 128]); S.dma("sp", ident[:], ident_d, writes=["ident"])
    tri = k.sb("tri", [128, 128]); S.dma("sp", tri[:], tri_d, writes=["tri"])
    onesf = k.sb("onesf", [128, 128]); S.op("dve", lambda e: e.memset(onesf[:], 1.0), writes=["onesf"])
    qT = k.sb("qT", [64, NS], BF16); kT = k.sb("kT", [64, NS], BF16)
    S.dma("pool", qT[:], qT_d, writes=["qT"]); S.dma("pool", kT[:], kT_d, writes=["kT"])
    kk = k.sb("k", [128, NC, 64])
    for c0 in range(0, NC, 64):
        c1 = min(NC, c0 + 64)
        S.dma("sp", kk[:, c0:c1, :], k_d[c0 * 128:c1 * 128, :].rearrange("(c p) d -> p c d", p=128), writes=["k"])
    v1 = k.sb("v1", [128, NC, 129], BF16)
    S.op("dve", lambda e: e.memset(v1[:, :, 128:129], 1.0), writes=["v1"])
    for c0 in range(0, NC, 64):
        c1 = min(NC, c0 + 64)
        S.dma("pool", v1[:, c0:c1, 0:128], v_d[c0 * 128:c1 * 128, :].rearrange("(c p) d -> p c d", p=128), writes=["v1"])
    names = ["ig", "fg", "logf", "b", "blast", "LW", "U", "abc", "mnext", "mprev", "cm", "mrow", "w", "decay", "rowf", "colf8", "winter8", "emr", "tmp"]
    A = {n: k.sb("a_" + n, [128, NC]) for n in names}
    fb = k.sb("fb", [128, 2]); S.dma("sp", fb[:, 0:1], fb_d, writes=["fb"])
    S.op("dve", lambda e: e.tensor_scalar(out=fb[:, 1:2], in0=fb[:, 0:1], scalar1=-1.0, scalar2=None, op0=ALU.mult), reads=["fb"], writes=["fb"])
    S.dma("sp", A["ig"][:], ig_d, writes=["ig"]); S.dma("sp", A["fg"][:], fg_d, writes=["fg"])
    pP = Rot(k, "pP", [128, 512], n=8, psum=True)

    def ew(eng, fn, reads, writes):
        S.op(eng, fn, reads=reads, writes=writes)
    ew("act", lambda e: e.activation(out=A["tmp"][:], in_=A["fg"][:], func=AF.Exp, scale=-1.0, bias=fb[:, 1:2]), ["fg", "fb"], ["tmp"])
    ew("act", lambda e: e.activation(out=A["tmp"][:], in_=A["tmp"][:], func=AF.Ln, bias=1.0), ["tmp"], ["tmp"])
    ew("dve", lambda e: e.tensor_scalar(out=A["logf"][:], in0=A["tmp"][:], scalar1=-1.0, scalar2=None, op0=ALU.mult), ["tmp"], ["logf"])
    p1, p1k = pP.next(); p2, p2k = pP.next()
    ew("pe", lambda e: e.matmul(p1[:, 0:NC], tri[:], A["logf"][:], start=True, stop=True), ["tri", "logf"], [p1k])
    ew("pe", lambda e: e.matmul(p2[:, 0:NC], onesf[:], A["logf"][:], start=True, stop=True), ["onesf", "logf"], [p2k])
    ew("dve", lambda e: e.tensor_copy(out=A["b"][:], in_=p1[:, 0:NC]), [p1k], ["b"])
    ew("dve", lambda e: e.tensor_copy(out=A["blast"][:], in_=p2[:, 0:NC]), [p2k], ["blast"])
    ew("dve", lambda e: e.tensor_tensor(out=A["U"][:], in0=A["ig"][:], in1=A["b"][:], op=ALU.subtract), ["ig", "b"], ["U"])
    ew("dve", lambda e: e.tensor_tensor(out=A["LW"][:], in0=A["U"][:], in1=A["blast"][:], op=ALU.add), ["U", "blast"], ["LW"])
    zer = k.sb("zer", [128, 128]); S.op("dve", lambda e: e.memset(zer[:], 0.0), writes=["zer"])
    am = k.sb("am", [128, 1]); lh = k.sb("lh", [128, 128]); ut = k.sb("ut", [128, 128]); cmt = k.sb("cmt", [128, 128])
    for c0 in range(0, NC, 128):
        cw = min(128, NC - c0)
        pt, ptk = pP.next()
        ew("pe", lambda e, pt=pt, c0=c0, cw=cw: e.transpose(out=pt[0:cw, 0:128], in_=A["LW"][:, c0:c0 + cw], identity=ident[:]), ["LW", "ident"], [ptk])
        ew("dve", lambda e, pt=pt, cw=cw: e.tensor_reduce(out=am[0:cw, :], in_=pt[0:cw, 0:128], axis=AX.X, op=ALU.max), [ptk], ["am"])
        ew("dve", lambda e, cw=cw: e.tensor_scalar(out=lh[0:cw, :], in0=onesf[0:cw, :], scalar1=am[0:cw, 0:1], scalar2=None, op0=ALU.mult), ["am", "onesf"], ["lh"])
        pa, pak = pP.next()
        ew("pe", lambda e, pa=pa, cw=cw: e.matmul(pa[:, 0:cw], lh[0:cw, :], ident[0:cw, 0:cw], start=True, stop=True), ["lh", "ident"], [pak])
        ew("dve", lambda e, pa=pa, c0=c0, cw=cw: e.tensor_copy(out=A["abc"][:, c0:c0 + cw], in_=pa[:, 0:cw]), [pak], ["abc"])
        pu, puk = pP.next()
        ew("pe", lambda e, pu=pu, c0=c0, cw=cw: e.transpose(out=pu[0:cw, 0:128], in_=A["U"][:, c0:c0 + cw], identity=ident[:]), ["U", "ident"], [puk])
        ew("dve", lambda e, pu=pu, cw=cw: e.tensor_copy(out=ut[0:cw, :], in_=pu[0:cw, 0:128]), [puk], ["ut"])
        ew("dve", lambda e, cw=cw: e.tensor_tensor_scan(out=cmt[0:cw, :], data0=zer[0:cw, :], data1=ut[0:cw, :], initial=-1e30, op0=ALU.add, op1=ALU.max), ["ut", "zer"], ["cmt"])
        pc, pck = pP.next()
        ew("pe", lambda e, pc=pc, cw=cw: e.transpose(out=pc[:, 0:cw], in_=cmt[0:cw, :], identity=ident[0:cw, 0:cw]), ["cmt", "ident"], [pck])
        ew("dve", lambda e, pc=pc, c0=c0, cw=cw: e.tensor_copy(out=A["cm"][:, c0:c0 + cw], in_=pc[:, 0:cw]), [pck], ["cm"])
    ew("dve", lambda e: e.tensor_tensor_scan(out=A["mnext"][:], data0=A["blast"][:], data1=A["abc"][:], initial=0.0, op0=ALU.add, op1=ALU.max), ["blast", "abc"], ["mnext"])
    ew("dve", lambda e: e.memset(A["mprev"][:, 0:1], 0.0), [], ["mprev"])
    ew("dve", lambda e: e.tensor_copy(out=A["mprev"][:, 1:NC], in_=A["mnext"][:, 0:NC - 1]), ["mnext"], ["mprev"])
    ew("dve", lambda e: e.tensor_tensor(out=A["mrow"][:], in0=A["mprev"][:], in1=A["cm"][:], op=ALU.max), ["mprev", "cm"], ["mrow"])
    ew("dve", lambda e: e.tensor_tensor(out=A["mrow"][:], in0=A["mrow"][:], in1=A["b"][:], op=ALU.add), ["mrow", "b"], ["mrow"])
    ew("dve", lambda e: e.tensor_tensor(out=A["tmp"][:], in0=A["LW"][:], in1=A["mnext"][:], op=ALU.subtract), ["LW", "mnext"], ["tmp"])
    ew("act", lambda e: e.activation(out=A["w"][:], in_=A["tmp"][:], func=AF.Exp), ["tmp"], ["w"])
    ew("dve", lambda e: e.tensor_tensor(out=A["tmp"][:], in0=A["blast"][:], in1=A["mprev"][:], op=ALU.add), ["blast", "mprev", "w"], ["tmp"])
    ew("dve", lambda e: e.tensor_tensor(out=A["tmp"][:], in0=A["tmp"][:], in1=A["mnext"][:], op=ALU.subtract), ["tmp", "mnext"], ["tmp"])
    ew("act", lambda e: e.activation(out=A["decay"][:], in_=A["tmp"][:], func=AF.Exp), ["tmp"], ["decay"])
    ew("dve", lambda e: e.tensor_tensor(out=A["tmp"][:], in0=A["b"][:], in1=A["mrow"][:], op=ALU.subtract), ["b", "mrow", "decay"], ["tmp"])
    ew("act", lambda e: e.activation(out=A["rowf"][:], in_=A["tmp"][:], func=AF.Exp), ["tmp"], ["rowf"])
    ew("dve", lambda e: e.tensor_tensor(out=A["tmp"][:], in0=A["tmp"][:], in1=A["mprev"][:], op=ALU.add), ["tmp", "mprev", "rowf"], ["tmp"])
    ew("dve", lambda e: e.tensor_scalar(out=A["tmp"][:], in0=A["tmp"][:], scalar1=-float(np.log(8.0)), scalar2=None, op0=ALU.add), ["tmp"], ["tmp"])
    ew("act", lambda e: e.activation(out=A["winter8"][:], in_=A["tmp"][:], func=AF.Exp), ["tmp"], ["winter8"])
    ew("dve", lambda e: e.tensor_scalar(out=A["tmp"][:], in0=A["U"][:], scalar1=-float(np.log(8.0)), scalar2=None, op0=ALU.add), ["U", "winter8"], ["tmp"])
    ew("act", lambda e: e.activation(out=A["colf8"][:], in_=A["tmp"][:], func=AF.Exp), ["tmp"], ["colf8"])
    ew("act", lambda e: e.activation(out=A["emr"][:], in_=A["mrow"][:], func=AF.Exp, scale=-1.0), ["mrow"], ["emr"])
    pre = ["w", "decay", "rowf", "colf8", "winter8", "emr"]
    Cx = k.sb("Cx", [64, 129]); S.op("dve", lambda e: e.memset(Cx[:], 0.0), writes=["Cx"])
    Cb = Rot(k, "Cb", [64, 129], BF16, n=2); stm = Rot(k, "stm", [128, 128], BF16, n=2); kw = Rot(k, "kw", [128, 64], BF16, n=2)
    t1 = Rot(k, "t1", [128, 129], n=2); dd = Rot(k, "dd", [128, 2], n=2); ho = Rot(k, "ho", [128, 128], n=2)
    for c in range(NC):
        cs = slice(c * 128, (c + 1) * 128)
        if c >= NCTX:
            ps, psk = pP.next()
            ew("pe", lambda e, ps=ps, cs=cs: e.matmul(ps[:, 0:128], kT[:, cs], qT[:, cs], start=True, stop=True), ["kT", "qT"], [psk])
            sm, smk = stm.next()
            ew("dve", lambda e, sm=sm, ps=ps, c=c: e.scalar_tensor_tensor(out=sm[:], in0=ps[:, 0:128], scalar=A["colf8"][:, c:c + 1], in1=tri[:], op0=ALU.mult, op1=ALU.mult),
               [psk, "colf8", "tri"], [smk])
            pa, pak = pP.next()
            ew("pe", lambda e, pa=pa, sm=sm, c=c: e.matmul(pa[:, 0:129], sm[:], v1[:, c, :], start=True, stop=True), [smk, "v1"], [pak])
            cb, cbk = Cb.next()
            ew("act", lambda e, cb=cb: e.copy(out=cb[:], in_=Cx[:]), ["Cx"], [cbk])
            pb, pbk = pP.next()
            ew("pe", lambda e, pb=pb, cb=cb, cs=cs: e.matmul(pb[:, 0:129], qT[:, cs], cb[:], start=True, stop=True), ["qT", cbk], [pbk])
            tt, ttk = t1.next()
            ew("act", lambda e, tt=tt, pa=pa, c=c: e.activation(out=tt[:], in_=pa[:, 0:129], func=AF.Identity, scale=A["rowf"][:, c:c + 1]), [pak, "rowf"], [ttk])
            ew("dve", lambda e, tt=tt, pb=pb, c=c: e.scalar_tensor_tensor(out=tt[:], in0=pb[:, 0:129], scalar=A["winter8"][:, c:c + 1], in1=tt[:], op0=ALU.mult, op1=ALU.add),
               [pbk, "winter8", ttk], [ttk])
            d2, d2k = dd.next()
            ew("dve", lambda e, d2=d2, tt=tt: e.tensor_scalar(out=d2[:, 1:2], in0=tt[:, 128:129], scalar1=-1.0, scalar2=None, op0=ALU.mult), [ttk], [d2k])
            ew("dve", lambda e, d2=d2, tt=tt: e.tensor_tensor(out=d2[:, 0:1], in0=tt[:, 128:129], in1=d2[:, 1:2], op=ALU.max), [ttk, d2k], [d2k])
            ew("dve", lambda e, d2=d2, c=c: e.tensor_tensor(out=d2[:, 0:1], in0=d2[:, 0:1], in1=A["emr"][:, c:c + 1], op=ALU.max), [d2k, "emr"], [d2k])
            ew("dve", lambda e, d2=d2: e.reciprocal(out=d2[:, 1:2], in_=d2[:, 0:1]), [d2k], [d2k])
            hh, hhk = ho.next()
            ew("dve", lambda e, hh=hh, tt=tt, d2=d2: e.tensor_scalar(out=hh[:], in0=tt[:, 0:128], scalar1=d2[:, 1:2], scalar2=None, op0=ALU.mult), [ttk, d2k], [hhk])
            S.dma("sp", h_d[(c - NCTX) * 128:(c - NCTX + 1) * 128, :], hh[:], reads=[hhk], writes=[("h", c)])
        if c < NC - 1:
            kq, kqk = kw.next()
            ew("pool", lambda e, kq=kq, c=c: e.tensor_scalar(out=kq[:], in0=kk[:, c, :], scalar1=A["w"][:, c:c + 1], scalar2=None, op0=ALU.mult), ["k", "w"], [kqk])
            pu, puk = pP.next()
            ew("pe", lambda e, pu=pu, kq=kq, c=c: e.matmul(pu[0:64, 0:129], kq[:], v1[:, c, :], start=True, stop=True), [kqk, "v1"], [puk])
            ew("dve", lambda e, pu=pu, c=c: e.scalar_tensor_tensor(out=Cx[:], in0=Cx[:], scalar=A["decay"][0:64, c:c + 1], in1=pu[0:64, 0:129], op0=ALU.mult, op1=ALU.add),
               ["Cx", "decay", puk], ["Cx"])
    S.finish([]); es.close()
    return nc


def build_hyproj(nc, T=2048):
    NT = T // 128
    es = ExitStack(); k = K(nc, es); S = k.S
    h_d = k.inp("h", [T, D]); modv_d = k.inp("modv", [6, D]); n1g_d = k.inp("norm1_g", [D])
    win_d = k.inp("w_in", [D, 3 * D]); binfm_d = k.inp("b_in_fm", [128, 24]); ident_d = k.inp("ident", [128, 128])
    z_d = k.outp("z0T", [3 * D, T])
    ident = k.sb("ident", [128, 128]); S.dma("sp", ident[:], ident_d, writes=["ident"])
    pA = Rot(k, "pA", [128, 1024], n=2, psum=True); pB = Rot(k, "pB", [128, 512], n=2, psum=True); pC = Rot(k, "pC", [128, 512], n=2, psum=True)
    A, fm = load_AB(k, n1g_d, [modv_d[0]], [modv_d[1]], ident, pC, 1)
    win = k.sb("win", [128, 8, 3 * D], BF16)
    S.dma("pool", win[:], win_d.rearrange("(kc p) n -> p kc n", p=128), writes=["win"])
    binfm = k.sb("binfm", [128, 24]); S.dma("sp", binfm[:], binfm_d, writes=["binfm"])
    aT = k.sb("aT", [128, 8, T], BF16)
    xin = Rot(k, "xin", [128, D], n=2); junk = k.sb("junk", [128, D], BF16)
    ssq = k.sb("ssq", [128, NT]); rstd = k.sb("rstd", [128, NT])
    for i in range(NT):
        xt, xk = xin.next()
        S.dma("sp", xt[:], h_d[i * 128:(i + 1) * 128, :], writes=[xk])
        S.op("act", lambda e, xt=xt, i=i: e.activation(out=junk[:], in_=xt[:], func=AF.Square, accum_out=ssq[:, i:i + 1]), reads=[xk], writes=["junk", ("ssq", i)])
        S.op("act", lambda e, i=i: e.activation(out=rstd[:, i:i + 1], in_=ssq[:, i:i + 1], func=AF.Sqrt, scale=1.0 / D, bias=EPS), reads=[("ssq", i)], writes=[("rstd", i)])
        S.op("dve", lambda e, i=i: e.reciprocal(out=rstd[:, i:i + 1], in_=rstd[:, i:i + 1]), reads=[("rstd", i)], writes=[("rstd", i)])
        S.op("dve", lambda e, xt=xt, i=i: e.tensor_scalar(out=xt[:], in0=xt[:], scalar1=rstd[:, i:i + 1], scalar2=None, op0=ALU.mult), reads=[xk, ("rstd", i)], writes=[xk])
        pt, ptk = pA.next()
        for c in range(8):
            S.op("pe", lambda e, pt=pt, xt=xt, c=c: e.transpose(out=pt[:, c * 128:(c + 1) * 128], in_=xt[:, c * 128:(c + 1) * 128], identity=ident[:]), reads=[xk, "ident"], writes=[ptk])
        for c in range(8):
            S.op("act", lambda e, pt=pt, c=c, i=i: e.activation(out=aT[:, c, i * 128:(i + 1) * 128], in_=pt[:, c * 128:(c + 1) * 128], func=AF.Identity,
                                                              scale=A[:, 0, c:c + 1], bias=fm[:, 1, c:c + 1]), reads=[ptk, "A1", "B1"], writes=[("aT", i)])
    zo = Rot(k, "zo", [128, 512], n=3)
    for j in range(24):
        for tb in range(T // 512):
            pg, pgk = pB.next()
            for kc in range(8):
                S.op("pe", lambda e, pg=pg, j=j, tb=tb, kc=kc: e.matmul(pg[:], win[:, kc, j * 128:(j + 1) * 128], aT[:, kc, tb * 512:(tb + 1) * 512], start=(kc == 0), stop=(kc == 7)),
                     reads=["win"] + [("aT", tb * 4 + q) for q in range(4)], writes=[pgk])
            z, zk = zo.next()
            S.op("act", lambda e, z=z, pg=pg, j=j: e.activation(out=z[:], in_=pg[:], func=AF.Identity, bias=binfm[:, j:j + 1], scale=1.0), reads=[pgk, "binfm"], writes=[zk])
            S.dma("sp", z_d[j * 128:(j + 1) * 128, tb * 512:(tb + 1) * 512], z[:], reads=[zk], writes=[("z", j, tb)])
    S.finish([]); es.close()
    return nc


TWO_PI = float(2 * np.pi)
MAGIC = 12582912.0


def build_hyconv(nc, L=16384, G=8, upto=4, ngroups=None, limit3=None):
    N2 = 2 * L
    assert L == 16384
    es = ExitStack(); k = K(nc, es); S = k.S
    z0_d = k.inp("z0", [3, 128, L]); cw_d = k.inp("cw", [128, 9]); cb_d = k.inp("cb", [128, 3]); skip_d = k.inp("skip", [128, 1]); ndel_d = k.inp("ndelta", [128, 1])
    w1_d = k.inp("fw1", [33, 64]); w2_d = k.inp("fw2", [64, 64]); w3_d = k.inp("fw3", [64, 64]); w4_d = k.inp("fw4", [64, 256])
    fbf_d = k.inp("fbf", [64, 4])
    zf_d = k.inp("zfeat", [33, N2]); tn_d = k.inp("tnorm", [N2])
    F1_d = k.inp("F1", [128, 2, 512]); TW_d = k.inp("TW", [128, 512]); F2_d = k.inp("F2", [128, 3, 128])
    Gt_d = k.inp("Gt", [128, 2, 256]); ITW_d = k.inp("ITW", [128, 2, 256]); F3_d = k.inp("F3", [128, 2, 2, 128])
    out_d = k.outp("zT", [128, L], BF16)
    vx_d = k.dram("vx_s", [128, L]); x0_d = k.dram("x0_s", [128, L]); taps_d = k.dram("taps_s", [128, N2]); y_d = k.dram("y_s", [128, L])
    pP = Rot(k, "pP", [128, 512], n=8, psum=True)
    ar = Arena(k, 204000)
    cw = k.sb("cw", [128, 9]); S.dma("sp", cw[:], cw_d, writes=["cw"]); cb = k.sb("cb", [128, 3]); S.dma("sp", cb[:], cb_d, writes=["cb"])
    skip = k.sb("skip", [128, 1]); S.dma("sp", skip[:], skip_d, writes=["skip"]); ndel = k.sb("ndel", [128, 1]); S.dma("sp", ndel[:], ndel_d, writes=["ndel"])
    TB = 2048
    m0 = ar.mark()
    zb = RotA(ar, "zb", [128, 3, TB + 2], n=2); yb = RotA(ar, "yb", [128, 3, TB], n=2)
    for b in range(L // TB):
        t0 = b * TB
        z, zk = zb.next(); y, yk = yb.next()
        lo = max(t0 - 1, 0); hi = min(t0 + TB + 1, L)
        if b == 0:
            S.op("dve", lambda e, z=z: e.memset(z[:, :, 0:1], 0.0), writes=[zk])
        if b == L // TB - 1:
            S.op("dve", lambda e, z=z: e.memset(z[:, :, TB + 1:TB + 2], 0.0), writes=[zk])
        S.dma("sp", z[:, :, lo - (t0 - 1):hi - (t0 - 1)], z0_d[:, :, lo:hi].rearrange("g p t -> p g t"), writes=[zk])
        for gi in range(3):
            eng = "dve"
            S.op(eng, lambda e, z=z, y=y, gi=gi: e.tensor_scalar(out=y[:, gi, :], in0=z[:, gi, 1:TB + 1], scalar1=cw[:, gi * 3 + 1:gi * 3 + 2], scalar2=cb[:, gi:gi + 1], op0=ALU.mult, op1=ALU.add),
                 reads=[zk, "cw", "cb"], writes=[(yk, gi)])
            S.op(eng, lambda e, z=z, y=y, gi=gi: e.scalar_tensor_tensor(out=y[:, gi, :], in0=z[:, gi, 0:TB], scalar=cw[:, gi * 3:gi * 3 + 1], in1=y[:, gi, :], op0=ALU.mult, op1=ALU.add),
                 reads=[zk, "cw", (yk, gi)], writes=[(yk, gi)])
            S.op(eng, lambda e, z=z, y=y, gi=gi: e.scalar_tensor_tensor(out=y[:, gi, :], in0=z[:, gi, 2:TB + 2], scalar=cw[:, gi * 3 + 2:gi * 3 + 3], in1=y[:, gi, :], op0=ALU.mult, op1=ALU.add),
                 reads=[zk, "cw", (yk, gi)], writes=[(yk, gi)])
        S.op("dve", lambda e, y=y: e.tensor_tensor(out=y[:, 2, :], in0=y[:, 2, :], in1=y[:, 1, :], op=ALU.mult), reads=[(yk, 1), (yk, 2)], writes=[(yk, 2)])
        S.dma("sp", vx_d[:, t0:t0 + TB], y[:, 2, :], reads=[(yk, 2)], writes=[("vx", b)])
        S.dma("act", x0_d[:, t0:t0 + TB], y[:, 0, :], reads=[(yk, 0)], writes=[("x0", b)])
    if upto < 2:
        S.finish([]); es.close(); return nc
    S.barrier(); ar.release(m0)
    w1 = ar.alloc([33, 64]); S.dma("sp", w1[:], w1_d, writes=["fw"]); w2 = ar.alloc([64, 64]); S.dma("sp", w2[:], w2_d, writes=["fw"])
    w3 = ar.alloc([64, 64]); S.dma("sp", w3[:], w3_d, writes=["fw"]); w4 = ar.alloc([64, 256]); S.dma("sp", w4[:], w4_d, writes=["fw"])
    fbf = ar.alloc([64, 8]); S.dma("sp", fbf[:, 0:4], fbf_d, writes=["fbf"])
    for i in range(3):
        S.op("dve", lambda e, i=i: e.tensor_tensor(out=fbf[:, 4 + i:5 + i], in0=fbf[:, 0:1], in1=fbf[:, 1 + i:2 + i], op=ALU.mult), reads=["fbf"], writes=["fbf"])
    zfr = RotA(ar, "zf", [33, 512], n=2); tnr = RotA(ar, "tn", [128, 512], n=2)
    pre = RotA(ar, "pre", [64, 512], n=2); tq = RotA(ar, "tq", [64, 512], n=2); hh = RotA(ar, "hh", [64, 512], n=2); tp = RotA(ar, "tp", [128, 512], n=2)
    ws = [w1, w2, w3]
    for b in range(N2 // 512):
        zf, zfk = zfr.next(); tn, tnk = tnr.next()
        S.dma("sp", zf[:], zf_d[:, b * 512:(b + 1) * 512], writes=[zfk])
        S.dma("act", tn[:], tn_d[b * 512:(b + 1) * 512].rearrange("(o n) -> o n", o=1).broadcast_to([128, 512]), writes=[tnk])
        cur, curk, kin = zf, zfk, 33
        for li in range(3):
            pp, ppk = pP.next()
            S.op("pe", lambda e, pp=pp, cur=cur, li=li, kin=kin: e.matmul(pp[0:64, :], ws[li][0:kin, :], cur[0:kin, :], start=True, stop=True), reads=["fw", curk], writes=[ppk])
            pr, prk = pre.next(); t, tk = tq.next(); h, hk = hh.next()
            S.op("act", lambda e, pr=pr, pp=pp, li=li: e.activation(out=pr[:], in_=pp[0:64, :], func=AF.Identity, scale=fbf[:, 0:1], bias=fbf[:, 4 + li:5 + li]), reads=[ppk, "fbf"], writes=[prk])
            S.op("dve", lambda e, t=t, pr=pr: e.tensor_scalar(out=t[:], in0=pr[:], scalar1=1.0 / TWO_PI, scalar2=MAGIC, op0=ALU.mult, op1=ALU.add), reads=[prk], writes=[tk])
            S.op("dve", lambda e, t=t: e.tensor_scalar(out=t[:], in0=t[:], scalar1=-MAGIC, scalar2=None, op0=ALU.add), reads=[tk], writes=[tk])
            S.op("dve", lambda e, t=t, pr=pr: e.scalar_tensor_tensor(out=t[:], in0=t[:], scalar=-TWO_PI, in1=pr[:], op0=ALU.mult, op1=ALU.add), reads=[tk, prk], writes=[tk])
            S.op("dve", lambda e, t=t: e.tensor_scalar(out=t[:], in0=t[:], scalar1=3.14159, scalar2=-3.14159, op0=ALU.min, op1=ALU.max), reads=[tk], writes=[tk])
            S.op("act", lambda e, h=h, t=t: e.activation(out=h[:], in_=t[:], func=AF.Sin), reads=[tk], writes=[hk])
            cur, curk, kin = h, hk, 64
        po, pok = pP.next()
        col0 = 0 if b < (L // 512) else 128
        S.op("pe", lambda e, po=po, cur=cur, col0=col0: e.matmul(po[:], w4[:, col0:col0 + 128], cur[:], start=True, stop=True), reads=["fw", curk], writes=[pok])
        S.op("act", lambda e, tn=tn: e.activation(out=tn[:], in_=tn[:], func=AF.Exp, scale=ndel[:, 0:1]), reads=[tnk, "ndel"], writes=[tnk])
        tpp, tpk = tp.next()
        S.op("dve", lambda e, tpp=tpp, po=po, tn=tn: e.tensor_tensor(out=tpp[:], in0=po[:], in1=tn[:], op=ALU.mult), reads=[pok, tnk], writes=[tpk])
        S.dma("sp", taps_d[:, b * 512:(b + 1) * 512], tpp[:], reads=[tpk], writes=[("taps", b)])
    if upto < 3:
        S.finish([]); es.close(); return nc
    S.barrier(); ar.release(m0)
    if limit3 is not None:
        S.nops = 0; S.limit = limit3
    F1 = ar.alloc([128, 2, 512]); S.dma("sp", F1[:], F1_d, writes=["tab"]); TW = ar.alloc([128, 512]); S.dma("sp", TW[:], TW_d, writes=["tab"])
    F2 = ar.alloc([128, 3, 128]); S.dma("sp", F2[:], F2_d, writes=["tab"]); Gt = ar.alloc([128, 2, 256]); S.dma("sp", Gt[:], Gt_d, writes=["tab"])
    ITW = ar.alloc([128, 2, 256]); S.dma("sp", ITW[:], ITW_d, writes=["tab"]); F3 = ar.alloc([128, 2, 2, 128]); S.dma("sp", F3[:], F3_d, writes=["tab"])
    xv = ar.alloc([128, G, 128]); xh = ar.alloc([128, 2, G, 128])
    B = {n: ar.alloc([128, G, 256]) for n in ["yre", "yim", "tre", "tim", "ta", "tb", "vre", "vim", "hre", "him"]}
    zre = ar.alloc([128, 2, G, 128]); zim = ar.alloc([128, 2, G, 128]); zr2 = ar.alloc([128, 2, G, 128]); zi2 = ar.alloc([128, 2, G, 128])
    zta = ar.alloc([128, 2, G, 128]); ztb = ar.alloc([128, 2, G, 128])
    yo = ar.alloc([128, G, 128])
    twc = bcast_ap(TW[:, 0:256], G); tws = bcast_ap(TW[:, 256:512], G)

    def fwd_fft(src_fn, nchunk, ore, oim, tag):
        for c in range(G):
            pp, ppk = pP.next()
            for q in range(nchunk):
                S.op("pe", lambda e, pp=pp, c=c, q=q: e.matmul(pp[:], src_fn(c, q), F1[:, q, :], start=(q == 0), stop=(q == nchunk - 1)), reads=[tag, "tab"], writes=[ppk])
            if c % 2 == 0:
                S.op("act", lambda e, pp=pp, c=c: e.copy(out=B["yre"][:, c, :], in_=pp[:, 0:256]), reads=[ppk], writes=["yre"])
                S.op("act", lambda e, pp=pp, c=c: e.copy(out=B["yim"][:, c, :], in_=pp[:, 256:512]), reads=[ppk], writes=["yim"])
            else:
                S.op("dve", lambda e, pp=pp, c=c: e.tensor_copy(out=B["yre"][:, c, :], in_=pp[:, 0:256]), reads=[ppk], writes=["yre"])
                S.op("dve", lambda e, pp=pp, c=c: e.tensor_copy(out=B["yim"][:, c, :], in_=pp[:, 256:512]), reads=[ppk], writes=["yim"])
        S.op("dve", lambda e: e.tensor_tensor(out=B["ta"][:], in0=B["yre"][:], in1=twc, op=ALU.mult), reads=["yre", "tab"], writes=["ta"])
        S.op("pool", lambda e: e.tensor_tensor(out=B["tb"][:], in0=B["yim"][:], in1=tws, op=ALU.mult), reads=["yim", "tab"], writes=["tb"])
        S.op("dve", lambda e: e.tensor_tensor(out=B["tre"][:], in0=B["ta"][:], in1=B["tb"][:], op=ALU.add), reads=["ta", "tb"], writes=["tre"])
        S.op("dve", lambda e: e.tensor_tensor(out=B["ta"][:], in0=B["yim"][:], in1=twc, op=ALU.mult), reads=["yim", "tab", "tre"], writes=["ta"])
        S.op("pool", lambda e: e.tensor_tensor(out=B["tb"][:], in0=B["yre"][:], in1=tws, op=ALU.mult), reads=["yre", "tab", "tre"], writes=["tb"])
        S.op("dve", lambda e: e.tensor_tensor(out=B["tim"][:], in0=B["ta"][:], in1=B["tb"][:], op=ALU.subtract), reads=["ta", "tb"], writes=["tim"])
        for j in range(G // 2):
            rre = B["tre"][:, 2 * j:2 * j + 2, :].rearrange("p c k -> p (c k)"); rim = B["tim"][:, 2 * j:2 * j + 2, :].rearrange("p c k -> p (c k)")
            p1, p1k = pP.next(); p2, p2k = pP.next()
            S.op("pe", lambda e, p1=p1, rre=rre: e.matmul(p1[:], F2[:, 0, :], rre, start=True, stop=False), reads=["tre", "tab"], writes=[p1k])
            S.op("pe", lambda e, p1=p1, rim=rim: e.matmul(p1[:], F2[:, 1, :], rim, start=False, stop=True), reads=["tim", "tab"], writes=[p1k])
            S.op("pe", lambda e, p2=p2, rim=rim: e.matmul(p2[:], F2[:, 0, :], rim, start=True, stop=False), reads=["tim", "tab"], writes=[p2k])
            S.op("pe", lambda e, p2=p2, rre=rre: e.matmul(p2[:], F2[:, 2, :], rre, start=False, stop=True), reads=["tre", "tab"], writes=[p2k])
            S.op("act", lambda e, p1=p1, j=j: e.copy(out=ore[:, 2 * j:2 * j + 2, :].rearrange("p c k -> p (c k)"), in_=p1[:]), reads=[p1k], writes=[tag + "re"])
            S.op("dve", lambda e, p2=p2, j=j: e.tensor_copy(out=oim[:, 2 * j:2 * j + 2, :].rearrange("p c k -> p (c k)"), in_=p2[:]), reads=[p2k], writes=[tag + "im"])

    for g in range(ngroups if ngroups else 128 // G):
        cs = slice(g * G, (g + 1) * G)
        S.dma("sp", xv[:], vx_d[cs, :].rearrange("c (n2 n1) -> n2 c n1", n1=128), reads=[("vx", b) for b in range(L // TB)], writes=["xv"])
        for q in range(2):
            S.dma("act", xh[:, q], taps_d[cs, q * L:(q + 1) * L].rearrange("c (n2 n1) -> n2 c n1", n1=128), writes=["xh"])
        fwd_fft(lambda c, q: xv[:, c, :], 1, B["vre"], B["vim"], "xv")
        fwd_fft(lambda c, q: xh[:, q, c, :], 2, B["hre"], B["him"], "xh")
        S.op("dve", lambda e: e.tensor_tensor(out=B["ta"][:], in0=B["vre"][:], in1=B["hre"][:], op=ALU.mult), reads=["xvre", "xhre", "tre", "tim"], writes=["ta"])
        S.op("pool", lambda e: e.tensor_tensor(out=B["tb"][:], in0=B["vim"][:], in1=B["him"][:], op=ALU.mult), reads=["xvim", "xhim", "tre", "tim"], writes=["tb"])
        S.op("dve", lambda e: e.tensor_tensor(out=B["tre"][:], in0=B["ta"][:], in1=B["tb"][:], op=ALU.subtract), reads=["ta", "tb"], writes=["tre"])
        S.op("dve", lambda e: e.tensor_tensor(out=B["ta"][:], in0=B["vre"][:], in1=B["him"][:], op=ALU.mult), reads=["xvre", "xhim", "tre"], writes=["ta"])
        S.op("pool", lambda e: e.tensor_tensor(out=B["tb"][:], in0=B["vim"][:], in1=B["hre"][:], op=ALU.mult), reads=["xvim", "xhre", "tre"], writes=["tb"])
        S.op("dve", lambda e: e.tensor_tensor(out=B["tim"][:], in0=B["ta"][:], in1=B["tb"][:], op=ALU.add), reads=["ta", "tb"], writes=["tim"])
        for c in range(G):
            for q in range(2):
                pz, pzk = pP.next()
                S.op("pe", lambda e, pz=pz, c=c, q=q: e.matmul(pz[:, 0:256], B["tre"][:, c, q * 128:(q + 1) * 128], Gt[:, 0, :], start=True, stop=False), reads=["tre", "tab"], writes=[pzk])
                S.op("pe", lambda e, pz=pz, c=c, q=q: e.matmul(pz[:, 0:256], B["tim"][:, c, q * 128:(q + 1) * 128], Gt[:, 1, :], start=False, stop=True), reads=["tim", "tab"], writes=[pzk])
                if q == 0:
                    S.op("act", lambda e, pz=pz, c=c, q=q: e.copy(out=zre[:, q, c, :], in_=pz[:, 0:128]), reads=[pzk], writes=["zre"])
                    S.op("act", lambda e, pz=pz, c=c, q=q: e.copy(out=zim[:, q, c, :], in_=pz[:, 128:256]), reads=[pzk], writes=["zim"])
                else:
                    S.op("dve", lambda e, pz=pz, c=c, q=q: e.tensor_copy(out=zre[:, q, c, :], in_=pz[:, 0:128]), reads=[pzk], writes=["zre"])
                    S.op("dve", lambda e, pz=pz, c=c, q=q: e.tensor_copy(out=zim[:, q, c, :], in_=pz[:, 128:256]), reads=[pzk], writes=["zim"])
        for q in range(2):
            ic = bcast_ap(ITW[:, q, 0:128], G); isn = bcast_ap(ITW[:, q, 128:256], G)
            S.op("dve", lambda e, q=q, ic=ic: e.tensor_tensor(out=zta[:, q], in0=zre[:, q], in1=ic, op=ALU.mult), reads=["zre", "tab"], writes=[("zta", q)])
            S.op("pool", lambda e, q=q, isn=isn: e.tensor_tensor(out=ztb[:, q], in0=zim[:, q], in1=isn, op=ALU.mult), reads=["zim", "tab"], writes=[("ztb", q)])
            S.op("dve", lambda e, q=q: e.tensor_tensor(out=zr2[:, q], in0=zta[:, q], in1=ztb[:, q], op=ALU.subtract), reads=[("zta", q), ("ztb", q)], writes=[("zr2", q)])
            S.op("dve", lambda e, q=q, isn=isn: e.tensor_tensor(out=zta[:, q], in0=zre[:, q], in1=isn, op=ALU.mult), reads=["zre", "tab", ("zr2", q)], writes=[("zta", q)])
            S.op("pool", lambda e, q=q, ic=ic: e.tensor_tensor(out=ztb[:, q], in0=zim[:, q], in1=ic, op=ALU.mult), reads=["zim", "tab", ("zr2", q)], writes=[("ztb", q)])
            S.op("dve", lambda e, q=q: e.tensor_tensor(out=zi2[:, q], in0=zta[:, q], in1=ztb[:, q], op=ALU.add), reads=[("zta", q), ("ztb", q)], writes=[("zi2", q)])
        for j in range(G // 4):
            py, pyk = pP.next()
            n = 0
            for q in range(2):
                for (zz, zk, ri) in ((zr2, "zr2", 0), (zi2, "zi2", 1)):
                    rhs = zz[:, q, 4 * j:4 * j + 4, :].rearrange("p c m -> p (c m)")
                    S.op("pe", lambda e, py=py, q=q, ri=ri, rhs=rhs, n=n: e.matmul(py[:], F3[:, q, ri, :], rhs, start=(n == 0), stop=(n == 3)), reads=[(zk, q), "tab"], writes=[pyk])
                    n += 1
            S.op("act", lambda e, py=py, j=j: e.copy(out=yo[:, 4 * j:4 * j + 4, :].rearrange("p c m -> p (c m)"), in_=py[:]), reads=[pyk], writes=["yo"])
        S.dma("sp", y_d[cs, :].rearrange("c (m2 m1) -> m2 c m1", m1=128), yo[:], reads=["yo"], writes=[("y", g)])
    if upto < 4:
        S.finish([]); es.close(); return nc
    S.barrier(); ar.release(m0)
    fa = RotA(ar, "fa", [128, TB], n=2); fbb = RotA(ar, "fbb", [128, TB], n=2); fc = RotA(ar, "fc", [128, TB], n=2); fo = RotA(ar, "fo", [128, TB], BF16, n=2)
    for b in range(L // TB):
        t0 = b * TB
        a, ak = fa.next(); bb, bk = fbb.next(); c, ck = fc.next(); o, ok = fo.next()
        S.dma("sp", a[:], y_d[:, t0:t0 + TB], writes=[ak]); S.dma("act", bb[:], vx_d[:, t0:t0 + TB], writes=[bk]); S.dma("sp", c[:], x0_d[:, t0:t0 + TB], writes=[ck])
        S.op("dve", lambda e, a=a, bb=bb: e.scalar_tensor_tensor(out=a[:], in0=bb[:], scalar=skip[:, 0:1], in1=a[:], op0=ALU.mult, op1=ALU.add), reads=[ak, bk, "skip"], writes=[ak])
        S.op("pool", lambda e, a=a, c=c, o=o: e.tensor_tensor(out=o[:], in0=a[:], in1=c[:], op=ALU.mult), reads=[ak, ck], writes=[ok])
        S.dma("sp", out_d[:, t0:t0 + TB], o[:], reads=[ok], writes=[("o", b)])
    S.finish([]); es.close()
    return nc


def hy_tables(L=16384):
    N = 2 * L
    f32 = np.float32
    p = np.arange(128)
    n2 = (np.arange(2)[:, None] * 128 + p[None, :]).T
    k2 = np.arange(256)
    ang = 2 * np.pi * (n2[:, :, None] * k2[None, None, :] % 256) / 256
    F1 = np.concatenate([np.cos(ang), -np.sin(ang)], -1).astype(f32)
    th = 2 * np.pi * (p[:, None] * k2[None, :]) / N
    TW = np.concatenate([np.cos(th), np.sin(th)], -1).astype(f32)
    a2 = 2 * np.pi * (p[:, None] * p[None, :] % 128) / 128
    F2 = np.stack([np.cos(a2), np.sin(a2), -np.sin(a2)], 1).astype(f32)
    Gt = np.stack([np.concatenate([np.cos(a2), np.sin(a2)], -1), np.concatenate([-np.sin(a2), np.cos(a2)], -1)], 1).astype(f32)
    ph = 2 * np.pi * (n2[:, :, None] * p[None, None, :]) / N
    ITW = np.concatenate([np.cos(ph), np.sin(ph)], -1).astype(f32)
    a3 = 2 * np.pi * (n2[:, :, None] * p[None, None, :] % 256) / 256
    F3 = np.stack([np.cos(a3) / N, -np.sin(a3) / N], 2).astype(f32)
    lag = np.concatenate([np.arange(L), L - np.arange(L)]).astype(np.int64)
    t = np.linspace(0.0, 1.0, L, dtype=f32)
    w = (2.0 * np.pi / L) * np.arange(L, dtype=f32)
    f = np.linspace(1e-4, 15, 16, dtype=f32)
    feat = np.concatenate([t[:, None], np.cos(f * w[:, None]), -np.sin(f * w[:, None])], -1).astype(f32)
    lagc = np.minimum(lag, L - 1)
    zfeat = np.ascontiguousarray(feat[lagc].T)
    tnorm = t[lagc].copy(); tnorm[L] = 1e6
    return dict(F1=F1, TW=TW, F2=F2, Gt=Gt, ITW=ITW, F3=F3, zfeat=zfeat, tnorm=tnorm.astype(f32))


import math
import ml_dtypes
from concourse.bass_utils import run_bass_kernel_spmd

NCORES = 8
_BF = ml_dtypes.bfloat16


def _run(build, maps, **kw):
    import sys, time
    t0 = time.time()
    nc = bass.Bass("TRN2", target_bir_lowering=False)
    build(nc, **kw)
    res = run_bass_kernel_spmd(nc, maps, core_ids=list(range(len(maps))))
    print("[launch %s] %.1fs" % (build.__name__, time.time() - t0), file=sys.stderr, flush=True)
    return res.results


def _rope_tables(L, W=64):
    row = np.repeat(np.arange(L // W), W); col = np.tile(np.arange(W), L // W)
    pos = np.stack([row, col], -1).astype(np.float32)
    inv = (np.float32(10000.0) ** (-np.arange(32, dtype=np.float32) / np.float32(32))).astype(np.float32)
    ang = pos[:, :, None] * inv
    return np.cos(ang).astype(np.float32).reshape(L, 64), np.sin(ang).astype(np.float32).reshape(L, 64)


def kernel(x, c, ctx, c_ctx, ada_w, ada_b, norm1_g, norm2_g, ev_w_in, ev_b_in, ev_q_gain, ev_k_gain,
           ev_f_bias, ev_h_gain, ev_w_out, ev_b_out, od_w_in, od_b_in, od_conv_w, od_conv_b, od_filt_w1,
           od_filt_b1, od_filt_freq, od_filt_w2, od_filt_b2, od_filt_w3, od_filt_b3, od_filt_w4, od_skip,
           od_w_out, od_b_out, router_w, router_b, moe_w_gu, moe_b_gu, moe_w_down, moe_b_down, final_g):
    f32 = np.float32
    A = lambda a: np.ascontiguousarray(np.asarray(a))
    x = np.asarray(x, f32); ctx = np.asarray(ctx, f32)
    L = x.shape[1]; TQ = L // NCORES; NCT = ctx.shape[1]
    ident = np.eye(128, dtype=f32)
    W = 6 * D // NCORES
    r = _run(build_ada, [dict(c=A(c), c_ctx=A(c_ctx), ada_w=A(ada_w[:, :, i * W:(i + 1) * W]), ada_b=A(ada_b[:, i * W:(i + 1) * W]), ident=ident) for i in range(NCORES)])
    modv = np.concatenate([q["modv"] for q in r], axis=-1)
    cosf, sinf = _rope_tables(L)
    mv0 = A(modv[0].reshape(2, 6, D))
    r = _run(build_proj0, [dict(x=A(x[0, i * TQ:(i + 1) * TQ]), ctx=A(ctx[0]), modv=mv0, norm1_g=A(norm1_g[0]), w_in=A(ev_w_in[0]), b_in=A(ev_b_in[0]),
                                q_gain=A(ev_q_gain[0]), k_gain=A(ev_k_gain[0]), cos=A(cosf[i * TQ:(i + 1) * TQ]), sin=A(sinf[i * TQ:(i + 1) * TQ]), ident=ident)
                           for i in range(NCORES)], TL=TQ, TC=NCT)
    QT = [q["QT"] for q in r]
    KT_all = A(np.concatenate([q["KT"][:, :, :TQ] for q in r] + [r[0]["KT"][:, :, TQ:]], axis=2))
    V_all = A(np.concatenate([q["V"][:TQ] for q in r] + [r[0]["V"][TQ:]], axis=0))
    PM_lat = np.concatenate([q["PM"][:TQ] for q in r], axis=0); PM_ctx = r[0]["PM"][TQ:]
    tri = np.triu(np.ones((128, 128), f32))
    maps = []
    for d in range(2):
        for hh in range(4):
            def seq(cols):
                a, b = PM_ctx[:, cols], PM_lat[:, cols]
                if d == 1:
                    a, b = a[::-1], b[::-1]
                return np.concatenate([a, b], axis=0)
            q_ = seq(slice(hh * 64, (hh + 1) * 64)); k_ = seq(slice(256 + hh * 64, 256 + (hh + 1) * 64)); v_ = seq(slice(512 + hh * 128, 512 + (hh + 1) * 128))
            ig_ = seq(slice(1024 + d * 4 + hh, 1024 + d * 4 + hh + 1))[:, 0]; fg_ = seq(slice(1032 + d * 4 + hh, 1032 + d * 4 + hh + 1))[:, 0]
            NCk = q_.shape[0] // 128
            maps.append(dict(qT=A(q_.T), kT=A(k_.T), k=A(k_), v=A(v_), ig=A(ig_.reshape(NCk, 128).T), fg=A(fg_.reshape(NCk, 128).T),
                             fb=np.full((128, 1), ev_f_bias[0, d, hh], f32), tri=tri, ident=ident))
    r = _run(build_mlstm, maps, NS=L + NCT, NCTX=NCT // 128)
    hf = np.concatenate([r[hh]["h"] for hh in range(4)], axis=1)
    hb = np.concatenate([r[4 + hh]["h"][::-1] for hh in range(4)], axis=1)
    r = _run(build_attn, [dict(QT=QT[i], KT=KT_all, V=V_all, hf=A(hf[i * TQ:(i + 1) * TQ]), hb=A(hb[i * TQ:(i + 1) * TQ]), og=A(PM_lat[i * TQ:(i + 1) * TQ, 1040:1552]),
                               h_gain=A(ev_h_gain[0]), ident=ident) for i in range(NCORES)], TQ=TQ, NK=L + NCT)
    mix0 = [q["mixT"] for q in r]

    NPC = 8
    TP = L // NPC

    def post(layer, hs, mix, w_out, b_out, final):
        hcat = np.concatenate(hs, axis=0); mcat = np.concatenate(mix, axis=1)
        r_ = _run(build_post, [dict(h=A(hcat[i * TP:(i + 1) * TP]), mixT=A(mcat[:, i * TP:(i + 1) * TP]), w_out=A(w_out), b_out=A(b_out), norm2_g=A(norm2_g[layer]),
                                    modv=A(modv[layer, 0].reshape(6, D)), router_w=A(router_w[layer]), router_b=A(router_b[layer]), w_gu=A(moe_w_gu[layer]),
                                    b_gu=A(moe_b_gu[layer]), w_down=A(moe_w_down[layer]), b_down=A(moe_b_down[layer]), final_g=A(final_g), ident=ident)
                               for i in range(NPC)], T=TP, TBM=1024, NEXP=NE, final=final)
        hall = np.concatenate([q["hout"] for q in r_], axis=0)
        return [dict(hout=A(hall[i * TQ:(i + 1) * TQ])) for i in range(NCORES)]
    r = post(0, [A(x[0, i * TQ:(i + 1) * TQ]) for i in range(NCORES)], mix0, ev_w_out[0], ev_b_out[0], False)
    h1 = [q["hout"] for q in r]
    r = _run(build_hyproj, [dict(h=h1[i], modv=A(modv[1, 0].reshape(6, D)), norm1_g=A(norm1_g[1]), w_in=A(od_w_in[0]), b_in_fm=A(od_b_in[0].reshape(24, 128).T), ident=ident)
                            for i in range(NCORES)], T=TQ)
    z0T = np.concatenate([q["z0T"] for q in r], axis=1)
    tabs = hy_tables(L)
    deltas = np.abs(np.linspace(math.log(1e-2) / 1.5, math.log(1e-2) / 0.3, D, dtype=f32))
    maps = []
    for j in range(NCORES):
        ch = slice(j * 128, (j + 1) * 128)
        cw = np.stack([od_conv_w[0][:, g * D + j * 128:g * D + (j + 1) * 128] for g in range(3)], 0)
        maps.append(dict(z0=A(np.stack([z0T[g * D + j * 128:g * D + (j + 1) * 128] for g in range(3)], 0)), cw=A(cw.transpose(2, 0, 1).reshape(128, 9)),
                         cb=A(np.stack([od_conv_b[0][g * D + j * 128:g * D + (j + 1) * 128] for g in range(3)], 1)), skip=A(od_skip[0][ch].reshape(128, 1)),
                         ndelta=A((-deltas[ch]).reshape(128, 1).astype(f32)), fw1=A(od_filt_w1[0]), fw2=A(od_filt_w2[0]), fw3=A(od_filt_w3[0]),
                         fw4=A(np.concatenate([od_filt_w4[0][:, ch], od_filt_w4[0][:, D + j * 128:D + (j + 1) * 128]], 1)),
                         fbf=A(np.stack([od_filt_freq[0], od_filt_b1[0], od_filt_b2[0], od_filt_b3[0]], 1)), **tabs))
    r = _run(build_hyconv, maps, L=L)
    zT = np.concatenate([q["zT"] for q in r], axis=0)
    mix1 = [A(zT[:, i * TQ:(i + 1) * TQ]) for i in range(NCORES)]
    r = post(1, h1, mix1, od_w_out[0], od_b_out[0], True)
    out = np.concatenate([q["hout"] for q in r], axis=0)[None]
    return out.astype(np.float32)
```

```python
import numpy as np
import concourse.bass as bass
import concourse.mybir as mybir

F32 = mybir.dt.float32
BF16 = mybir.dt.bfloat16
I32 = mybir.dt.int32
U32 = mybir.dt.uint32
AF = mybir.ActivationFunctionType
ALU = mybir.AluOpType
AX = mybir.AxisListType


class Sched:
    CE = ("pe", "act", "dve", "pool", "sp")

    def __init__(self, nc, n_dma_sems=8):
        self.nc = nc
        self.ops = {e: [] for e in self.CE}
        self.sem = {}
        self.cnt = {}
        self.seen = {e: {} for e in self.CE}
        self.last_w = {}
        self.readers = {}
        self._stack = []
        for e in self.CE:
            self.sem[e] = nc.alloc_semaphore(name="s_" + e)
            self.cnt[e] = 0
        self.dq = {}
        for q in ("sp", "act", "pool"):
            lst = []
            for i in range(n_dma_sems):
                nm = "d_%s%d" % (q, i)
                self.sem[nm] = nc.alloc_semaphore(name=nm)
                self.cnt[nm] = 0
                lst.append(nm)
            self.dq[q] = [lst, 0]
        self.n_inst = 0
        self.limit = None
        self.nops = 0

    def _deps(self, eng, reads, writes):
        deps = {}
        def add(tok):
            f, v = tok
            if deps.get(f, 0) < v:
                deps[f] = v
        for k in reads:
            t = self.last_w.get(k)
            if t is not None:
                add(t)
        for k in writes:
            t = self.last_w.get(k)
            if t is not None:
                add(t)
            for t in self.readers.get(k, {}).items():
                if t[0] != eng:
                    add(t)
        out = []
        for f, v in deps.items():
            if f == eng and eng == "pe":
                continue
            if self.seen[eng].get(f, 0) >= v:
                continue
            self.seen[eng][f] = v
            out.append((f, v))
        return out

    def _commit(self, tok, reads, writes):
        for k in writes:
            self.last_w[k] = tok
            self.readers[k] = {}
        for k in reads:
            self.readers.setdefault(k, {})[tok[0]] = tok[1]

    def op(self, eng, fn, reads=(), writes=()):
        self.nops += 1
        if self.limit is not None and self.nops > self.limit:
            return None
        waits = self._deps(eng, reads, writes)
        self.cnt[eng] += 1
        tok = (eng, self.cnt[eng])
        sem = self.sem[eng]
        sems = self.sem
        def emit(e, fn=fn, waits=waits, sem=sem):
            for f, v in waits:
                e.wait_ge(sems[f], v)
            fn(e).then_inc(sem, 1)
        self.ops[eng].append(emit)
        self._commit(tok, reads, writes)
        self.n_inst += 1 + len(waits)
        return tok

    def dma(self, q, out, in_, reads=(), writes=(), **kw):
        self.nops += 1
        if self.limit is not None and self.nops > self.limit:
            return None
        lst, idx = self.dq[q]
        nm = lst[idx % len(lst)]
        self.dq[q][1] = idx + 1
        waits = self._deps(q, reads, writes)
        prev = self.cnt[nm]
        if prev > 0 and self.seen[q].get(nm, 0) < prev:
            self.seen[q][nm] = prev
            waits.append((nm, prev))
        self.cnt[nm] += 16
        tok = (nm, self.cnt[nm])
        sem = self.sem[nm]
        sems = self.sem
        def emit(e, waits=waits, sem=sem, out=out, in_=in_, kw=kw):
            for f, v in waits:
                e.wait_ge(sems[f], v)
            e.dma_start(out=out, in_=in_, **kw).then_inc(sem, 16)
        self.ops[q].append(emit)
        self._commit(tok, reads, writes)
        self.n_inst += 1 + len(waits)
        return tok

    def barrier(self):
        sems = self.sem
        for e in self.CE:
            waits = []
            for f, v in self.cnt.items():
                if f == e or v == 0:
                    continue
                if self.seen[e].get(f, 0) >= v:
                    continue
                self.seen[e][f] = v
                waits.append((f, v))
            if waits:
                def emit(eng, waits=waits):
                    for f, v in waits:
                        eng.wait_ge(sems[f], v)
                self.ops[e].append(emit)
                self.n_inst += len(waits)
        self.last_w = {}
        self.readers = {}

    def finish(self, out_keys):
        self.barrier()
        waits = []
        sems = self.sem
        def emit(e, waits=waits):
            for f, v in waits:
                e.wait_ge(sems[f], v)
        self.ops["sp"].append(emit)
        nc = self.nc
        ops = self.ops
        with nc.Block() as block:
            @block.tensor
            def _(e):
                for f in ops["pe"]:
                    f(e)
            @block.scalar
            def _(e):
                for f in ops["act"]:
                    f(e)
            @block.vector
            def _(e):
                for f in ops["dve"]:
                    f(e)
            @block.gpsimd
            def _(e):
                for f in ops["pool"]:
                    f(e)
            @block.sync
            def _(e):
                for f in ops["sp"]:
                    f(e)


from contextlib import ExitStack

D = 1024
NE = 32
EPS = 1e-6


class K:
    def __init__(self, nc, es):
        self.nc = nc
        self.es = es
        self.S = Sched(nc)
        self.nm = 0

    def sb(self, name, shape, dt=F32):
        return self.es.enter_context(self.nc.sbuf_tensor("s_" + name, shape, dt))

    def ps(self, name, shape, dt=F32):
        return self.es.enter_context(self.nc.psum_tensor("p_" + name, shape, dt))

    def dram(self, name, shape, dt=F32, kind="Internal"):
        return self.nc.dram_tensor(name, shape, dt, kind=kind).ap()

    def inp(self, name, shape, dt=F32):
        return self.nc.dram_tensor(name, shape, dt, kind="ExternalInput").ap()

    def outp(self, name, shape, dt=F32):
        return self.nc.dram_tensor(name, shape, dt, kind="ExternalOutput").ap()


class Rot:
    def __init__(self, k, name, shape, dt=F32, n=2, psum=False):
        self.t = [(k.ps if psum else k.sb)("%s%d" % (name, i), shape, dt) for i in range(n)]
        self.keys = ["%s%d" % (name, i) for i in range(n)]
        self.i = 0

    def next(self):
        j = self.i % len(self.t)
        self.i += 1
        return self.t[j], self.keys[j]


def load_fm(k, dst, dst_key, src2d, n, ident, scratch, scratch_key, pst, pst_key):
    S = k.S
    S.dma("sp", scratch[0:n, 0:128], src2d, writes=[scratch_key])
    S.op("pe", lambda e: e.transpose(out=pst[:, 0:n], in_=scratch[0:n, 0:128], identity=ident[0:n, 0:n]),
         reads=[scratch_key, "ident"], writes=[pst_key])
    S.op("dve", lambda e: e.tensor_copy(out=dst, in_=pst[:, 0:n]), reads=[pst_key], writes=[dst_key])


def adaln_fm(k, layer, c_in, cctx_in, ada_w, ada_b, ident, modfm, tmp):
    S = k.S
    sc, pst, wbuf = tmp["sc"], tmp["pst"], tmp["wbuf"]
    S.dma("sp", sc[0:8, 0:128], c_in.rearrange("o (k p) -> (o k) p", p=128), writes=["sc"])
    S.dma("sp", sc[8:16, 0:128], cctx_in.rearrange("(k p) -> k p", p=128), writes=["sc"])
    S.op("act", lambda e: e.activation(out=sc[0:16, 128:256], in_=sc[0:16, 0:128], func=AF.Silu), reads=["sc"], writes=["sc"])
    S.op("pe", lambda e: e.transpose(out=pst[:, 0:16], in_=sc[0:16, 128:256], identity=ident[0:16, 0:16]),
         reads=["sc", "ident"], writes=["pst"])
    cfm = tmp["cfm"]
    S.op("dve", lambda e: e.tensor_copy(out=cfm[:], in_=pst[:, 0:16]), reads=["pst"], writes=["cfm"])
    bfm = tmp["bfm"]
    load_fm(k, bfm[:], "bfm", ada_b[layer].rearrange("(j p) -> j p", p=128), 48, ident, sc, "sc", pst, "pst")
    aw = ada_w[layer].rearrange("(kc p) n -> p kc n", p=128)
    pm = tmp["pm"]
    for g in range(12):
        wt, wk = wbuf.next()
        S.dma("sp" if g % 2 == 0 else "act", wt[:], aw[:, :, g * 512:(g + 1) * 512], writes=[wk])
        for j in range(4):
            jj = g * 4 + j
            for kc in range(8):
                S.op("pe", lambda e, wt=wt, j=j, kc=kc, jj=jj: e.matmul(
                    pm[:, jj * 2:jj * 2 + 2], wt[:, kc, j * 128:(j + 1) * 128],
                    cfm[:, kc:kc + 9:8], start=(kc == 0), stop=(kc == 7)),
                    reads=[wk, "cfm"], writes=["pm"])
    for col in range(2):
        S.op("dve", lambda e, col=col: e.tensor_tensor(out=modfm[:, :, col], in0=pm[:, col:96:2], in1=bfm[:], op=ALU.add),
             reads=["pm", "bfm"], writes=["modfm"])


def bcast_rows(k, dst, dst_key, src_fm, src_key, n, ident, ones, diag, pb):
    S = k.S
    for j in range(n):
        dg, dk = diag.next()
        pt, pk = pb.next()
        S.op("dve", lambda e, dg=dg, j=j: e.tensor_scalar(out=dg[:], in0=ident[:], scalar1=src_fm[:, j:j + 1], scalar2=None, op0=ALU.mult),
             reads=["ident", src_key], writes=[dk])
        S.op("pe", lambda e, dg=dg, pt=pt: e.matmul(pt[:, 0:128], ones[:], dg[:], start=True, stop=True),
             reads=["ones", dk], writes=[pk])
        S.op("act", lambda e, pt=pt, j=j: e.copy(out=dst[:, j * 128:(j + 1) * 128], in_=pt[:, 0:128]),
             reads=[pk], writes=[dst_key])


class Arena:
    def __init__(self, k, nbytes=212000):
        self.k = k
        self.t = k.es.enter_context(k.nc.sbuf_tensor("arena", [128, nbytes], mybir.dt.uint8))
        self.n = nbytes
        self.off = 0

    def alloc(self, shape, dt=F32):
        sz = {F32: 4, BF16: 2, I32: 4, U32: 4}[dt]
        n = sz
        for s in shape[1:]:
            n *= s
        n_al = (n + 63) // 64 * 64
        assert self.off + n_al <= self.n, ("arena overflow", self.off, n_al, self.n)
        v = self.t[:, self.off:self.off + n].bitcast(dt)
        self.off += n_al
        if len(shape) == 3:
            v = v.rearrange("p (a b) -> p a b", a=shape[1])
        elif len(shape) == 4:
            v = v.rearrange("p (a b c) -> p a b c", a=shape[1], b=shape[2])
        if shape[0] < 128:
            v = v[0:shape[0]]
        return v

    def mark(self):
        return self.off

    def release(self, m):
        self.off = m


class RotA:
    def __init__(self, ar, name, shape, dt=F32, n=2):
        self.t = [ar.alloc(shape, dt) for _ in range(n)]
        self.keys = ["%s%d" % (name, i) for i in range(n)]
        self.i = 0

    def next(self):
        j = self.i % len(self.t)
        self.i += 1
        return self.t[j], self.keys[j]


def bc_load(k, dst, key, vec_ap, n):
    k.S.dma("sp", dst, vec_ap.rearrange("(o n) -> o n", o=1).broadcast_to([128, n]), writes=[key])


def build_post(nc, T=2048, TBM=1024, NEXP=32, final=False):
    NT = T // 128
    es = ExitStack()
    k = K(nc, es)
    S = k.S
    h_d = k.inp("h", [T, D]); mixT_d = k.inp("mixT", [D, T], BF16)
    wout_d = k.inp("w_out", [D, D]); bout_d = k.inp("b_out", [D])
    n2g_d = k.inp("norm2_g", [D]); modv_d = k.inp("modv", [6, D])
    rw_d = k.inp("router_w", [D, NE]); rb_d = k.inp("router_b", [NE])
    wgu_d = k.inp("w_gu", [NEXP, D, 2 * D]); bgu_d = k.inp("b_gu_fm", [128, 16, NE])
    wdn_d = k.inp("w_down", [NEXP, D, D]); bdn_d = k.inp("b_down", [NE, D])
    fg_d = k.inp("final_g", [D]); ident_d = k.inp("ident", [128, 128])
    out_d = k.outp("hout", [T, D])
    hmid_d = k.dram("hmid", [T, D])

    ident = k.sb("ident", [128, 128]); S.dma("sp", ident[:], ident_d, writes=["ident"])
    g2bc = k.sb("g2bc", [128, D]); bc_load(k, g2bc[:], "g2bc", modv_d[2], D)
    g5bc = g2bc
    boutbc = k.sb("boutbc", [128, D]); bc_load(k, boutbc[:], "boutbc", bout_d, D)
    rbbc = k.sb("rbbc", [128, NE]); bc_load(k, rbbc[:], "rbbc", rb_d, NE)
    fgbc = boutbc
    pA = Rot(k, "pA", [128, 1024], n=2, psum=True)
    pB = Rot(k, "pB", [128, 512], n=2, psum=True)
    pC = Rot(k, "pC", [128, 512], n=2, psum=True)
    sc = k.sb("sc", [128, 128])
    fmv = k.sb("fmv", [128, 3, 8])
    for i, src in enumerate((n2g_d, modv_d[3], modv_d[4])):
        pt, pk = pC.next()
        load_fm(k, fmv[:, i, :], "fmv", src.rearrange("(j p) -> j p", p=128), 8, ident, sc, "sc", pt, pk)
    A2 = k.sb("A2", [128, 8])
    S.op("dve", lambda e: e.scalar_tensor_tensor(out=A2[:], in0=fmv[:, 2, :], scalar=1.0, in1=fmv[:, 0, :], op0=ALU.add, op1=ALU.mult),
         reads=["fmv"], writes=["A2"])
    rw = k.sb("rw", [128, 8, NE]); S.dma("sp", rw[:], rw_d.rearrange("(kc p) n -> p kc n", p=128), writes=["rw"])
    bgu = k.sb("bgu", [128, 16, NE]); S.dma("sp", bgu[:], bgu_d, writes=["bgu"])
    bdn = k.sb("bdn", [NE, D]); S.dma("sp", bdn[:], bdn_d, writes=["bdn"])
    wdn = k.sb("wdn", [128, 8, D], BF16)
    S.dma("pool", wdn[:], wout_d.rearrange("(kc p) n -> p kc n", p=128), writes=["wdn"])

    a2T = k.sb("a2T", [128, 8, T], BF16)
    gate = k.sb("gate", [128, NT, NE])
    ssq = k.sb("ssq", [128, NT]); rstd = k.sb("rstd", [128, NT])
    hin = Rot(k, "hin", [128, D], n=2)
    hTf = k.sb("hT", [128, max(8 * TBM, 11264)], BF16)
    hT = hTf[:, 0:8 * TBM].rearrange("p (a b) -> p a b", a=8)

    class RotV:
        def __init__(self, views, name):
            self.t = views; self.keys = ["%s%d" % (name, i) for i in range(len(views))]; self.i = 0

        def next(self):
            j = self.i % len(self.t); self.i += 1
            return self.t[j], self.keys[j]
    tmpf = RotV([hTf[:, 0:2048].bitcast(F32), hTf[:, 2048:4096].bitcast(F32)], "tmpf")
    a2f = RotV([hTf[:, 4096:6144].bitcast(F32).rearrange("p (c t) -> p c t", c=8), hTf[:, 6144:8192].bitcast(F32).rearrange("p (c t) -> p c t", c=8)], "a2f")
    mixt = RotV([hTf[:, 8192:9216].rearrange("p (c t) -> p c t", c=8), hTf[:, 9216:10240].rearrange("p (c t) -> p c t", c=8)], "mixt")
    junk = hTf[:, 10240:11264]
    sm = Rot(k, "sm", [128, 4, NE], n=2)
    mixv = mixT_d.rearrange("(kc p) t -> p kc t", p=128)

    for tt in range(NT):
        ht, hk = hin.next(); mt, mk_ = mixt.next(); tf, tk = tmpf.next()
        S.dma("sp", ht[:], h_d[tt * 128:(tt + 1) * 128, :], writes=[hk])
        S.dma("act", mt[:], mixv[:, :, tt * 128:(tt + 1) * 128], writes=[mk_])
        py, pyk = pA.next()
        for cg in range(2):
            for kc in range(8):
                S.op("pe", lambda e, py=py, mt=mt, cg=cg, kc=kc: e.matmul(
                    py[:, cg * 512:(cg + 1) * 512], mt[:, kc, :], wdn[:, kc, cg * 512:(cg + 1) * 512],
                    start=(kc == 0), stop=(kc == 7)), reads=[mk_, "wdn"], writes=[pyk])
        S.op("dve", lambda e, tf=tf, py=py: e.tensor_tensor(out=tf[:], in0=py[:], in1=boutbc[:], op=ALU.add),
             reads=[pyk, "boutbc"], writes=[tk])
        S.op("pool", lambda e, tf=tf: e.tensor_tensor(out=tf[:], in0=tf[:], in1=g2bc[:], op=ALU.mult),
             reads=[tk, "g2bc"], writes=[tk])
        S.op("dve", lambda e, tf=tf, ht=ht: e.tensor_tensor(out=ht[:], in0=ht[:], in1=tf[:], op=ALU.add),
             reads=[tk, hk], writes=[hk])
        S.dma("sp", hmid_d[tt * 128:(tt + 1) * 128, :], ht[:], reads=[hk], writes=[("hmid", tt)])
        S.op("act", lambda e, ht=ht, tt=tt: e.activation(out=junk[:], in_=ht[:], func=AF.Square, accum_out=ssq[:, tt:tt + 1]),
             reads=[hk], writes=["junk", ("ssq", tt)])
        S.op("act", lambda e, tt=tt: e.activation(out=rstd[:, tt:tt + 1], in_=ssq[:, tt:tt + 1], func=AF.Sqrt, scale=1.0 / D, bias=EPS),
             reads=[("ssq", tt)], writes=[("rstd", tt)])
        S.op("dve", lambda e, tt=tt: e.reciprocal(out=rstd[:, tt:tt + 1], in_=rstd[:, tt:tt + 1]),
             reads=[("rstd", tt)], writes=[("rstd", tt)])
        S.op("dve", lambda e, tf=tf, ht=ht, tt=tt: e.tensor_scalar(out=tf[:], in0=ht[:], scalar1=rstd[:, tt:tt + 1], scalar2=None, op0=ALU.mult),
             reads=[hk, ("rstd", tt)], writes=[tk])
        pt, ptk = pA.next()
        for c in range(8):
            S.op("pe", lambda e, pt=pt, tf=tf, c=c: e.transpose(out=pt[:, c * 128:(c + 1) * 128], in_=tf[:, c * 128:(c + 1) * 128], identity=ident[:]),
                 reads=[tk, "ident"], writes=[ptk])
        af, ak = a2f.next()
        for c in range(8):
            S.op("act", lambda e, pt=pt, af=af, c=c: e.activation(out=af[:, c, :], in_=pt[:, c * 128:(c + 1) * 128], func=AF.Identity,
                                                                 scale=A2[:, c:c + 1], bias=fmv[:, 1, c:c + 1]),
                 reads=[ptk, "A2", "fmv"], writes=[ak])
        S.op("pool", lambda e, af=af, tt=tt: e.tensor_copy(out=a2T[:, :, tt * 128:(tt + 1) * 128], in_=af[:]),
             reads=[ak], writes=[("a2T", tt)])
        pl, plk = pC.next()
        for c in range(8):
            S.op("pe", lambda e, pl=pl, af=af, c=c: e.matmul(pl[:, 0:NE], af[:, c, :], rw[:, c, :], start=(c == 0), stop=(c == 7)),
                 reads=[ak, "rw"], writes=[plk])
        s4, sk = sm.next()
        S.op("dve", lambda e, s4=s4, pl=pl: e.tensor_tensor(out=s4[:, 0, :], in0=pl[:, 0:NE], in1=rbbc[:], op=ALU.add),
             reads=[plk, "rbbc"], writes=[sk])
        S.op("dve", lambda e, s4=s4: e.max(out=s4[:, 3, 0:8], in_=s4[:, 0, :]), reads=[sk], writes=[sk])
        S.op("dve", lambda e, s4=s4: e.tensor_scalar(out=s4[:, 1, :], in0=s4[:, 0, :], scalar1=s4[:, 3, 3:4], scalar2=None, op0=ALU.is_ge),
             reads=[sk], writes=[sk])
        S.op("dve", lambda e, s4=s4: e.tensor_scalar(out=s4[:, 3, 8:9], in0=s4[:, 3, 0:1], scalar1=-1.0, scalar2=None, op0=ALU.mult),
             reads=[sk], writes=[sk])
        S.op("act", lambda e, s4=s4: e.activation(out=s4[:, 2, :], in_=s4[:, 0, :], func=AF.Exp, bias=s4[:, 3, 8:9], scale=1.0),
             reads=[sk], writes=[sk])
        S.op("dve", lambda e, s4=s4: e.tensor_tensor(out=s4[:, 2, :], in0=s4[:, 2, :], in1=s4[:, 1, :], op=ALU.mult),
             reads=[sk], writes=[sk])
        S.op("dve", lambda e, s4=s4: e.tensor_reduce(out=s4[:, 3, 9:10], in_=s4[:, 2, :], axis=AX.X, op=ALU.add),
             reads=[sk], writes=[sk])
        S.op("dve", lambda e, s4=s4: e.reciprocal(out=s4[:, 3, 10:11], in_=s4[:, 3, 9:10]), reads=[sk], writes=[sk])
        S.op("dve", lambda e, s4=s4, tt=tt: e.tensor_scalar(out=gate[:, tt, :], in0=s4[:, 2, :], scalar1=s4[:, 3, 10:11], scalar2=None, op0=ALU.mult),
             reads=[sk], writes=[("gate", tt)])

    NTB = TBM // 128
    NB5 = TBM // 512
    acc = k.sb("acc", [128, NTB, D])
    S.barrier()
    bc_load(k, g5bc[:], "g2bc", modv_d[5], D)
    if final:
        bc_load(k, fgbc[:], "boutbc", fg_d, D)
    slab = Rot(k, "slab", [128, 8, 256], BF16, n=3)
    gsr = Rot(k, "gs", [128, 512], n=2); usr = Rot(k, "us", [128, 512], n=2); sgr = Rot(k, "sg", [128, 512], n=2)
    gTr = Rot(k, "gT", [NE, 128], n=2)
    wguv = wgu_d.rearrange("e (kc p) n -> e p kc n", p=128)
    wdnv = wdn_d.rearrange("e (kc p) n -> e p kc n", p=128)
    for ps_ in range(T // TBM):
        t0 = ps_ * NTB
        for tl in range(NTB):
            tt = t0 + tl
            pt, pk = pC.next(); gt, gk = gTr.next()
            S.op("pe", lambda e, pt=pt, tt=tt: e.transpose(out=pt[0:NE, 0:128], in_=gate[:, tt, :], identity=ident[:]),
                 reads=[("gate", tt), "ident"], writes=[pk])
            S.op("dve", lambda e, pt=pt, gt=gt: e.tensor_copy(out=gt[:], in_=pt[0:NE, 0:128]), reads=[pk], writes=[gk])
            pa, pak = pA.next()
            for cg in range(2):
                S.op("pe", lambda e, pa=pa, gt=gt, cg=cg: e.matmul(pa[:, cg * 512:(cg + 1) * 512], gt[:], bdn[:, cg * 512:(cg + 1) * 512], start=True, stop=True),
                     reads=[gk, "bdn"], writes=[pak])
            S.op("act", lambda e, pa=pa, tl=tl: e.copy(out=acc[:, tl, :], in_=pa[:]), reads=[pak], writes=[("acc", tl)])
        for ex in range(NEXP):
            S.dma("pool", wdn[:], wdnv[ex], writes=["wdn"])
            for j in range(8):
                sl, slk = slab.next()
                S.dma("pool", sl[:, :, 0:128], wguv[ex][:, :, j * 128:(j + 1) * 128], writes=[slk])
                S.dma("pool", sl[:, :, 128:256], wguv[ex][:, :, D + j * 128:D + (j + 1) * 128], writes=[slk])
                for tb in range(NB5):
                    pg, pgk = pA.next()
                    for half in range(2):
                        for kc in range(8):
                            S.op("pe", lambda e, pg=pg, sl=sl, half=half, kc=kc, tb=tb, t0=t0: e.matmul(
                                pg[:, half * 512:(half + 1) * 512], sl[:, kc, half * 128:(half + 1) * 128],
                                a2T[:, kc, t0 * 128 + tb * 512:t0 * 128 + (tb + 1) * 512], start=(kc == 0), stop=(kc == 7)),
                                reads=[slk] + [("a2T", t0 + tb * 4 + q) for q in range(4)], writes=[pgk])
                    gs, gsk = gsr.next(); us, usk = usr.next(); sg, sgk = sgr.next()
                    S.op("dve", lambda e, gs=gs, pg=pg, j=j, ex=ex: e.tensor_scalar(out=gs[:], in0=pg[:, 0:512], scalar1=bgu[:, j, ex:ex + 1], scalar2=7.0, op0=ALU.add, op1=ALU.min),
                         reads=[pgk, "bgu"], writes=[gsk])
                    S.op("act", lambda e, gs=gs, sg=sg: e.activation(out=sg[:], in_=gs[:], func=AF.Sigmoid, scale=1.702),
                         reads=[gsk], writes=[sgk])
                    S.op("dve", lambda e, us=us, pg=pg, j=j, ex=ex: e.tensor_scalar(out=us[:], in0=pg[:, 512:1024], scalar1=bgu[:, 8 + j, ex:ex + 1], scalar2=7.0, op0=ALU.add, op1=ALU.min),
                         reads=[pgk, "bgu"], writes=[usk])
                    S.op("dve", lambda e, us=us: e.tensor_scalar(out=us[:], in0=us[:], scalar1=-7.0, scalar2=1.0, op0=ALU.max, op1=ALU.add),
                         reads=[usk], writes=[usk])
                    S.op("pool", lambda e, gs=gs, sg=sg: e.tensor_tensor(out=gs[:], in0=gs[:], in1=sg[:], op=ALU.mult),
                         reads=[gsk, sgk], writes=[gsk])
                    S.op("pool", lambda e, gs=gs, us=us, j=j, tb=tb: e.tensor_tensor(out=hT[:, j, tb * 512:(tb + 1) * 512], in0=gs[:], in1=us[:], op=ALU.mult),
                         reads=[gsk, usk], writes=[("hT", j, tb)])
            for tl in range(NTB):
                for cg in range(2):
                    pd, pdk = pB.next()
                    for c in range(8):
                        S.op("pe", lambda e, pd=pd, tl=tl, c=c, cg=cg: e.matmul(pd[:], hT[:, c, tl * 128:(tl + 1) * 128], wdn[:, c, cg * 512:(cg + 1) * 512],
                                                                               start=(c == 0), stop=(c == 7)),
                             reads=[("hT", c, tl // 4), "wdn"], writes=[pdk])
                    S.op("dve", lambda e, pd=pd, tl=tl, cg=cg, ex=ex, t0=t0: e.scalar_tensor_tensor(
                        out=acc[:, tl, cg * 512:(cg + 1) * 512], in0=pd[:], scalar=gate[:, t0 + tl, ex:ex + 1],
                        in1=acc[:, tl, cg * 512:(cg + 1) * 512], op0=ALU.mult, op1=ALU.add),
                        reads=[pdk, ("gate", t0 + tl), ("acc", tl)], writes=[("acc", tl)])
        for tl in range(NTB):
            tt = t0 + tl
            ht, hk = hin.next()
            S.dma("sp", ht[:], hmid_d[tt * 128:(tt + 1) * 128, :], reads=[("hmid", tt)], writes=[hk])
            S.op("pool", lambda e, tl=tl: e.tensor_tensor(out=acc[:, tl, :], in0=acc[:, tl, :], in1=g5bc[:], op=ALU.mult),
                 reads=[("acc", tl), "g2bc"], writes=[("acc", tl)])
            S.op("dve", lambda e, ht=ht, tl=tl: e.tensor_tensor(out=ht[:], in0=ht[:], in1=acc[:, tl, :], op=ALU.add),
                 reads=[hk, ("acc", tl)], writes=[hk])
            if final:
                S.op("act", lambda e, ht=ht, tt=tt, tl=tl: e.activation(out=acc[:, tl, :], in_=ht[:], func=AF.Square, accum_out=ssq[:, tt:tt + 1]),
                     reads=[hk, ("acc", tl)], writes=[("acc", tl), ("ssq", tt)])
                S.op("act", lambda e, tt=tt: e.activation(out=rstd[:, tt:tt + 1], in_=ssq[:, tt:tt + 1], func=AF.Sqrt, scale=1.0 / D, bias=EPS),
                     reads=[("ssq", tt)], writes=[("rstd", tt)])
                S.op("dve", lambda e, tt=tt: e.reciprocal(out=rstd[:, tt:tt + 1], in_=rstd[:, tt:tt + 1]),
                     reads=[("rstd", tt)], writes=[("rstd", tt)])
                S.op("dve", lambda e, ht=ht, tt=tt: e.scalar_tensor_tensor(out=ht[:], in0=ht[:], scalar=rstd[:, tt:tt + 1], in1=fgbc[:], op0=ALU.mult, op1=ALU.mult),
                     reads=[hk, ("rstd", tt), "boutbc"], writes=[hk])
            S.dma("sp", out_d[tt * 128:(tt + 1) * 128, :], ht[:], reads=[hk], writes=[("out", tt)])
    S.finish([])
    es.close()
    return nc


def build_ada(nc, NL=2, W=768):
    es = ExitStack(); k = K(nc, es); S = k.S
    c_d = k.inp("c", [1, D]); cc_d = k.inp("c_ctx", [D]); aw_d = k.inp("ada_w", [NL, D, W]); ab_d = k.inp("ada_b", [NL, W])
    ident_d = k.inp("ident", [128, 128]); out_d = k.outp("modv", [NL, 2, W])
    ident = k.sb("ident", [128, 128]); S.dma("sp", ident[:], ident_d, writes=["ident"])
    sc = k.sb("sc", [16, 256])
    S.dma("sp", sc[0:8, 0:128], c_d.rearrange("o (k p) -> (o k) p", p=128), writes=["sc"])
    S.dma("sp", sc[8:16, 0:128], cc_d.rearrange("(k p) -> k p", p=128), writes=["sc"])
    S.op("act", lambda e: e.activation(out=sc[0:16, 128:256], in_=sc[0:16, 0:128], func=AF.Silu), reads=["sc"], writes=["sc"])
    pst = k.ps("pst", [128, 512])
    S.op("pe", lambda e: e.transpose(out=pst[:, 0:16], in_=sc[0:16, 128:256], identity=ident[0:16, 0:16]), reads=["sc", "ident"], writes=["pst"])
    cfm = k.sb("cfm", [128, 16])
    S.op("dve", lambda e: e.tensor_copy(out=cfm[:], in_=pst[:, 0:16]), reads=["pst"], writes=["cfm"])
    pm = Rot(k, "pm", [128, 512], n=2, psum=True)
    for l in range(NL):
        w = k.sb("w%d" % l, [128, 8, W]); S.dma("sp", w[:], aw_d[l].rearrange("(kc p) n -> p kc n", p=128), writes=["w%d" % l])
        bb = k.sb("bb%d" % l, [2, W]); S.dma("act", bb[:], ab_d[l].rearrange("(o n) -> o n", o=1).broadcast_to([2, W]), writes=["bb%d" % l])
        res = k.sb("res%d" % l, [2, W])
        for c0 in range(0, W, 512):
            cw = min(512, W - c0)
            p, pk = pm.next()
            for kc in range(8):
                S.op("pe", lambda e, p=p, w=w, kc=kc, c0=c0, cw=cw: e.matmul(p[0:2, 0:cw], cfm[:, kc:kc + 9:8], w[:, kc, c0:c0 + cw], start=(kc == 0), stop=(kc == 7)),
                     reads=["cfm", "w%d" % l], writes=[pk])
            S.op("dve", lambda e, p=p, res=res, bb=bb, c0=c0, cw=cw: e.tensor_tensor(out=res[:, c0:c0 + cw], in0=p[0:2, 0:cw], in1=bb[:, c0:c0 + cw], op=ALU.add),
                 reads=[pk, "bb%d" % l], writes=["res%d" % l])
        S.dma("sp", out_d[l], res[:], reads=["res%d" % l], writes=[("o", l)])
    S.finish([]); es.close()
    return nc


def norm_mod_T(k, x_src, ntiles, A, B, aT_fn, pA, ident, tag="n"):
    S = k.S
    xin = Rot(k, tag + "xin", [128, D], n=2)
    junk = k.sb(tag + "junk", [128, D], BF16)
    ssq = k.sb(tag + "ssq", [128, ntiles]); rstd = k.sb(tag + "rstd", [128, ntiles])
    for i in range(ntiles):
        xt, xk = xin.next()
        S.dma("sp" if i % 2 == 0 else "act", xt[:], x_src(i), writes=[xk])
        S.op("act", lambda e, xt=xt, i=i: e.activation(out=junk[:], in_=xt[:], func=AF.Square, accum_out=ssq[:, i:i + 1]),
             reads=[xk], writes=[tag + "junk", (tag + "ssq", i)])
        S.op("act", lambda e, i=i: e.activation(out=rstd[:, i:i + 1], in_=ssq[:, i:i + 1], func=AF.Sqrt, scale=1.0 / D, bias=EPS),
             reads=[(tag + "ssq", i)], writes=[(tag + "rstd", i)])
        S.op("dve", lambda e, i=i: e.reciprocal(out=rstd[:, i:i + 1], in_=rstd[:, i:i + 1]), reads=[(tag + "rstd", i)], writes=[(tag + "rstd", i)])
        S.op("dve", lambda e, xt=xt, i=i: e.tensor_scalar(out=xt[:], in0=xt[:], scalar1=rstd[:, i:i + 1], scalar2=None, op0=ALU.mult),
             reads=[xk, (tag + "rstd", i)], writes=[xk])
        pt, ptk = pA.next()
        for c in range(8):
            S.op("pe", lambda e, pt=pt, xt=xt, c=c: e.transpose(out=pt[:, c * 128:(c + 1) * 128], in_=xt[:, c * 128:(c + 1) * 128], identity=ident[:]),
                 reads=[xk, "ident"], writes=[ptk])
        dst, dk = aT_fn(i)
        sA, sB, skeys = A(i), B(i), ["A1", "B1"]
        for c in range(8):
            S.op("act", lambda e, pt=pt, dst=dst, c=c, sA=sA, sB=sB: e.activation(out=dst[:, c, :], in_=pt[:, c * 128:(c + 1) * 128], func=AF.Identity,
                                                                               scale=sA[:, c:c + 1], bias=sB[:, c:c + 1]),
                 reads=[ptk] + skeys, writes=[dk])


def load_AB(k, g_d, shift_rows, scale_rows, ident, pC, nvar):
    S = k.S
    sc = k.sb("ABsc", [128, 128])
    fm = k.sb("ABfm", [128, 1 + 2 * nvar, 8])
    srcs = [g_d] + list(shift_rows) + list(scale_rows)
    for i, src in enumerate(srcs):
        pt, pk = pC.next()
        load_fm(k, fm[:, i, :], "B1", src.rearrange("(j p) -> j p", p=128), 8, ident, sc, "ABsc", pt, pk)
    A = k.sb("ABA", [128, nvar, 8])
    for v in range(nvar):
        S.op("dve", lambda e, v=v: e.scalar_tensor_tensor(out=A[:, v, :], in0=fm[:, 1 + nvar + v, :], scalar=1.0, in1=fm[:, 0, :], op0=ALU.add, op1=ALU.mult),
             reads=["B1"], writes=["A1"])
    return A, fm


def bcast_ap(ap, n):
    a = [list(x) for x in ap.ap]
    return bass.AP(ap.tensor, ap.offset, [a[0], [0, n]] + a[1:])


def build_proj0(nc, TL=2048, TC=256):
    NTL, NTC = TL // 128, TC // 128
    NT = NTL + NTC
    E_IN = 2576
    es = ExitStack(); k = K(nc, es); S = k.S
    x_d = k.inp("x", [TL, D]); ctx_d = k.inp("ctx", [TC, D]); modv_d = k.inp("modv", [2, 6, D]); n1g_d = k.inp("norm1_g", [D])
    win_d = k.inp("w_in", [D, E_IN]); bin_d = k.inp("b_in", [E_IN]); qg_d = k.inp("q_gain", [128]); kg_d = k.inp("k_gain", [128])
    cos_d = k.inp("cos", [TL, 64]); sin_d = k.inp("sin", [TL, 64]); ident_d = k.inp("ident", [128, 128])
    QT_d = k.outp("QT", [128, 4, TL], BF16); KT_d = k.outp("KT", [128, 2, TL + TC], BF16)
    V_d = k.outp("V", [TL + TC, 256], BF16); PM_d = k.outp("PM", [TL + TC, 1552])
    ident = k.sb("ident", [128, 128]); S.dma("sp", ident[:], ident_d, writes=["ident"])
    pA = Rot(k, "pA", [128, 1024], n=2, psum=True); pB = Rot(k, "pB", [128, 512], n=2, psum=True); pC = Rot(k, "pC", [128, 512], n=2, psum=True)
    A, fm = load_AB(k, n1g_d, [modv_d[0, 0], modv_d[1, 0]], [modv_d[0, 1], modv_d[1, 1]], ident, pC, 2)
    win = k.sb("win", [128, 8, E_IN], BF16)
    S.dma("pool", win[:], win_d.rearrange("(kc p) n -> p kc n", p=128), writes=["win"])
    binbc = k.sb("binbc", [128, E_IN]); bc_load(k, binbc[:], "binbc", bin_d, E_IN)
    gbc = k.sb("gbc", [128, 2, 128]); bc_load(k, gbc[:, 0, :], "gbc", qg_d, 128); bc_load(k, gbc[:, 1, :], "gbc", kg_d, 128)
    aTr = Rot(k, "aT", [128, 8, 128], BF16, n=2)
    aT_cur = {}

    def aT_fn(i):
        t, kk = aTr.next(); aT_cur[i] = (t, kk); return t, kk
    x_src = lambda i: (x_d[i * 128:(i + 1) * 128, :] if i < NTL else ctx_d[(i - NTL) * 128:(i - NTL + 1) * 128, :])
    var = lambda i: 0 if i < NTL else 1
    pr = Rot(k, "p", [128, E_IN], n=2)
    sqr = k.sb("sq", [128, 768]); hs = Rot(k, "hs", [128, 8], n=2)
    cs = Rot(k, "cs", [128, 2, 64], n=2)
    rt = Rot(k, "rt", [128, 4, 6, 64], n=1)
    qkT = Rot(k, "qkT", [128, 6, 128], BF16, n=2); vb = Rot(k, "vb", [128, 256], BF16, n=2)
    groups = [(0, 512), (512, 512), (1024, 512), (1536, 512), (2048, 512), (2560, 16)]
    xin = Rot(k, "xin", [128, D], n=2); junk = k.sb("junk", [128, D], BF16)
    ssq = k.sb("ssq", [128, NT]); rstd = k.sb("rstd", [128, NT])
    for i in range(NT):
        lat = i < NTL
        tok0 = i * 128 if lat else TL + (i - NTL) * 128
        xt, xk = xin.next()
        S.dma("sp", xt[:], x_src(i), writes=[xk])
        S.op("act", lambda e, xt=xt, i=i: e.activation(out=junk[:], in_=xt[:], func=AF.Square, accum_out=ssq[:, i:i + 1]), reads=[xk], writes=["junk", ("ssq", i)])
        S.op("act", lambda e, i=i: e.activation(out=rstd[:, i:i + 1], in_=ssq[:, i:i + 1], func=AF.Sqrt, scale=1.0 / D, bias=EPS), reads=[("ssq", i)], writes=[("rstd", i)])
        S.op("dve", lambda e, i=i: e.reciprocal(out=rstd[:, i:i + 1], in_=rstd[:, i:i + 1]), reads=[("rstd", i)], writes=[("rstd", i)])
        S.op("dve", lambda e, xt=xt, i=i: e.tensor_scalar(out=xt[:], in0=xt[:], scalar1=rstd[:, i:i + 1], scalar2=None, op0=ALU.mult), reads=[xk, ("rstd", i)], writes=[xk])
        pt, ptk = pA.next()
        for c in range(8):
            S.op("pe", lambda e, pt=pt, xt=xt, c=c: e.transpose(out=pt[:, c * 128:(c + 1) * 128], in_=xt[:, c * 128:(c + 1) * 128], identity=ident[:]), reads=[xk, "ident"], writes=[ptk])
        at, ak = aTr.next()
        v = var(i)
        for c in range(8):
            S.op("act", lambda e, pt=pt, at=at, c=c, v=v: e.activation(out=at[:, c, :], in_=pt[:, c * 128:(c + 1) * 128], func=AF.Identity,
                                                                    scale=A[:, v, c:c + 1], bias=fm[:, 1 + v, c:c + 1]), reads=[ptk, "A1", "B1"], writes=[ak])
        p, pk = pr.next()
        for (c0, cw) in groups:
            pg, pgk = pB.next()
            for kc in range(8):
                S.op("pe", lambda e, pg=pg, at=at, kc=kc, c0=c0, cw=cw: e.matmul(pg[:, 0:cw], at[:, kc, :], win[:, kc, c0:c0 + cw], start=(kc == 0), stop=(kc == 7)),
                     reads=[ak, "win"], writes=[pgk])
            S.op("dve", lambda e, p=p, pg=pg, c0=c0, cw=cw: e.tensor_tensor(out=p[:, c0:c0 + cw], in0=pg[:, 0:cw], in1=binbc[:, c0:c0 + cw], op=ALU.add),
                 reads=[pgk, "binbc"], writes=[pk])
        S.op("pool", lambda e, p=p: e.tensor_tensor(out=sqr[:], in0=p[:, 0:768], in1=p[:, 0:768], op=ALU.mult), reads=[pk], writes=["sq"])
        h8, hk = hs.next()
        S.op("dve", lambda e, h8=h8: e.tensor_reduce(out=h8[:, 0:6], in_=sqr[:].rearrange("p (h d) -> p h d", h=6), axis=AX.X, op=ALU.add), reads=["sq"], writes=[hk])
        S.op("act", lambda e, h8=h8: e.activation(out=h8[:, 0:6], in_=h8[:, 0:6], func=AF.Sqrt, scale=1.0 / 128, bias=EPS), reads=[hk], writes=[hk])
        S.op("dve", lambda e, h8=h8: e.reciprocal(out=h8[:, 0:6], in_=h8[:, 0:6]), reads=[hk], writes=[hk])
        for h in range(6):
            S.op("dve", lambda e, p=p, h=h, h8=h8: e.scalar_tensor_tensor(out=p[:, h * 128:(h + 1) * 128], in0=p[:, h * 128:(h + 1) * 128], scalar=h8[:, h:h + 1],
                                                                          in1=gbc[:, 0 if h < 4 else 1, :], op0=ALU.mult, op1=ALU.mult), reads=[pk, hk, "gbc"], writes=[pk])
        if lat:
            ct, ck = cs.next()
            S.dma("act", ct[:, 0, :], cos_d[tok0:tok0 + 128, :], writes=[ck]); S.dma("act", ct[:, 1, :], sin_d[tok0:tok0 + 128, :], writes=[ck])
            r, rk = rt.next()
            x5 = p[:, 0:768].rearrange("p (h a t d) -> p h a t d", h=6, a=2, t=2)
            x1, x2 = x5[:, :, :, 0, :], x5[:, :, :, 1, :]
            cosb = bcast_ap(ct[:, 0, :].rearrange("p (a d) -> p a d", a=2), 6); sinb = bcast_ap(ct[:, 1, :].rearrange("p (a d) -> p a d", a=2), 6)
            r4 = [r[:, q, :, :].rearrange("p h (a d) -> p h a d", a=2) for q in range(4)]
            S.op("dve", lambda e, r4=r4, x1=x1, cosb=cosb: e.tensor_tensor(out=r4[0], in0=x1, in1=cosb, op=ALU.mult), reads=[pk, ck], writes=[rk])
            S.op("pool", lambda e, r4=r4, x2=x2, sinb=sinb: e.tensor_tensor(out=r4[1], in0=x2, in1=sinb, op=ALU.mult), reads=[pk, ck], writes=[rk])
            S.op("dve", lambda e, r4=r4, x2=x2, cosb=cosb: e.tensor_tensor(out=r4[2], in0=x2, in1=cosb, op=ALU.mult), reads=[pk, ck], writes=[rk])
            S.op("pool", lambda e, r4=r4, x1=x1, sinb=sinb: e.tensor_tensor(out=r4[3], in0=x1, in1=sinb, op=ALU.mult), reads=[pk, ck], writes=[rk])
            S.op("dve", lambda e, r4=r4, x1=x1: e.tensor_tensor(out=x1, in0=r4[0], in1=r4[1], op=ALU.subtract), reads=[rk], writes=[pk])
            S.op("pool", lambda e, r4=r4, x2=x2: e.tensor_tensor(out=x2, in0=r4[2], in1=r4[3], op=ALU.add), reads=[rk], writes=[pk])
        pq, pqk = pA.next()
        for h in range(6):
            S.op("pe", lambda e, pq=pq, p=p, h=h: e.transpose(out=pq[:, h * 128:(h + 1) * 128], in_=p[:, h * 128:(h + 1) * 128], identity=ident[:]), reads=[pk, "ident"], writes=[pqk])
        qt, qk_ = qkT.next()
        S.op("act", lambda e, qt=qt, pq=pq: e.copy(out=qt[:].rearrange("p h d -> p (h d)"), in_=pq[:, 0:768]), reads=[pqk], writes=[qk_])
        if lat:
            S.dma("sp", QT_d[:, :, tok0:tok0 + 128], qt[:, 0:4, :], reads=[qk_], writes=[("QT", i)])
        S.dma("sp", KT_d[:, :, tok0:tok0 + 128], qt[:, 4:6, :], reads=[qk_], writes=[("KT", i)])
        vt, vk = vb.next()
        S.op("act", lambda e, vt=vt, p=p: e.copy(out=vt[:], in_=p[:, 768:1024]), reads=[pk], writes=[vk])
        S.dma("sp", V_d[tok0:tok0 + 128, :], vt[:], reads=[vk], writes=[("V", i)])
        S.dma("sp", PM_d[tok0:tok0 + 128, :], p[:, 1024:E_IN], reads=[pk], writes=[("PM", i)])
    S.finish([]); es.close()
    return nc


def build_attn(nc, TQ=2048, NK=16640):
    NKT = NK // 128
    es = ExitStack(); k = K(nc, es); S = k.S
    QT_d = k.inp("QT", [128, 4, TQ], BF16); KT_d = k.inp("KT", [128, 2, NK], BF16); V_d = k.inp("V", [NK, 256], BF16)
    hf_d = k.inp("hf", [TQ, 512]); hb_d = k.inp("hb", [TQ, 512]); og_d = k.inp("og", [TQ, 512]); hg_d = k.inp("h_gain", [512])
    ident_d = k.inp("ident", [128, 128])
    mix_d = k.outp("mixT", [D, TQ], BF16)
    ident = k.sb("ident", [128, 128]); S.dma("sp", ident[:], ident_d, writes=["ident"])
    KT = k.sb("KT", [128, 2, NK], BF16); V = k.sb("V", [128, NKT, 256], BF16); QT = k.sb("QT", [128, 4, TQ], BF16)
    S.dma("sp", QT[:], QT_d, writes=["QT"])
    NCH = 13 if NKT % 13 == 0 else 1
    step = NKT // NCH
    Vv = V_d.rearrange("(t p) c -> p t c", p=128)
    for ci in range(NCH):
        S.dma("sp", KT[:, :, ci * step * 128:(ci + 1) * step * 128], KT_d[:, :, ci * step * 128:(ci + 1) * step * 128], writes=[("KT", ci)])
        S.dma("act", V[:, ci * step:(ci + 1) * step, :], Vv[:, ci * step:(ci + 1) * step, :], writes=[("V", ci)])
    ones = k.sb("ones", [128, 128], BF16); S.op("dve", lambda e: e.memset(ones[:], 1.0), writes=["ones"])
    pST = Rot(k, "pST", [128, 512], n=2, psum=True); pOT = Rot(k, "pOT", [128, 512], n=2, psum=True)
    pDN = Rot(k, "pDN", [128, 512], n=2, psum=True); pM = Rot(k, "pM", [128, 512], n=2, psum=True)
    PT = Rot(k, "PT", [128, 512], BF16, n=3)
    rd = Rot(k, "rd", [128, 512], n=2); ob = Rot(k, "ob", [128, 512], BF16, n=2)
    sc = 128.0 ** -0.5
    for h in range(4):
        g = h // 2
        for qb in range(TQ // 512):
            ot, otk = pOT.next(); dn, dnk = pDN.next()
            for kt in range(NKT):
                st, stk = pST.next(); pt, ptk = PT.next()
                S.op("pe", lambda e, st=st, g=g, kt=kt, h=h, qb=qb: e.matmul(st[:], KT[:, g, kt * 128:(kt + 1) * 128], QT[:, h, qb * 512:(qb + 1) * 512], start=True, stop=True),
                     reads=[("KT", kt // step), "QT"], writes=[stk])
                S.op("act", lambda e, st=st, pt=pt: e.activation(out=pt[:], in_=st[:], func=AF.Exp, scale=sc), reads=[stk], writes=[ptk])
                S.op("pe", lambda e, ot=ot, pt=pt, g=g, kt=kt: e.matmul(ot[:], V[:, kt, g * 128:(g + 1) * 128], pt[:], start=(kt == 0), stop=(kt == NKT - 1)),
                     reads=[("V", kt // step), ptk], writes=[otk])
                S.op("pe", lambda e, dn=dn, pt=pt, kt=kt: e.matmul(dn[:], ones[:], pt[:], start=(kt == 0), stop=(kt == NKT - 1)),
                     reads=["ones", ptk], writes=[dnk])
            r, rk = rd.next(); o, ok = ob.next()
            S.op("dve", lambda e, r=r, dn=dn: e.reciprocal(out=r[:], in_=dn[:]), reads=[dnk], writes=[rk])
            S.op("dve", lambda e, o=o, ot=ot, r=r: e.tensor_tensor(out=o[:], in0=ot[:], in1=r[:], op=ALU.mult), reads=[otk, rk], writes=[ok])
            S.dma("sp", mix_d[h * 128:(h + 1) * 128, qb * 512:(qb + 1) * 512], o[:], reads=[ok], writes=[("mix", h, qb)])
    hgbc = k.sb("hgbc", [128, 512]); bc_load(k, hgbc[:], "hgbc", hg_d, 512)
    hfr = Rot(k, "hfr", [128, 512], n=2); hbr = Rot(k, "hbr", [128, 512], n=2); ogr = Rot(k, "ogr", [128, 512], n=2)
    sq = k.sb("sq", [128, 512]); h4 = Rot(k, "h4", [128, 4], n=2); mt = Rot(k, "mt", [128, 4, 128], BF16, n=2)
    for i in range(TQ // 128):
        a, ak = hfr.next(); b, bk = hbr.next(); o, ok = ogr.next()
        S.dma("sp", a[:], hf_d[i * 128:(i + 1) * 128, :], writes=[ak]); S.dma("act", b[:], hb_d[i * 128:(i + 1) * 128, :], writes=[bk])
        S.dma("sp", o[:], og_d[i * 128:(i + 1) * 128, :], writes=[ok])
        S.op("dve", lambda e, a=a, b=b: e.tensor_tensor(out=a[:], in0=a[:], in1=b[:], op=ALU.add), reads=[ak, bk], writes=[ak])
        S.op("pool", lambda e, a=a: e.tensor_tensor(out=sq[:], in0=a[:], in1=a[:], op=ALU.mult), reads=[ak], writes=["sq"])
        hh, hk = h4.next()
        S.op("dve", lambda e, hh=hh: e.tensor_reduce(out=hh[:], in_=sq[:].rearrange("p (h d) -> p h d", h=4), axis=AX.X, op=ALU.add), reads=["sq"], writes=[hk])
        S.op("act", lambda e, hh=hh: e.activation(out=hh[:], in_=hh[:], func=AF.Sqrt, scale=1.0 / 128, bias=EPS), reads=[hk], writes=[hk])
        S.op("dve", lambda e, hh=hh: e.reciprocal(out=hh[:], in_=hh[:]), reads=[hk], writes=[hk])
        S.op("act", lambda e, o=o: e.activation(out=o[:], in_=o[:], func=AF.Sigmoid), reads=[ok], writes=[ok])
        for h in range(4):
            S.op("dve", lambda e, a=a, hh=hh, h=h: e.scalar_tensor_tensor(out=a[:, h * 128:(h + 1) * 128], in0=a[:, h * 128:(h + 1) * 128], scalar=hh[:, h:h + 1],
                                                                          in1=hgbc[:, h * 128:(h + 1) * 128], op0=ALU.mult, op1=ALU.mult), reads=[ak, hk, "hgbc"], writes=[ak])
        S.op("pool", lambda e, a=a, o=o: e.tensor_tensor(out=a[:], in0=a[:], in1=o[:], op=ALU.mult), reads=[ak, ok], writes=[ak])
        pm, pmk = pM.next()
        for h in range(4):
            S.op("pe", lambda e, pm=pm, a=a, h=h: e.transpose(out=pm[:, h * 128:(h + 1) * 128], in_=a[:, h * 128:(h + 1) * 128], identity=ident[:]), reads=[ak, "ident"], writes=[pmk])
        m, mk_ = mt.next()
        S.op("act", lambda e, m=m, pm=pm: e.copy(out=m[:].rearrange("p h d -> p (h d)"), in_=pm[:]), reads=[pmk], writes=[mk_])
        S.dma("sp", mix_d[512:1024, i * 128:(i + 1) * 128].rearrange("(h p) t -> p h t", p=128), m[:], reads=[mk_], writes=[("mixm", i)])
    S.finish([]); es.close()
    return nc


def build_mlstm(nc, NS=16640, NCTX=2):
    NC = NS // 128
    es = ExitStack(); k = K(nc, es); S = k.S
    qT_d = k.inp("qT", [64, NS]); kT_d = k.inp("kT", [64, NS]); k_d = k.inp("k", [NS, 64]); v_d = k.inp("v", [NS, 128])
    ig_d = k.inp("ig", [128, NC]); fg_d = k.inp("fg", [128, NC]); fb_d = k.inp("fb", [128, 1]); tri_d = k.inp("tri", [128, 128]); ident_d = k.inp("ident", [128, 128])
    h_d = k.outp("h", [NS - NCTX * 128, 128])
    ident = k.sb("ident", [128, 128]); S.dma("sp", ident[:], ident_d, writes=["ident"])
    tri = k.sb("tri", [128, 128]); S.dma("sp", tri[:], tri_d, writes=["tri"])
    onesf = k.sb("onesf", [128, 128]); S.op("dve", lambda e: e.memset(onesf[:], 1.0), writes=["onesf"])
    qT = k.sb("qT", [64, NS], BF16); kT = k.sb("kT", [64, NS], BF16)
    S.dma("pool", qT[:], qT_d, writes=["qT"]); S.dma("pool", kT[:], kT_d, writes=["kT"])
    kk = k.sb("k", [128, NC, 64])
    for c0 in range(0, NC, 64):
        c1 = min(NC, c0 + 64)
        S.dma("sp", kk[:, c0:c1, :], k_d[c0 * 128:c1 * 128, :].rearrange("(c p) d -> p c d", p=128), writes=["k"])
    v1 = k.sb("v1", [128, NC, 129], BF16)
    S.op("dve", lambda e: e.memset(v1[:, :, 128:129], 1.0), writes=["v1"])
    for c0 in range(0, NC, 64):
        c1 = min(NC, c0 + 64)
        S.dma("pool", v1[:, c0:c1, 0:128], v_d[c0 * 128:c1 * 128, :].rearrange("(c p) d -> p c d", p=128), writes=["v1"])
    names = ["ig", "fg", "logf", "b", "blast", "LW", "U", "abc", "mnext", "mprev", "cm", "mrow", "w", "decay", "rowf", "colf8", "winter8", "emr", "tmp"]
    A = {n: k.sb("a_" + n, [128, NC]) for n in names}
    fb = k.sb("fb", [128, 2]); S.dma("sp", fb[:, 0:1], fb_d, writes=["fb"])
    S.op("dve", lambda e: e.tensor_scalar(out=fb[:, 1:2], in0=fb[:, 0:1], scalar1=-1.0, scalar2=None, op0=ALU.mult), reads=["fb"], writes=["fb"])
    S.dma("sp", A["ig"][:], ig_d, writes=["ig"]); S.dma("sp", A["fg"][:], fg_d, writes=["fg"])
    pP = Rot(k, "pP", [128, 512], n=8, psum=True)

    def ew(eng, fn, reads, writes):
        S.op(eng, fn, reads=reads, writes=writes)
    ew("act", lambda e: e.activation(out=A["tmp"][:], in_=A["fg"][:], func=AF.Exp, scale=-1.0, bias=fb[:, 1:2]), ["fg", "fb"], ["tmp"])
    ew("act", lambda e: e.activation(out=A["tmp"][:], in_=A["tmp"][:], func=AF.Ln, bias=1.0), ["tmp"], ["tmp"])
    ew("dve", lambda e: e.tensor_scalar(out=A["logf"][:], in0=A["tmp"][:], scalar1=-1.0, scalar2=None, op0=ALU.mult), ["tmp"], ["logf"])
    p1, p1k = pP.next(); p2, p2k = pP.next()
    ew("pe", lambda e: e.matmul(p1[:, 0:NC], tri[:], A["logf"][:], start=True, stop=True), ["tri", "logf"], [p1k])
    ew("pe", lambda e: e.matmul(p2[:, 0:NC], onesf[:], A["logf"][:], start=True, stop=True), ["onesf", "logf"], [p2k])
    ew("dve", lambda e: e.tensor_copy(out=A["b"][:], in_=p1[:, 0:NC]), [p1k], ["b"])
    ew("dve", lambda e: e.tensor_copy(out=A["blast"][:], in_=p2[:, 0:NC]), [p2k], ["blast"])
    ew("dve", lambda e: e.tensor_tensor(out=A["U"][:], in0=A["ig"][:], in1=A["b"][:], op=ALU.subtract), ["ig", "b"], ["U"])
    ew("dve", lambda e: e.tensor_tensor(out=A["LW"][:], in0=A["U"][:], in1=A["blast"][:], op=ALU.add), ["U", "blast"], ["LW"])
    zer = k.sb("zer", [128, 128]); S.op("dve", lambda e: e.memset(zer[:], 0.0), writes=["zer"])
    am = k.sb("am", [128, 1]); lh = k.sb("lh", [128, 128]); ut = k.sb("ut", [128, 128]); cmt = k.sb("cmt", [128, 128])
    for c0 in range(0, NC, 128):
        cw = min(128, NC - c0)
        pt, ptk = pP.next()
        ew("pe", lambda e, pt=pt, c0=c0, cw=cw: e.transpose(out=pt[0:cw, 0:128], in_=A["LW"][:, c0:c0 + cw], identity=ident[:]), ["LW", "ident"], [ptk])
        ew("dve", lambda e, pt=pt, cw=cw: e.tensor_reduce(out=am[0:cw, :], in_=pt[0:cw, 0:128], axis=AX.X, op=ALU.max), [ptk], ["am"])
        ew("dve", lambda e, cw=cw: e.tensor_scalar(out=lh[0:cw, :], in0=onesf[0:cw, :], scalar1=am[0:cw, 0:1], scalar2=None, op0=ALU.mult), ["am", "onesf"], ["lh"])
        pa, pak = pP.next()
        ew("pe", lambda e, pa=pa, cw=cw: e.matmul(pa[:, 0:cw], lh[0:cw, :], ident[0:cw, 0:cw], start=True, stop=True), ["lh", "ident"], [pak])
        ew("dve", lambda e, pa=pa, c0=c0, cw=cw: e.tensor_copy(out=A["abc"][:, c0:c0 + cw], in_=pa[:, 0:cw]), [pak], ["abc"])
        pu, puk = pP.next()
        ew("pe", lambda e, pu=pu, c0=c0, cw=cw: e.transpose(out=pu[0:cw, 0:128], in_=A["U"][:, c0:c0 + cw], identity=ident[:]), ["U", "ident"], [puk])
        ew("dve", lambda e, pu=pu, cw=cw: e.tensor_copy(out=ut[0:cw, :], in_=pu[0:cw, 0:128]), [puk], ["ut"])
        ew("dve", lambda e, cw=cw: e.tensor_tensor_scan(out=cmt[0:cw, :], data0=zer[0:cw, :], data1=ut[0:cw, :], initial=-1e30, op0=ALU.add, op1=ALU.max), ["ut", "zer"], ["cmt"])
        pc, pck = pP.next()
        ew("pe", lambda e, pc=pc, cw=cw: e.transpose(out=pc[:, 0:cw], in_=cmt[0:cw, :], identity=ident[0:cw, 0:cw]), ["cmt", "ident"], [pck])
        ew("dve", lambda e, pc=pc, c0=c0, cw=cw: e.tensor_copy(out=A["cm"][:, c0:c0 + cw], in_=pc[:, 0:cw]), [pck], ["cm"])
    ew("dve", lambda e: e.tensor_tensor_scan(out=A["mnext"][:], data0=A["blast"][:], data1=A["abc"][:], initial=0.0, op0=ALU.add, op1=ALU.max), ["blast", "abc"], ["mnext"])
    ew("dve", lambda e: e.memset(A["mprev"][:, 0:1], 0.0), [], ["mprev"])
    ew("dve", lambda e: e.tensor_copy(out=A["mprev"][:, 1:NC], in_=A["mnext"][:, 0:NC - 1]), ["mnext"], ["mprev"])
    ew("dve", lambda e: e.tensor_tensor(out=A["mrow"][:], in0=A["mprev"][:], in1=A["cm"][:], op=ALU.max), ["mprev", "cm"], ["mrow"])
    ew("dve", lambda e: e.tensor_tensor(out=A["mrow"][:], in0=A["mrow"][:], in1=A["b"][:], op=ALU.add), ["mrow", "b"], ["mrow"])
    ew("dve", lambda e: e.tensor_tensor(out=A["tmp"][:], in0=A["LW"][:], in1=A["mnext"][:], op=ALU.subtract), ["LW", "mnext"], ["tmp"])
    ew("act", lambda e: e.activation(out=A["w"][:], in_=A["tmp"][:], func=AF.Exp), ["tmp"], ["w"])
    ew("dve", lambda e: e.tensor_tensor(out=A["tmp"][:], in0=A["blast"][:], in1=A["mprev"][:], op=ALU.add), ["blast", "mprev", "w"], ["tmp"])
    ew("dve", lambda e: e.tensor_tensor(out=A["tmp"][:], in0=A["tmp"][:], in1=A["mnext"][:], op=ALU.subtract), ["tmp", "mnext"], ["tmp"])
    ew("act", lambda e: e.activation(out=A["decay"][:], in_=A["tmp"][:], func=AF.Exp), ["tmp"], ["decay"])
    ew("dve", lambda e: e.tensor_tensor(out=A["tmp"][:], in0=A["b"][:], in1=A["mrow"][:], op=ALU.subtract), ["b", "mrow", "decay"], ["tmp"])
    ew("act", lambda e: e.activation(out=A["rowf"][:], in_=A["tmp"][:], func=AF.Exp), ["tmp"], ["rowf"])
    ew("dve", lambda e: e.tensor_tensor(out=A["tmp"][:], in0=A["tmp"][:], in1=A["mprev"][:], op=ALU.add), ["tmp", "mprev", "rowf"], ["tmp"])
    ew("dve", lambda e: e.tensor_scalar(out=A["tmp"][:], in0=A["tmp"][:], scalar1=-float(np.log(8.0)), scalar2=None, op0=ALU.add), ["tmp"], ["tmp"])
    ew("act", lambda e: e.activation(out=A["winter8"][:], in_=A["tmp"][:], func=AF.Exp), ["tmp"], ["winter8"])
    ew("dve", lambda e: e.tensor_scalar(out=A["tmp"][:], in0=A["U"][:], scalar1=-float(np.log(8.0)), scalar2=None, op0=ALU.add), ["U", "winter8"], ["tmp"])
    ew("act", lambda e: e.activation(out=A["colf8"][:], in_=A["tmp"][:], func=AF.Exp), ["tmp"], ["colf8"])
    ew("act", lambda e: e.activation(out=A["emr"][:], in_=A["mrow"][:], func=AF.Exp, scale=-1.0), ["mrow"], ["emr"])
    pre = ["w", "decay", "rowf", "colf8", "winter8", "emr"]
    Cx = k.sb("Cx", [64, 129]); S.op("dve", lambda e: e.memset(Cx[:], 0.0), writes=["Cx"])
    Cb = Rot(k, "Cb", [64, 129], BF16, n=2); stm = Rot(k, "stm", [128, 128], BF16, n=2); kw = Rot(k, "kw", [128, 64], BF16, n=2)
    t1 = Rot(k, "t1", [128, 129], n=2); dd = Rot(k, "dd", [128, 2], n=2); ho = Rot(k, "ho", [128, 128], n=2)
    for c in range(NC):
        cs = slice(c * 128, (c + 1) * 128)
        if c >= NCTX:
            ps, psk = pP.next()
            ew("pe", lambda e, ps=ps, cs=cs: e.matmul(ps[:, 0:128], kT[:, cs], qT[:, cs], start=True, stop=True), ["kT", "qT"], [psk])
            sm, smk = stm.next()
            ew("dve", lambda e, sm=sm, ps=ps, c=c: e.scalar_tensor_tensor(out=sm[:], in0=ps[:, 0:128], scalar=A["colf8"][:, c:c + 1], in1=tri[:], op0=ALU.mult, op1=ALU.mult),
               [psk, "colf8", "tri"], [smk])
            pa, pak = pP.next()
            ew("pe", lambda e, pa=pa, sm=sm, c=c: e.matmul(pa[:, 0:129], sm[:], v1[:, c, :], start=True, stop=True), [smk, "v1"], [pak])
            cb, cbk = Cb.next()
            ew("act", lambda e, cb=cb: e.copy(out=cb[:], in_=Cx[:]), ["Cx"], [cbk])
            pb, pbk = pP.next()
            ew("pe", lambda e, pb=pb, cb=cb, cs=cs: e.matmul(pb[:, 0:129], qT[:, cs], cb[:], start=True, stop=True), ["qT", cbk], [pbk])
            tt, ttk = t1.next()
            ew("act", lambda e, tt=tt, pa=pa, c=c: e.activation(out=tt[:], in_=pa[:, 0:129], func=AF.Identity, scale=A["rowf"][:, c:c + 1]), [pak, "rowf"], [ttk])
            ew("dve", lambda e, tt=tt, pb=pb, c=c: e.scalar_tensor_tensor(out=tt[:], in0=pb[:, 0:129], scalar=A["winter8"][:, c:c + 1], in1=tt[:], op0=ALU.mult, op1=ALU.add),
               [pbk, "winter8", ttk], [ttk])
            d2, d2k = dd.next()
            ew("dve", lambda e, d2=d2, tt=tt: e.tensor_scalar(out=d2[:, 1:2], in0=tt[:, 128:129], scalar1=-1.0, scalar2=None, op0=ALU.mult), [ttk], [d2k])
            ew("dve", lambda e, d2=d2, tt=tt: e.tensor_tensor(out=d2[:, 0:1], in0=tt[:, 128:129], in1=d2[:, 1:2], op=ALU.max), [ttk, d2k], [d2k])
            ew("dve", lambda e, d2=d2, c=c: e.tensor_tensor(out=d2[:, 0:1], in0=d2[:, 0:1], in1=A["emr"][:, c:c + 1], op=ALU.max), [d2k, "emr"], [d2k])
            ew("dve", lambda e, d2=d2: e.reciprocal(out=d2[:, 1:2], in_=d2[:, 0:1]), [d2k], [d2k])
            hh, hhk = ho.next()
            ew("dve", lambda e, hh=hh, tt=tt, d2=d2: e.tensor_scalar(out=hh[:], in0=tt[:, 0:128], scalar1=d2[:, 1:2], scalar2=None, op0=ALU.mult), [ttk, d2k], [hhk])
            S.dma("sp", h_d[(c - NCTX) * 128:(c - NCTX + 1) * 128, :], hh[:], reads=[hhk], writes=[("h", c)])
        if c < NC - 1:
            kq, kqk = kw.next()
            ew("pool", lambda e, kq=kq, c=c: e.tensor_scalar(out=kq[:], in0=kk[:, c, :], scalar1=A["w"][:, c:c + 1], scalar2=None, op0=ALU.mult), ["k", "w"], [kqk])
            pu, puk = pP.next()
            ew("pe", lambda e, pu=pu, kq=kq, c=c: e.matmul(pu[0:64, 0:129], kq[:], v1[:, c, :], start=True, stop=True), [kqk, "v1"], [puk])
            ew("dve", lambda e, pu=pu, c=c: e.scalar_tensor_tensor(out=Cx[:], in0=Cx[:], scalar=A["decay"][0:64, c:c + 1], in1=pu[0:64, 0:129], op0=ALU.mult, op1=ALU.add),
               ["Cx", "decay", puk], ["Cx"])
    S.finish([]); es.close()
    return nc


def build_hyproj(nc, T=2048):
    NT = T // 128
    es = ExitStack(); k = K(nc, es); S = k.S
    h_d = k.inp("h", [T, D]); modv_d = k.inp("modv", [6, D]); n1g_d = k.inp("norm1_g", [D])
    win_d = k.inp("w_in", [D, 3 * D]); binfm_d = k.inp("b_in_fm", [128, 24]); ident_d = k.inp("ident", [128, 128])
    z_d = k.outp("z0T", [3 * D, T])
    ident = k.sb("ident", [128, 128]); S.dma("sp", ident[:], ident_d, writes=["ident"])
    pA = Rot(k, "pA", [128, 1024], n=2, psum=True); pB = Rot(k, "pB", [128, 512], n=2, psum=True); pC = Rot(k, "pC", [128, 512], n=2, psum=True)
    A, fm = load_AB(k, n1g_d, [modv_d[0]], [modv_d[1]], ident, pC, 1)
    win = k.sb("win", [128, 8, 3 * D], BF16)
    S.dma("pool", win[:], win_d.rearrange("(kc p) n -> p kc n", p=128), writes=["win"])
    binfm = k.sb("binfm", [128, 24]); S.dma("sp", binfm[:], binfm_d, writes=["binfm"])
    aT = k.sb("aT", [128, 8, T], BF16)
    xin = Rot(k, "xin", [128, D], n=2); junk = k.sb("junk", [128, D], BF16)
    ssq = k.sb("ssq", [128, NT]); rstd = k.sb("rstd", [128, NT])
    for i in range(NT):
        xt, xk = xin.next()
        S.dma("sp", xt[:], h_d[i * 128:(i + 1) * 128, :], writes=[xk])
        S.op("act", lambda e, xt=xt, i=i: e.activation(out=junk[:], in_=xt[:], func=AF.Square, accum_out=ssq[:, i:i + 1]), reads=[xk], writes=["junk", ("ssq", i)])
        S.op("act", lambda e, i=i: e.activation(out=rstd[:, i:i + 1], in_=ssq[:, i:i + 1], func=AF.Sqrt, scale=1.0 / D, bias=EPS), reads=[("ssq", i)], writes=[("rstd", i)])
        S.op("dve", lambda e, i=i: e.reciprocal(out=rstd[:, i:i + 1], in_=rstd[:, i:i + 1]), reads=[("rstd", i)], writes=[("rstd", i)])
        S.op("dve", lambda e, xt=xt, i=i: e.tensor_scalar(out=xt[:], in0=xt[:], scalar1=rstd[:, i:i + 1], scalar2=None, op0=ALU.mult), reads=[xk, ("rstd", i)], writes=[xk])
        pt, ptk = pA.next()
        for c in range(8):
            S.op("pe", lambda e, pt=pt, xt=xt, c=c: e.transpose(out=pt[:, c * 128:(c + 1) * 128], in_=xt[:, c * 128:(c + 1) * 128], identity=ident[:]), reads=[xk, "ident"], writes=[ptk])
        for c in range(8):
            S.op("act", lambda e, pt=pt, c=c, i=i: e.activation(out=aT[:, c, i * 128:(i + 1) * 128], in_=pt[:, c * 128:(c + 1) * 128], func=AF.Identity,
                                                              scale=A[:, 0, c:c + 1], bias=fm[:, 1, c:c + 1]), reads=[ptk, "A1", "B1"], writes=[("aT", i)])
    zo = Rot(k, "zo", [128, 512], n=3)
    for j in range(24):
        for tb in range(T // 512):
            pg, pgk = pB.next()
            for kc in range(8):
                S.op("pe", lambda e, pg=pg, j=j, tb=tb, kc=kc: e.matmul(pg[:], win[:, kc, j * 128:(j + 1) * 128], aT[:, kc, tb * 512:(tb + 1) * 512], start=(kc == 0), stop=(kc == 7)),
                     reads=["win"] + [("aT", tb * 4 + q) for q in range(4)], writes=[pgk])
            z, zk = zo.next()
            S.op("act", lambda e, z=z, pg=pg, j=j: e.activation(out=z[:], in_=pg[:], func=AF.Identity, bias=binfm[:, j:j + 1], scale=1.0), reads=[pgk, "binfm"], writes=[zk])
            S.dma("sp", z_d[j * 128:(j + 1) * 128, tb * 512:(tb + 1) * 512], z[:], reads=[zk], writes=[("z", j, tb)])
    S.finish([]); es.close()
    return nc


TWO_PI = float(2 * np.pi)
MAGIC = 12582912.0


def build_hyconv(nc, L=16384, G=8, upto=4, ngroups=None, limit3=None):
    N2 = 2 * L
    assert L == 16384
    es = ExitStack(); k = K(nc, es); S = k.S
    z0_d = k.inp("z0", [3, 128, L]); cw_d = k.inp("cw", [128, 9]); cb_d = k.inp("cb", [128, 3]); skip_d = k.inp("skip", [128, 1]); ndel_d = k.inp("ndelta", [128, 1])
    w1_d = k.inp("fw1", [33, 64]); w2_d = k.inp("fw2", [64, 64]); w3_d = k.inp("fw3", [64, 64]); w4_d = k.inp("fw4", [64, 256])
    fbf_d = k.inp("fbf", [64, 4])
    zf_d = k.inp("zfeat", [33, N2]); tn_d = k.inp("tnorm", [N2])
    F1_d = k.inp("F1", [128, 2, 512]); TW_d = k.inp("TW", [128, 512]); F2_d = k.inp("F2", [128, 3, 128])
    Gt_d = k.inp("Gt", [128, 2, 256]); ITW_d = k.inp("ITW", [128, 2, 256]); F3_d = k.inp("F3", [128, 2, 2, 128])
    out_d = k.outp("zT", [128, L], BF16)
    vx_d = k.dram("vx_s", [128, L]); x0_d = k.dram("x0_s", [128, L]); taps_d = k.dram("taps_s", [128, N2]); y_d = k.dram("y_s", [128, L])
    pP = Rot(k, "pP", [128, 512], n=8, psum=True)
    ar = Arena(k, 204000)
    cw = k.sb("cw", [128, 9]); S.dma("sp", cw[:], cw_d, writes=["cw"]); cb = k.sb("cb", [128, 3]); S.dma("sp", cb[:], cb_d, writes=["cb"])
    skip = k.sb("skip", [128, 1]); S.dma("sp", skip[:], skip_d, writes=["skip"]); ndel = k.sb("ndel", [128, 1]); S.dma("sp", ndel[:], ndel_d, writes=["ndel"])
    TB = 2048
    m0 = ar.mark()
    zb = RotA(ar, "zb", [128, 3, TB + 2], n=2); yb = RotA(ar, "yb", [128, 3, TB], n=2)
    for b in range(L // TB):
        t0 = b * TB
        z, zk = zb.next(); y, yk = yb.next()
        lo = max(t0 - 1, 0); hi = min(t0 + TB + 1, L)
        if b == 0:
            S.op("dve", lambda e, z=z: e.memset(z[:, :, 0:1], 0.0), writes=[zk])
        if b == L // TB - 1:
            S.op("dve", lambda e, z=z: e.memset(z[:, :, TB + 1:TB + 2], 0.0), writes=[zk])
        S.dma("sp", z[:, :, lo - (t0 - 1):hi - (t0 - 1)], z0_d[:, :, lo:hi].rearrange("g p t -> p g t"), writes=[zk])
        for gi in range(3):
            eng = "dve"
            S.op(eng, lambda e, z=z, y=y, gi=gi: e.tensor_scalar(out=y[:, gi, :], in0=z[:, gi, 1:TB + 1], scalar1=cw[:, gi * 3 + 1:gi * 3 + 2], scalar2=cb[:, gi:gi + 1], op0=ALU.mult, op1=ALU.add),
                 reads=[zk, "cw", "cb"], writes=[(yk, gi)])
            S.op(eng, lambda e, z=z, y=y, gi=gi: e.scalar_tensor_tensor(out=y[:, gi, :], in0=z[:, gi, 0:TB], scalar=cw[:, gi * 3:gi * 3 + 1], in1=y[:, gi, :], op0=ALU.mult, op1=ALU.add),
                 reads=[zk, "cw", (yk, gi)], writes=[(yk, gi)])
            S.op(eng, lambda e, z=z, y=y, gi=gi: e.scalar_tensor_tensor(out=y[:, gi, :], in0=z[:, gi, 2:TB + 2], scalar=cw[:, gi * 3 + 2:gi * 3 + 3], in1=y[:, gi, :], op0=ALU.mult, op1=ALU.add),
                 reads=[zk, "cw", (yk, gi)], writes=[(yk, gi)])
        S.op("dve", lambda e, y=y: e.tensor_tensor(out=y[:, 2, :], in0=y[:, 2, :], in1=y[:, 1, :], op=ALU.mult), reads=[(yk, 1), (yk, 2)], writes=[(yk, 2)])
        S.dma("sp", vx_d[:, t0:t0 + TB], y[:, 2, :], reads=[(yk, 2)], writes=[("vx", b)])
        S.dma("act", x0_d[:, t0:t0 + TB], y[:, 0, :], reads=[(yk, 0)], writes=[("x0", b)])
    if upto < 2:
        S.finish([]); es.close(); return nc
    S.barrier(); ar.release(m0)
    w1 = ar.alloc([33, 64]); S.dma("sp", w1[:], w1_d, writes=["fw"]); w2 = ar.alloc([64, 64]); S.dma("sp", w2[:], w2_d, writes=["fw"])
    w3 = ar.alloc([64, 64]); S.dma("sp", w3[:], w3_d, writes=["fw"]); w4 = ar.alloc([64, 256]); S.dma("sp", w4[:], w4_d, writes=["fw"])
    fbf = ar.alloc([64, 8]); S.dma("sp", fbf[:, 0:4], fbf_d, writes=["fbf"])
    for i in range(3):
        S.op("dve", lambda e, i=i: e.tensor_tensor(out=fbf[:, 4 + i:5 + i], in0=fbf[:, 0:1], in1=fbf[:, 1 + i:2 + i], op=ALU.mult), reads=["fbf"], writes=["fbf"])
    zfr = RotA(ar, "zf", [33, 512], n=2); tnr = RotA(ar, "tn", [128, 512], n=2)
    pre = RotA(ar, "pre", [64, 512], n=2); tq = RotA(ar, "tq", [64, 512], n=2); hh = RotA(ar, "hh", [64, 512], n=2); tp = RotA(ar, "tp", [128, 512], n=2)
    ws = [w1, w2, w3]
    for b in range(N2 // 512):
        zf, zfk = zfr.next(); tn, tnk = tnr.next()
        S.dma("sp", zf[:], zf_d[:, b * 512:(b + 1) * 512], writes=[zfk])
        S.dma("act", tn[:], tn_d[b * 512:(b + 1) * 512].rearrange("(o n) -> o n", o=1).broadcast_to([128, 512]), writes=[tnk])
        cur, curk, kin = zf, zfk, 33
        for li in range(3):
            pp, ppk = pP.next()
            S.op("pe", lambda e, pp=pp, cur=cur, li=li, kin=kin: e.matmul(pp[0:64, :], ws[li][0:kin, :], cur[0:kin, :], start=True, stop=True), reads=["fw", curk], writes=[ppk])
            pr, prk = pre.next(); t, tk = tq.next(); h, hk = hh.next()
            S.op("act", lambda e, pr=pr, pp=pp, li=li: e.activation(out=pr[:], in_=pp[0:64, :], func=AF.Identity, scale=fbf[:, 0:1], bias=fbf[:, 4 + li:5 + li]), reads=[ppk, "fbf"], writes=[prk])
            S.op("dve", lambda e, t=t, pr=pr: e.tensor_scalar(out=t[:], in0=pr[:], scalar1=1.0 / TWO_PI, scalar2=MAGIC, op0=ALU.mult, op1=ALU.add), reads=[prk], writes=[tk])
            S.op("dve", lambda e, t=t: e.tensor_scalar(out=t[:], in0=t[:], scalar1=-MAGIC, scalar2=None, op0=ALU.add), reads=[tk], writes=[tk])
            S.op("dve", lambda e, t=t, pr=pr: e.scalar_tensor_tensor(out=t[:], in0=t[:], scalar=-TWO_PI, in1=pr[:], op0=ALU.mult, op1=ALU.add), reads=[tk, prk], writes=[tk])
            S.op("dve", lambda e, t=t: e.tensor_scalar(out=t[:], in0=t[:], scalar1=3.14159, scalar2=-3.14159, op0=ALU.min, op1=ALU.max), reads=[tk], writes=[tk])
            S.op("act", lambda e, h=h, t=t: e.activation(out=h[:], in_=t[:], func=AF.Sin), reads=[tk], writes=[hk])
            cur, curk, kin = h, hk, 64
        po, pok = pP.next()
        col0 = 0 if b < (L // 512) else 128
        S.op("pe", lambda e, po=po, cur=cur, col0=col0: e.matmul(po[:], w4[:, col0:col0 + 128], cur[:], start=True, stop=True), reads=["fw", curk], writes=[pok])
        S.op("act", lambda e, tn=tn: e.activation(out=tn[:], in_=tn[:], func=AF.Exp, scale=ndel[:, 0:1]), reads=[tnk, "ndel"], writes=[tnk])
        tpp, tpk = tp.next()
        S.op("dve", lambda e, tpp=tpp, po=po, tn=tn: e.tensor_tensor(out=tpp[:], in0=po[:], in1=tn[:], op=ALU.mult), reads=[pok, tnk], writes=[tpk])
        S.dma("sp", taps_d[:, b * 512:(b + 1) * 512], tpp[:], reads=[tpk], writes=[("taps", b)])
    if upto < 3:
        S.finish([]); es.close(); return nc
    S.barrier(); ar.release(m0)
    if limit3 is not None:
        S.nops = 0; S.limit = limit3
    F1 = ar.alloc([128, 2, 512]); S.dma("sp", F1[:], F1_d, writes=["tab"]); TW = ar.alloc([128, 512]); S.dma("sp", TW[:], TW_d, writes=["tab"])
    F2 = ar.alloc([128, 3, 128]); S.dma("sp", F2[:], F2_d, writes=["tab"]); Gt = ar.alloc([128, 2, 256]); S.dma("sp", Gt[:], Gt_d, writes=["tab"])
    ITW = ar.alloc([128, 2, 256]); S.dma("sp", ITW[:], ITW_d, writes=["tab"]); F3 = ar.alloc([128, 2, 2, 128]); S.dma("sp", F3[:], F3_d, writes=["tab"])
    xv = ar.alloc([128, G, 128]); xh = ar.alloc([128, 2, G, 128])
    B = {n: ar.alloc([128, G, 256]) for n in ["yre", "yim", "tre", "tim", "ta", "tb", "vre", "vim", "hre", "him"]}
    zre = ar.alloc([128, 2, G, 128]); zim = ar.alloc([128, 2, G, 128]); zr2 = ar.alloc([128, 2, G, 128]); zi2 = ar.alloc([128, 2, G, 128])
    zta = ar.alloc([128, 2, G, 128]); ztb = ar.alloc([128, 2, G, 128])
    yo = ar.alloc([128, G, 128])
    twc = bcast_ap(TW[:, 0:256], G); tws = bcast_ap(TW[:, 256:512], G)

    def fwd_fft(src_fn, nchunk, ore, oim, tag):
        for c in range(G):
            pp, ppk = pP.next()
            for q in range(nchunk):
                S.op("pe", lambda e, pp=pp, c=c, q=q: e.matmul(pp[:], src_fn(c, q), F1[:, q, :], start=(q == 0), stop=(q == nchunk - 1)), reads=[tag, "tab"], writes=[ppk])
            if c % 2 == 0:
                S.op("act", lambda e, pp=pp, c=c: e.copy(out=B["yre"][:, c, :], in_=pp[:, 0:256]), reads=[ppk], writes=["yre"])
                S.op("act", lambda e, pp=pp, c=c: e.copy(out=B["yim"][:, c, :], in_=pp[:, 256:512]), reads=[ppk], writes=["yim"])
            else:
                S.op("dve", lambda e, pp=pp, c=c: e.tensor_copy(out=B["yre"][:, c, :], in_=pp[:, 0:256]), reads=[ppk], writes=["yre"])
                S.op("dve", lambda e, pp=pp, c=c: e.tensor_copy(out=B["yim"][:, c, :], in_=pp[:, 256:512]), reads=[ppk], writes=["yim"])
        S.op("dve", lambda e: e.tensor_tensor(out=B["ta"][:], in0=B["yre"][:], in1=twc, op=ALU.mult), reads=["yre", "tab"], writes=["ta"])
        S.op("pool", lambda e: e.tensor_tensor(out=B["tb"][:], in0=B["yim"][:], in1=tws, op=ALU.mult), reads=["yim", "tab"], writes=["tb"])
        S.op("dve", lambda e: e.tensor_tensor(out=B["tre"][:], in0=B["ta"][:], in1=B["tb"][:], op=ALU.add), reads=["ta", "tb"], writes=["tre"])
        S.op("dve", lambda e: e.tensor_tensor(out=B["ta"][:], in0=B["yim"][:], in1=twc, op=ALU.mult), reads=["yim", "tab", "tre"], writes=["ta"])
        S.op("pool", lambda e: e.tensor_tensor(out=B["tb"][:], in0=B["yre"][:], in1=tws, op=ALU.mult), reads=["yre", "tab", "tre"], writes=["tb"])
        S.op("dve", lambda e: e.tensor_tensor(out=B["tim"][:], in0=B["ta"][:], in1=B["tb"][:], op=ALU.subtract), reads=["ta", "tb"], writes=["tim"])
        for j in range(G // 2):
            rre = B["tre"][:, 2 * j:2 * j + 2, :].rearrange("p c k -> p (c k)"); rim = B["tim"][:, 2 * j:2 * j + 2, :].rearrange("p c k -> p (c k)")
            p1, p1k = pP.next(); p2, p2k = pP.next()
            S.op("pe", lambda e, p1=p1, rre=rre: e.matmul(p1[:], F2[:, 0, :], rre, start=True, stop=False), reads=["tre", "tab"], writes=[p1k])
            S.op("pe", lambda e, p1=p1, rim=rim: e.matmul(p1[:], F2[:, 1, :], rim, start=False, stop=True), reads=["tim", "tab"], writes=[p1k])
            S.op("pe", lambda e, p2=p2, rim=rim: e.matmul(p2[:], F2[:, 0, :], rim, start=True, stop=False), reads=["tim", "tab"], writes=[p2k])
            S.op("pe", lambda e, p2=p2, rre=rre: e.matmul(p2[:], F2[:, 2, :], rre, start=False, stop=True), reads=["tre", "tab"], writes=[p2k])
            S.op("act", lambda e, p1=p1, j=j: e.copy(out=ore[:, 2 * j:2 * j + 2, :].rearrange("p c k -> p (c k)"), in_=p1[:]), reads=[p1k], writes=[tag + "re"])
            S.op("dve", lambda e, p2=p2, j=j: e.tensor_copy(out=oim[:, 2 * j:2 * j + 2, :].rearrange("p c k -> p (c k)"), in_=p2[:]), reads=[p2k], writes=[tag + "im"])

    for g in range(ngroups if ngroups else 128 // G):
        cs = slice(g * G, (g + 1) * G)
        S.dma("sp", xv[:], vx_d[cs, :].rearrange("c (n2 n1) -> n2 c n1", n1=128), reads=[("vx", b) for b in range(L // TB)], writes=["xv"])
        for q in range(2):
            S.dma("act", xh[:, q], taps_d[cs, q * L:(q + 1) * L].rearrange("c (n2 n1) -> n2 c n1", n1=128), writes=["xh"])
        fwd_fft(lambda c, q: xv[:, c, :], 1, B["vre"], B["vim"], "xv")
        fwd_fft(lambda c, q: xh[:, q, c, :], 2, B["hre"], B["him"], "xh")
        S.op("dve", lambda e: e.tensor_tensor(out=B["ta"][:], in0=B["vre"][:], in1=B["hre"][:], op=ALU.mult), reads=["xvre", "xhre", "tre", "tim"], writes=["ta"])
        S.op("pool", lambda e: e.tensor_tensor(out=B["tb"][:], in0=B["vim"][:], in1=B["him"][:], op=ALU.mult), reads=["xvim", "xhim", "tre", "tim"], writes=["tb"])
        S.op("dve", lambda e: e.tensor_tensor(out=B["tre"][:], in0=B["ta"][:], in1=B["tb"][:], op=ALU.subtract), reads=["ta", "tb"], writes=["tre"])
        S.op("dve", lambda e: e.tensor_tensor(out=B["ta"][:], in0=B["vre"][:], in1=B["him"][:], op=ALU.mult), reads=["xvre", "xhim", "tre"], writes=["ta"])
        S.op("pool", lambda e: e.tensor_tensor(out=B["tb"][:], in0=B["vim"][:], in1=B["hre"][:], op=ALU.mult), reads=["xvim", "xhre", "tre"], writes=["tb"])
        S.op("dve", lambda e: e.tensor_tensor(out=B["tim"][:], in0=B["ta"][:], in1=B["tb"][:], op=ALU.add), reads=["ta", "tb"], writes=["tim"])
        for c in range(G):
            for q in range(2):
                pz, pzk = pP.next()
                S.op("pe", lambda e, pz=pz, c=c, q=q: e.matmul(pz[:, 0:256], B["tre"][:, c, q * 128:(q + 1) * 128], Gt[:, 0, :], start=True, stop=False), reads=["tre", "tab"], writes=[pzk])
                S.op("pe", lambda e, pz=pz, c=c, q=q: e.matmul(pz[:, 0:256], B["tim"][:, c, q * 128:(q + 1) * 128], Gt[:, 1, :], start=False, stop=True), reads=["tim", "tab"], writes=[pzk])
                if q == 0:
                    S.op("act", lambda e, pz=pz, c=c, q=q: e.copy(out=zre[:, q, c, :], in_=pz[:, 0:128]), reads=[pzk], writes=["zre"])
                    S.op("act", lambda e, pz=pz, c=c, q=q: e.copy(out=zim[:, q, c, :], in_=pz[:, 128:256]), reads=[pzk], writes=["zim"])
                else:
                    S.op("dve", lambda e, pz=pz, c=c, q=q: e.tensor_copy(out=zre[:, q, c, :], in_=pz[:, 0:128]), reads=[pzk], writes=["zre"])
                    S.op("dve", lambda e, pz=pz, c=c, q=q: e.tensor_copy(out=zim[:, q, c, :], in_=pz[:, 128:256]), reads=[pzk], writes=["zim"])
        for q in range(2):
            ic = bcast_ap(ITW[:, q, 0:128], G); isn = bcast_ap(ITW[:, q, 128:256], G)
            S.op("dve", lambda e, q=q, ic=ic: e.tensor_tensor(out=zta[:, q], in0=zre[:, q], in1=ic, op=ALU.mult), reads=["zre", "tab"], writes=[("zta", q)])
            S.op("pool", lambda e, q=q, isn=isn: e.tensor_tensor(out=ztb[:, q], in0=zim[:, q], in1=isn, op=ALU.mult), reads=["zim", "tab"], writes=[("ztb", q)])
            S.op("dve", lambda e, q=q: e.tensor_tensor(out=zr2[:, q], in0=zta[:, q], in1=ztb[:, q], op=ALU.subtract), reads=[("zta", q), ("ztb", q)], writes=[("zr2", q)])
            S.op("dve", lambda e, q=q, isn=isn: e.tensor_tensor(out=zta[:, q], in0=zre[:, q], in1=isn, op=ALU.mult), reads=["zre", "tab", ("zr2", q)], writes=[("zta", q)])
            S.op("pool", lambda e, q=q, ic=ic: e.tensor_tensor(out=ztb[:, q], in0=zim[:, q], in1=ic, op=ALU.mult), reads=["zim", "tab", ("zr2", q)], writes=[("ztb", q)])
            S.op("dve", lambda e, q=q: e.tensor_tensor(out=zi2[:, q], in0=zta[:, q], in1=ztb[:, q], op=ALU.add), reads=[("zta", q), ("ztb", q)], writes=[("zi2", q)])
        for j in range(G // 4):
            py, pyk = pP.next()
            n = 0
            for q in range(2):
                for (zz, zk, ri) in ((zr2, "zr2", 0), (zi2, "zi2", 1)):
                    rhs = zz[:, q, 4 * j:4 * j + 4, :].rearrange("p c m -> p (c m)")
                    S.op("pe", lambda e, py=py, q=q, ri=ri, rhs=rhs, n=n: e.matmul(py[:], F3[:, q, ri, :], rhs, start=(n == 0), stop=(n == 3)), reads=[(zk, q), "tab"], writes=[pyk])
                    n += 1
            S.op("act", lambda e, py=py, j=j: e.copy(out=yo[:, 4 * j:4 * j + 4, :].rearrange("p c m -> p (c m)"), in_=py[:]), reads=[pyk], writes=["yo"])
        S.dma("sp", y_d[cs, :].rearrange("c (m2 m1) -> m2 c m1", m1=128), yo[:], reads=["yo"], writes=[("y", g)])
    if upto < 4:
        S.finish([]); es.close(); return nc
    S.barrier(); ar.release(m0)
    fa = RotA(ar, "fa", [128, TB], n=2); fbb = RotA(ar, "fbb", [128, TB], n=2); fc = RotA(ar, "fc", [128, TB], n=2); fo = RotA(ar, "fo", [128, TB], BF16, n=2)
    for b in range(L // TB):
        t0 = b * TB
        a, ak = fa.next(); bb, bk = fbb.next(); c, ck = fc.next(); o, ok = fo.next()
        S.dma("sp", a[:], y_d[:, t0:t0 + TB], writes=[ak]); S.dma("act", bb[:], vx_d[:, t0:t0 + TB], writes=[bk]); S.dma("sp", c[:], x0_d[:, t0:t0 + TB], writes=[ck])
        S.op("dve", lambda e, a=a, bb=bb: e.scalar_tensor_tensor(out=a[:], in0=bb[:], scalar=skip[:, 0:1], in1=a[:], op0=ALU.mult, op1=ALU.add), reads=[ak, bk, "skip"], writes=[ak])
        S.op("pool", lambda e, a=a, c=c, o=o: e.tensor_tensor(out=o[:], in0=a[:], in1=c[:], op=ALU.mult), reads=[ak, ck], writes=[ok])
        S.dma("sp", out_d[:, t0:t0 + TB], o[:], reads=[ok], writes=[("o", b)])
    S.finish([]); es.close()
    return nc


def hy_tables(L=16384):
    N = 2 * L
    f32 = np.float32
    p = np.arange(128)
    n2 = (np.arange(2)[:, None] * 128 + p[None, :]).T
    k2 = np.arange(256)
    ang = 2 * np.pi * (n2[:, :, None] * k2[None, None, :] % 256) / 256
    F1 = np.concatenate([np.cos(ang), -np.sin(ang)], -1).astype(f32)
    th = 2 * np.pi * (p[:, None] * k2[None, :]) / N
    TW = np.concatenate([np.cos(th), np.sin(th)], -1).astype(f32)
    a2 = 2 * np.pi * (p[:, None] * p[None, :] % 128) / 128
    F2 = np.stack([np.cos(a2), np.sin(a2), -np.sin(a2)], 1).astype(f32)
    Gt = np.stack([np.concatenate([np.cos(a2), np.sin(a2)], -1), np.concatenate([-np.sin(a2), np.cos(a2)], -1)], 1).astype(f32)
    ph = 2 * np.pi * (n2[:, :, None] * p[None, None, :]) / N
    ITW = np.concatenate([np.cos(ph), np.sin(ph)], -1).astype(f32)
    a3 = 2 * np.pi * (n2[:, :, None] * p[None, None, :] % 256) / 256
    F3 = np.stack([np.cos(a3) / N, -np.sin(a3) / N], 2).astype(f32)
    lag = np.concatenate([np.arange(L), L - np.arange(L)]).astype(np.int64)
    t = np.linspace(0.0, 1.0, L, dtype=f32)
    w = (2.0 * np.pi / L) * np.arange(L, dtype=f32)
    f = np.linspace(1e-4, 15, 16, dtype=f32)
    feat = np.concatenate([t[:, None], np.cos(f * w[:, None]), -np.sin(f * w[:, None])], -1).astype(f32)
    lagc = np.minimum(lag, L - 1)
    zfeat = np.ascontiguousarray(feat[lagc].T)
    tnorm = t[lagc].copy(); tnorm[L] = 1e6
    return dict(F1=F1, TW=TW, F2=F2, Gt=Gt, ITW=ITW, F3=F3, zfeat=zfeat, tnorm=tnorm.astype(f32))


import math
import ml_dtypes
from concourse.bass_utils import run_bass_kernel_spmd

NCORES = 8
_BF = ml_dtypes.bfloat16


def _run(build, maps, **kw):
    import sys, time
    t0 = time.time()
    nc = bass.Bass("TRN2", target_bir_lowering=False)
    build(nc, **kw)
    res = run_bass_kernel_spmd(nc, maps, core_ids=list(range(len(maps))))
    print("[launch %s] %.1fs" % (build.__name__, time.time() - t0), file=sys.stderr, flush=True)
    return res.results


def _rope_tables(L, W=64):
    row = np.repeat(np.arange(L // W), W); col = np.tile(np.arange(W), L // W)
    pos = np.stack([row, col], -1).astype(np.float32)
    inv = (np.float32(10000.0) ** (-np.arange(32, dtype=np.float32) / np.float32(32))).astype(np.float32)
    ang = pos[:, :, None] * inv
    return np.cos(ang).astype(np.float32).reshape(L, 64), np.sin(ang).astype(np.float32).reshape(L, 64)


def kernel(x, c, ctx, c_ctx, ada_w, ada_b, norm1_g, norm2_g, ev_w_in, ev_b_in, ev_q_gain, ev_k_gain,
           ev_f_bias, ev_h_gain, ev_w_out, ev_b_out, od_w_in, od_b_in, od_conv_w, od_conv_b, od_filt_w1,
           od_filt_b1, od_filt_freq, od_filt_w2, od_filt_b2, od_filt_w3, od_filt_b3, od_filt_w4, od_skip,
           od_w_out, od_b_out, router_w, router_b, moe_w_gu, moe_b_gu, moe_w_down, moe_b_down, final_g):
    f32 = np.float32
    A = lambda a: np.ascontiguousarray(np.asarray(a))
    x = np.asarray(x, f32); ctx = np.asarray(ctx, f32)
    L = x.shape[1]; TQ = L // NCORES; NCT = ctx.shape[1]
    ident = np.eye(128, dtype=f32)
    W = 6 * D // NCORES
    r = _run(build_ada, [dict(c=A(c), c_ctx=A(c_ctx), ada_w=A(ada_w[:, :, i * W:(i + 1) * W]), ada_b=A(ada_b[:, i * W:(i + 1) * W]), ident=ident) for i in range(NCORES)])
    modv = np.concatenate([q["modv"] for q in r], axis=-1)
    cosf, sinf = _rope_tables(L)
    mv0 = A(modv[0].reshape(2, 6, D))
    r = _run(build_proj0, [dict(x=A(x[0, i * TQ:(i + 1) * TQ]), ctx=A(ctx[0]), modv=mv0, norm1_g=A(norm1_g[0]), w_in=A(ev_w_in[0]), b_in=A(ev_b_in[0]),
                                q_gain=A(ev_q_gain[0]), k_gain=A(ev_k_gain[0]), cos=A(cosf[i * TQ:(i + 1) * TQ]), sin=A(sinf[i * TQ:(i + 1) * TQ]), ident=ident)
                           for i in range(NCORES)], TL=TQ, TC=NCT)
    QT = [q["QT"] for q in r]
    KT_all = A(np.concatenate([q["KT"][:, :, :TQ] for q in r] + [r[0]["KT"][:, :, TQ:]], axis=2))
    V_all = A(np.concatenate([q["V"][:TQ] for q in r] + [r[0]["V"][TQ:]], axis=0))
    PM_lat = np.concatenate([q["PM"][:TQ] for q in r], axis=0); PM_ctx = r[0]["PM"][TQ:]
    tri = np.triu(np.ones((128, 128), f32))
    maps = []
    for d in range(2):
        for hh in range(4):
            def seq(cols):
                a, b = PM_ctx[:, cols], PM_lat[:, cols]
                if d == 1:
                    a, b = a[::-1], b[::-1]
                return np.concatenate([a, b], axis=0)
            q_ = seq(slice(hh * 64, (hh + 1) * 64)); k_ = seq(slice(256 + hh * 64, 256 + (hh + 1) * 64)); v_ = seq(slice(512 + hh * 128, 512 + (hh + 1) * 128))
            ig_ = seq(slice(1024 + d * 4 + hh, 1024 + d * 4 + hh + 1))[:, 0]; fg_ = seq(slice(1032 + d * 4 + hh, 1032 + d * 4 + hh + 1))[:, 0]
            NCk = q_.shape[0] // 128
            maps.append(dict(qT=A(q_.T), kT=A(k_.T), k=A(k_), v=A(v_), ig=A(ig_.reshape(NCk, 128).T), fg=A(fg_.reshape(NCk, 128).T),
                             fb=np.full((128, 1), ev_f_bias[0, d, hh], f32), tri=tri, ident=ident))
    r = _run(build_mlstm, maps, NS=L + NCT, NCTX=NCT // 128)
    hf = np.concatenate([r[hh]["h"] for hh in range(4)], axis=1)
    hb = np.concatenate([r[4 + hh]["h"][::-1] for hh in range(4)], axis=1)
    r = _run(build_attn, [dict(QT=QT[i], KT=KT_all, V=V_all, hf=A(hf[i * TQ:(i + 1) * TQ]), hb=A(hb[i * TQ:(i + 1) * TQ]), og=A(PM_lat[i * TQ:(i + 1) * TQ, 1040:1552]),
                               h_gain=A(ev_h_gain[0]), ident=ident) for i in range(NCORES)], TQ=TQ, NK=L + NCT)
    mix0 = [q["mixT"] for q in r]

    NPC = 8
    TP = L // NPC

    def post(layer, hs, mix, w_out, b_out, final):
        hcat = np.concatenate(hs, axis=0); mcat = np.concatenate(mix, axis=1)
        r_ = _run(build_post, [dict(h=A(hcat[i * TP:(i + 1) * TP]), mixT=A(mcat[:, i * TP:(i + 1) * TP]), w_out=A(w_out), b_out=A(b_out), norm2_g=A(norm2_g[layer]),
                                    modv=A(modv[layer, 0].reshape(6, D)), router_w=A(router_w[layer]), router_b=A(router_b[layer]), w_gu=A(moe_w_gu[layer]),
                                    b_gu_fm=A(np.asarray(moe_b_gu[layer]).reshape(NE, 16, 128).transpose(2, 1, 0)), w_down=A(moe_w_down[layer]), b_down=A(moe_b_down[layer]), final_g=A(final_g), ident=ident)
                               for i in range(NPC)], T=TP, TBM=2048, NEXP=NE, final=final)
        hall = np.concatenate([q["hout"] for q in r_], axis=0)
        return [dict(hout=A(hall[i * TQ:(i + 1) * TQ])) for i in range(NCORES)]
    r = post(0, [A(x[0, i * TQ:(i + 1) * TQ]) for i in range(NCORES)], mix0, ev_w_out[0], ev_b_out[0], False)
    h1 = [q["hout"] for q in r]
    r = _run(build_hyproj, [dict(h=h1[i], modv=A(modv[1, 0].reshape(6, D)), norm1_g=A(norm1_g[1]), w_in=A(od_w_in[0]), b_in_fm=A(od_b_in[0].reshape(24, 128).T), ident=ident)
                            for i in range(NCORES)], T=TQ)
    z0T = np.concatenate([q["z0T"] for q in r], axis=1)
    tabs = hy_tables(L)
    deltas = np.abs(np.linspace(math.log(1e-2) / 1.5, math.log(1e-2) / 0.3, D, dtype=f32))
    maps = []
    for j in range(NCORES):
        ch = slice(j * 128, (j + 1) * 128)
        cw = np.stack([od_conv_w[0][:, g * D + j * 128:g * D + (j + 1) * 128] for g in range(3)], 0)
        maps.append(dict(z0=A(np.stack([z0T[g * D + j * 128:g * D + (j + 1) * 128] for g in range(3)], 0)), cw=A(cw.transpose(2, 0, 1).reshape(128, 9)),
                         cb=A(np.stack([od_conv_b[0][g * D + j * 128:g * D + (j + 1) * 128] for g in range(3)], 1)), skip=A(od_skip[0][ch].reshape(128, 1)),
                         ndelta=A((-deltas[ch]).reshape(128, 1).astype(f32)), fw1=A(od_filt_w1[0]), fw2=A(od_filt_w2[0]), fw3=A(od_filt_w3[0]),
                         fw4=A(np.concatenate([od_filt_w4[0][:, ch], od_filt_w4[0][:, D + j * 128:D + (j + 1) * 128]], 1)),
                         fbf=A(np.stack([od_filt_freq[0], od_filt_b1[0], od_filt_b2[0], od_filt_b3[0]], 1)), **tabs))
    r = _run(build_hyconv, maps, L=L)
    zT = np.concatenate([q["zT"] for q in r], axis=0)
    mix1 = [A(zT[:, i * TQ:(i + 1) * TQ]) for i in range(NCORES)]
    r = post(1, h1, mix1, od_w_out[0], od_b_out[0], True)
    out = np.concatenate([q["hout"] for q in r], axis=0)[None]
    return out.astype(np.float32)
```
